# Optimizing a Trainium2 kernel written in Bass

```python
import math
import jax, jax.numpy as jnp
from jax import lax
import numpy as np

D_MODEL = 2048
BATCH = 4
SEQ = 2048
DEPTH = 1
DEC_BATCH = 128
DEC_SEQ = 4
PAST_LEN = 16384
PAGE_SIZE = 128

RET_HEADS = 8
RET_DK = 128
RET_DV = 256
RET_CHUNK = 128
ROPE_BASE = 10000.0
RET_Q = RET_HEADS * RET_DK
RET_V = RET_HEADS * RET_DV
WKV_HEADS = 16
WKV_N = 64
WKV_DIM = WKV_HEADS * WKV_N
W_LORA = 64
A_LORA = 64
G_LORA = 160
WKV_GN_EPS = 64e-5
RWKV_PROJ = 3 * WKV_DIM + W_LORA + A_LORA + G_LORA
IN_COLS = 2 * RET_Q + 2 * RET_V + RWKV_PROJ + 2 * D_MODEL
N_GROUPS = 4
EXPERTS_PER_GROUP = 8
N_EXPERTS = N_GROUPS * EXPERTS_PER_GROUP
TOP_K = 2
D_EXPERT = 1024
MOE_BLOCK = 128
D_PLE = 256
RMS_EPS = 1e-6

kernel_name = "hybrid_retention_rwkv7_hmoe_step"


def rmsnorm(x, g, eps=RMS_EPS):
    xf = x.astype(jnp.float32)
    y = xf * lax.rsqrt(jnp.mean(xf * xf, -1, keepdims=True) + eps)
    return (y * g.astype(jnp.float32)).astype(x.dtype)


def split_cols(z, sizes):
    offs = np.cumsum((0,) + tuple(sizes))
    return [z[..., int(offs[j]):int(offs[j + 1])] for j in range(len(sizes))]


def rotary(x, pos):
    half = x.shape[-1] // 2
    inv = ROPE_BASE ** (-jnp.arange(half, dtype=jnp.float32) / half)
    ang = pos.astype(jnp.float32)[:, None] * inv[None, :]
    cos = jnp.cos(ang)[None, :, None, :]
    sin = jnp.sin(ang)[None, :, None, :]
    x1, x2 = x[..., :half], x[..., half:]
    return jnp.concatenate([x1 * cos - x2 * sin, x1 * sin + x2 * cos], -1)


def retention_chunked(q, k, v, s0):
    B, L = q.shape[0], q.shape[1]
    C = RET_CHUNK if L % RET_CHUNK == 0 else L
    n = L // C
    log_g = jnp.log1p(-jnp.exp2(-5.0 - jnp.arange(RET_HEADS, dtype=jnp.float32)))
    idx = jnp.arange(C, dtype=jnp.float32)
    diff = idx[:, None] - idx[None, :]
    decay_mask = jnp.where(diff >= 0, jnp.exp(log_g[:, None, None] * jnp.maximum(diff, 0.0)), 0.0)
    q_decay = jnp.exp(log_g[:, None] * (idx + 1.0))[..., None]
    k_decay = jnp.exp(log_g[:, None] * (C - 1.0 - idx))[..., None]
    chunk_decay = jnp.exp(log_g * C)[:, None, None]

    def to_chunks(t):
        return t.reshape(B, n, C, RET_HEADS, t.shape[-1]).transpose(1, 0, 3, 2, 4)

    def step(s, inp):
        qc, kc, vc = inp
        scores = jnp.einsum('bhid,bhjd->bhij', qc, kc) * decay_mask
        o = (jnp.einsum('bhij,bhje->bhie', scores, vc)
             + jnp.einsum('bhid,bhde->bhie', qc * q_decay, s))
        s = s * chunk_decay + jnp.einsum('bhjd,bhje->bhde', kc * k_decay, vc)
        return s, o

    s, o = lax.scan(step, s0, (to_chunks(q), to_chunks(k), to_chunks(v)))
    o = o.transpose(1, 0, 3, 2, 4).reshape(B, L, RET_HEADS, RET_DV)
    return o, s


def wkv7_scan(r, w, k, v, a, b, s0):
    def step(s, inp):
        rt, wt, kt, vt, at, bt = inp
        sa = jnp.einsum('bhij,bhj->bhi', s, at)
        s = s * wt[:, :, None, :] + sa[..., None] * bt[:, :, None, :] + vt[..., None] * kt[:, :, None, :]
        y = jnp.einsum('bhij,bhj->bhi', s, rt)
        return s, y

    xs = tuple(t.transpose(1, 0, 2, 3) for t in (r, w, k, v, a, b))
    s, y = lax.scan(step, s0, xs)
    return y.transpose(1, 0, 2, 3), s


def rwkv7_branch(zb, shift0, s0, mu, w0, w2, a0, a2, g2, k_k, k_a, r_k, ln_w, ln_b):
    B, L = zb.shape[0], zb.shape[1]
    prev = jnp.concatenate([shift0[:, None, :].astype(zb.dtype), zb[:, :-1]], 1)
    u = (zb + (prev - zb) * mu).astype(jnp.float32)
    r, kx, vx, wd, ad, gd = split_cols(u, (WKV_DIM, WKV_DIM, WKV_DIM, W_LORA, A_LORA, G_LORA))
    w_log = -jax.nn.softplus(-(w0 + jnp.tanh(wd) @ w2)) - 0.5
    decay = jnp.exp(-jnp.exp(w_log))
    a = jax.nn.sigmoid(a0 + ad @ a2)
    g = jax.nn.sigmoid(gd) @ g2

    def heads(t):
        return t.reshape(B, L, WKV_HEADS, WKV_N)

    kk = heads(kx * k_k)
    kk = kk / jnp.maximum(jnp.sqrt(jnp.sum(kk * kk, -1, keepdims=True)), 1e-12)
    kmod = kx * (1.0 + (a - 1.0) * k_a)
    rh, kh, vh, ah = heads(r), heads(kmod), heads(vx), heads(a)
    y, s = wkv7_scan(rh, heads(decay), kh, vh, -kk, kk * ah, s0)
    mean = jnp.mean(y, -1, keepdims=True)
    var = jnp.mean(jnp.square(y - mean), -1, keepdims=True)
    yn = ((y - mean) * lax.rsqrt(var + WKV_GN_EPS)).reshape(B, L, WKV_DIM) * ln_w + ln_b
    bonus = (jnp.sum(rh * kh * r_k, -1, keepdims=True) * vh).reshape(B, L, WKV_DIM)
    out = (yn + bonus) * g
    return out, s, zb[:, -1]


def hier_moe(h, rg_w, rg_b, re_w, re_b, e_gate, e_up, e_down):
    T = h.shape[0]
    hf = h.astype(jnp.float32)
    gp = jax.nn.softmax(hf @ rg_w.astype(jnp.float32) + rg_b.astype(jnp.float32), -1)
    g_idx = jnp.argmax(gp, -1).astype(jnp.int32)
    g_prob = jnp.max(gp, -1)
    el = (hf @ re_w.astype(jnp.float32) + re_b.astype(jnp.float32)).reshape(T, N_GROUPS, EXPERTS_PER_GROUP)
    sel = jnp.broadcast_to(g_idx[:, None, None], (T, 1, EXPERTS_PER_GROUP))
    el = jnp.take_along_axis(el, sel, axis=1)[:, 0]
    ep = jax.nn.softmax(el, -1)
    top_v, top_i = lax.top_k(ep, TOP_K)
    wts = g_prob[:, None] * top_v / jnp.sum(top_v, -1, keepdims=True)
    e_idx = g_idx[:, None] * EXPERTS_PER_GROUP + top_i.astype(jnp.int32)

    A = T * TOP_K
    flat_e = e_idx.reshape(-1)
    order = jnp.argsort(flat_e).astype(jnp.int32)
    sorted_e = flat_e[order]
    counts = jnp.zeros((N_EXPERTS,), jnp.int32).at[flat_e].add(1)
    padded = (counts + MOE_BLOCK - 1) // MOE_BLOCK * MOE_BLOCK
    pad_end = jnp.cumsum(padded)
    pad_start = pad_end - padded
    start = jnp.cumsum(counts) - counts
    dest_sorted = pad_start[sorted_e] + jnp.arange(A, dtype=jnp.int32) - start[sorted_e]
    n_blocks = -(-A // MOE_BLOCK) + N_EXPERTS
    cap = n_blocks * MOE_BLOCK
    slot_token = jnp.full((cap,), T, jnp.int32).at[dest_sorted].set(order // TOP_K)
    block_start = jnp.arange(n_blocks, dtype=jnp.int32) * MOE_BLOCK
    block_expert = jnp.minimum(jnp.searchsorted(pad_end, block_start, side='right'), N_EXPERTS - 1).astype(jnp.int32)
    h_pad = jnp.concatenate([h, jnp.zeros((1, h.shape[1]), h.dtype)], 0)
    xb = h_pad[slot_token].reshape(n_blocks, MOE_BLOCK, h.shape[1])

    def expert_block(args):
        xe, e = args
        return (jax.nn.silu(xe @ e_gate[e]) * (xe @ e_up[e])) @ e_down[e]

    yb = lax.map(expert_block, (xb, block_expert)).reshape(cap, h.shape[1])
    slot_of_assign = jnp.zeros((A,), jnp.int32).at[order].set(dest_sorted)
    y = yb[slot_of_assign].reshape(T, TOP_K, h.shape[1])
    return jnp.einsum('tkd,tk->td', y, wts.astype(y.dtype))


def trunk_layer(x, p, pos, s_ret, s_wkv, s_shift,
                g_mix, w_in, w_oa, mu, w0, w2, a0, a2, g2, k_k, k_a, r_k, ln_w, ln_b, w_ob, w_out,
                g_ffn, rg_w, rg_b, re_w, re_b, e_gate, e_up, e_down, g_ple, w_ple_gate, w_ple_proj):
    B, L = x.shape[0], x.shape[1]
    h = rmsnorm(x, g_mix)
    z = h @ w_in
    q, k, v, gr, zb, gates = split_cols(z, (RET_Q, RET_Q, RET_V, RET_V, RWKV_PROJ, 2 * D_MODEL))
    q = rotary(q.reshape(B, L, RET_HEADS, RET_DK).astype(jnp.float32), pos)
    k = rotary(k.reshape(B, L, RET_HEADS, RET_DK).astype(jnp.float32), pos) * (RET_DK ** -0.5)
    v = v.reshape(B, L, RET_HEADS, RET_DV).astype(jnp.float32)
    o, ret_new = retention_chunked(q, k, v, s_ret.astype(jnp.float32))
    o = o * lax.rsqrt(jnp.mean(o * o, -1, keepdims=True) + RMS_EPS)
    o = o.reshape(B, L, RET_V) * jax.nn.silu(gr.astype(jnp.float32))
    y_a = o.astype(x.dtype) @ w_oa
    ob, wkv_new, shift_new = rwkv7_branch(zb, s_shift, s_wkv.astype(jnp.float32), mu, w0, w2, a0, a2, g2,
                                          k_k, k_a, r_k, ln_w, ln_b)
    y_b = ob.astype(x.dtype) @ w_ob
    g_a, g_b = split_cols(gates, (D_MODEL, D_MODEL))
    x = x + (jax.nn.sigmoid(g_a) * y_a + jax.nn.sigmoid(g_b) * y_b) @ w_out
    h2 = rmsnorm(x, g_ffn)
    x = x + hier_moe(h2.reshape(B * L, D_MODEL), rg_w, rg_b, re_w, re_b, e_gate, e_up, e_down).reshape(B, L, D_MODEL)
    h3 = rmsnorm(x, g_ple)
    x = x + jax.nn.sigmoid(h3 @ w_ple_gate) * (p.astype(x.dtype) @ w_ple_proj)
    return x, ret_new, wkv_new, shift_new


def setup_inputs(seed: int = 0) -> dict:
    key = jax.random.key(seed)
    ks = iter(jax.random.split(key, 48))

    def nrm(shape, scale):
        return jax.random.normal(next(ks), shape, jnp.float32) * scale

    def gain(shape):
        return 1.0 + 0.05 * jax.random.normal(next(ks), shape, jnp.float32)

    def unif(shape, lo, hi):
        return jax.random.uniform(next(ks), shape, jnp.float32, lo, hi)

    D = D_MODEL
    return {
        'x_prompt': nrm((BATCH, SEQ, D), 1.0),
        'x_sample': nrm((DEC_BATCH, DEC_SEQ, D), 1.0),
        'state_ret': nrm((DEPTH, DEC_BATCH, RET_HEADS, RET_DK, RET_DV), 0.5),
        'state_wkv': nrm((DEPTH, DEC_BATCH, WKV_HEADS, WKV_N, WKV_N), 0.3),
        'state_shift': nrm((DEPTH, DEC_BATCH, RWKV_PROJ), 1.0),
        'p_prompt': nrm((DEPTH, BATCH, SEQ, D_PLE), 1.0),
        'p_sample': nrm((DEPTH, DEC_BATCH, DEC_SEQ, D_PLE), 1.0),
        'g_mix': gain((DEPTH, D)),
        'w_in': nrm((DEPTH, D, IN_COLS), D ** -0.5),
        'w_oa': nrm((DEPTH, RET_V, D), RET_V ** -0.5),
        'wkv_mu': unif((DEPTH, RWKV_PROJ), 0.0, 1.0),
        'wkv_w0': unif((DEPTH, WKV_DIM), -6.0, 0.0),
        'wkv_w2': nrm((DEPTH, W_LORA, WKV_DIM), 0.1),
        'wkv_a0': nrm((DEPTH, WKV_DIM), 0.1),
        'wkv_a2': nrm((DEPTH, A_LORA, WKV_DIM), A_LORA ** -0.5),
        'wkv_g2': nrm((DEPTH, G_LORA, WKV_DIM), G_LORA ** -0.5),
        'wkv_k_k': 0.85 + nrm((DEPTH, WKV_DIM), 0.05),
        'wkv_k_a': gain((DEPTH, WKV_DIM)),
        'wkv_r_k': nrm((DEPTH, WKV_HEADS, WKV_N), 0.1),
        'wkv_ln_w': gain((DEPTH, WKV_DIM)),
        'wkv_ln_b': nrm((DEPTH, WKV_DIM), 0.01),
        'w_ob': nrm((DEPTH, WKV_DIM, D), WKV_DIM ** -0.5),
        'w_out': nrm((DEPTH, D, D), D ** -0.5),
        'g_ffn': gain((DEPTH, D)),
        'router_g_w': nrm((DEPTH, D, N_GROUPS), D ** -0.5),
        'router_g_b': nrm((DEPTH, N_GROUPS), 0.01),
        'router_e_w': nrm((DEPTH, D, N_EXPERTS), D ** -0.5),
        'router_e_b': nrm((DEPTH, N_EXPERTS), 0.01),
        'e_gate': nrm((DEPTH, N_EXPERTS, D, D_EXPERT), D ** -0.5),
        'e_up': nrm((DEPTH, N_EXPERTS, D, D_EXPERT), D ** -0.5),
        'e_down': nrm((DEPTH, N_EXPERTS, D_EXPERT, D), D_EXPERT ** -0.5),
        'g_ple': gain((DEPTH, D)),
        'w_ple_gate': nrm((DEPTH, D, D), D ** -0.5),
        'w_ple_proj': nrm((DEPTH, D_PLE, D), D_PLE ** -0.5),
        'g_final': gain((D,)),
    }


def reference(x_prompt, x_sample, state_ret, state_wkv, state_shift, p_prompt, p_sample,
              g_mix, w_in, w_oa, wkv_mu, wkv_w0, wkv_w2, wkv_a0, wkv_a2, wkv_g2, wkv_k_k, wkv_k_a,
              wkv_r_k, wkv_ln_w, wkv_ln_b, w_ob, w_out, g_ffn, router_g_w, router_g_b, router_e_w,
              router_e_b, e_gate, e_up, e_down, g_ple, w_ple_gate, w_ple_proj, g_final):
    B, S = x_prompt.shape[0], x_prompt.shape[1]
    Bd, Sd = x_sample.shape[0], x_sample.shape[1]
    pos_prompt = jnp.arange(S, dtype=jnp.int32)
    pos_sample = PAST_LEN + jnp.arange(Sd, dtype=jnp.int32)
    xp, xs = x_prompt, x_sample
    ret_p, wkv_p, sh_p, ret_s, wkv_s, sh_s = [], [], [], [], [], []
    for i in range(DEPTH):
        lw = (g_mix[i], w_in[i], w_oa[i], wkv_mu[i], wkv_w0[i], wkv_w2[i], wkv_a0[i], wkv_a2[i], wkv_g2[i],
              wkv_k_k[i], wkv_k_a[i], wkv_r_k[i], wkv_ln_w[i], wkv_ln_b[i], w_ob[i], w_out[i], g_ffn[i],
              router_g_w[i], router_g_b[i], router_e_w[i], router_e_b[i], e_gate[i], e_up[i], e_down[i],
              g_ple[i], w_ple_gate[i], w_ple_proj[i])
        xp, r1, w1, s1 = trunk_layer(
            xp, p_prompt[i], pos_prompt,
            jnp.zeros((B, RET_HEADS, RET_DK, RET_DV), jnp.float32),
            jnp.zeros((B, WKV_HEADS, WKV_N, WKV_N), jnp.float32),
            jnp.zeros((B, RWKV_PROJ), xp.dtype), *lw)
        xs, r2, w2_, s2 = trunk_layer(xs, p_sample[i], pos_sample, state_ret[i], state_wkv[i], state_shift[i], *lw)
        ret_p.append(r1); wkv_p.append(w1); sh_p.append(s1)
        ret_s.append(r2); wkv_s.append(w2_); sh_s.append(s2)
    y_prompt = rmsnorm(xp, g_final)
    y_sample = rmsnorm(xs, g_final)
    return (y_prompt, y_sample, jnp.stack(ret_p), jnp.stack(wkv_p), jnp.stack(sh_p),
            jnp.stack(ret_s), jnp.stack(wkv_s), jnp.stack(sh_s))
```

```python
import numpy as np
from contextlib import ExitStack
import ml_dtypes
import concourse.bass as bass
import concourse.mybir as mybir
from concourse.bass_utils import run_bass_kernel_spmd

F32 = mybir.dt.float32
BF16 = mybir.dt.bfloat16
I32 = mybir.dt.int32
AF = mybir.ActivationFunctionType
ALU = mybir.AluOpType
AX = mybir.AxisListType

D = 2048
NT = 17
T = NT * 128
KC = 16
RH, RDK, RDV = 8, 128, 256
WH, WN = 16, 64
RWKV_PROJ = 3360
IN_COLS = 13600
OFF_Q, OFF_K, OFF_V, OFF_GR, OFF_ZB, OFF_G = 0, 1024, 2048, 4096, 6144, 9504
NE, EPG, DE = 32, 8, 1024
DPLE = 256
PAST = 16384
EPS = 1e-6
CAP = 256
NSLOT = NE * CAP


class Sched:
    def __init__(self, nc):
        self.nc = nc
        self.eng = {'pe': nc.tensor, 'dve': nc.vector, 'act': nc.scalar, 'pool': nc.gpsimd, 'sp': nc.sync}
        self.sem = {}
        self.cnt = {}
        self.nsem = 0
        for e in self.eng:
            self._newsem(e)
        self.waited = {e: {} for e in self.eng}
        self.lastw = {}
        self.readers = {}
        self.RING = 8
        self.ring = {}
        for q in ('sp', 'pool', 'act'):
            self.ring[q] = [[self._mksem(), 0] for _ in range(self.RING)]
        self.ring_i = {q: 0 for q in self.ring}
        self.nins = 0

    def _mksem(self):
        self.nsem += 1
        return (self.nc.semaphore(f"sm{self.nsem}").__enter__(), self.nsem)

    def _newsem(self, e):
        self.sem[e] = self._mksem()
        self.cnt[e] = 0

    def _wait(self, e, tickets):
        w = self.waited[e]
        for (sem, sid, val) in tickets:
            if w.get(sid, 0) >= val:
                continue
            self.eng[e].wait_ge(sem, val)
            w[sid] = val

    @staticmethod
    def _is_psum(k):
        n = k[0] if isinstance(k, tuple) else k
        return isinstance(n, str) and (n in ('psT', 'bkA', 'bkO', 'psZ', 'psP', 'psB') or (len(n) == 2 and n[0] == 'b' and n[1].isdigit()))

    def _deps(self, R, W, me=None):
        t = []
        for k in R:
            if k in self.lastw:
                t.append(self.lastw[k])
            if self._is_psum(k):
                r = self.readers.get(k)
                if r:
                    t.extend(v for s_, v in r.items() if s_ != me)
        for k in W:
            if k in self.lastw:
                t.append(self.lastw[k])
            r = self.readers.get(k)
            if r:
                t.extend(r.values())
        return t

    def _record(self, tk, slot, R, W):
        for k in R:
            self.readers.setdefault(k, {})[slot] = tk
        for k in W:
            self.lastw[k] = tk
            self.readers[k] = {}

    def op(self, e, fn, R=(), W=()):
        deps = self._deps(R, W, e)
        if e == 'pe':
            pesid = self.sem['pe'][1]
            deps = [d for d in deps if d[1] != pesid]
        self._wait(e, deps)
        ins = fn(self.eng[e])
        if self.cnt[e] >= 30000:
            self._newsem(e)
        self.cnt[e] += 1
        sem, sid = self.sem[e]
        ins.then_inc(sem, 1)
        self.nins += 1
        self._record((sem, sid, self.cnt[e]), e, R, W)

    def dma(self, q, out, in_, R=(), W=(), fn=None):
        deps = self._deps(R, W)
        i = self.ring_i[q]
        self.ring_i[q] = (i + 1) % self.RING
        slot = self.ring[q][i]
        (sem, sid), val = slot
        if val > 0:
            deps.append((sem, sid, val))
        self._wait(q, deps)
        if fn is None:
            ins = self.eng[q].dma_start(out=out, in_=in_)
        else:
            ins = fn(self.eng[q])
        slot[1] = val + 16
        ins.then_inc(sem, 16)
        self.nins += 1
        self._record((sem, sid, val + 16), (q, i), R, W)

    def barrier(self):
        tk = []
        for q in self.ring:
            for (sem, sid), val in self.ring[q]:
                if val > 0:
                    tk.append((sem, sid, val))
        for e in ('pe', 'dve', 'act', 'pool'):
            sem, sid = self.sem[e]
            if self.cnt[e] > 0:
                tk.append((sem, sid, self.cnt[e]))
        for e in ('pe', 'dve', 'act', 'pool', 'sp'):
            self._wait(e, tk)

    def finish(self):
        tk = []
        for q in self.ring:
            for (sem, sid), val in self.ring[q]:
                if val > 0:
                    tk.append((sem, sid, val))
        for e in ('pe', 'dve', 'act', 'pool'):
            sem, sid = self.sem[e]
            if self.cnt[e] > 0:
                tk.append((sem, sid, self.cnt[e]))
        self._wait('sp', tk)


def _ret_consts():
    h = np.arange(RH, dtype=np.float64)
    log_g = np.log1p(-np.exp2(-5.0 - h))
    C = 128
    idx = np.arange(C, dtype=np.float64)
    diff = idx[:, None] - idx[None, :]
    scale = RDK ** -0.5
    mask = np.where(diff >= 0, np.exp(log_g[:, None, None] * np.maximum(diff, 0.0)), 0.0)
    maskT = mask.transpose(0, 2, 1) * scale
    qdec = np.exp(log_g[:, None] * (idx + 1.0))
    kdec = np.exp(log_g[:, None] * (C - 1.0 - idx)) * scale
    cdec = np.exp(log_g * C)
    r = np.arange(128)
    s_id = r // 4
    t_id = (r % 4).astype(np.float64)
    valid = r < 64
    same = (s_id[:, None] == s_id[None, :]) & valid[:, None] & valid[None, :]
    dd = t_id[:, None] - t_id[None, :]
    mask_s = np.where(same & (dd >= 0), np.exp(log_g[:, None, None] * np.maximum(dd, 0.0)), 0.0)
    maskT_s = mask_s.transpose(0, 2, 1) * scale
    qdec_s = np.exp(log_g[:, None] * (t_id + 1.0)) * valid
    kdec_s = np.exp(log_g[:, None] * (3.0 - t_id)) * scale * valid
    cdec_s = np.exp(log_g * 4.0)
    out = {
        'c_maskT': np.ascontiguousarray(maskT.transpose(1, 0, 2)).astype(np.float32),
        'c_maskT_s': np.ascontiguousarray(maskT_s.transpose(1, 0, 2)).astype(np.float32),
        'c_qdec': np.broadcast_to(qdec[None], (128, RH, 128)).astype(np.float32).copy(),
        'c_qdec_s': np.broadcast_to(qdec_s[None], (128, RH, 128)).astype(np.float32).copy(),
        'c_kdec': np.ascontiguousarray(kdec.T).astype(np.float32),
        'c_kdec_s': np.ascontiguousarray(kdec_s.T).astype(np.float32),
    }
    smi = np.zeros((128, 16, 128), np.float32)
    for s in range(16):
        smi[:, s, 4 * s:4 * s + 4] = 1.0
    smj = np.zeros((128, 16), np.float32)
    for s in range(16):
        smj[4 * s:4 * s + 4, s] = 1.0
    out['c_smi'] = smi
    out['c_smj'] = smj
    return out, [float(x) for x in cdec], [float(x) for x in cdec_s]


def _wkv_consts():
    r = np.arange(128)
    up = (r[:, None] <= r[None, :])
    sup = (r[:, None] < r[None, :])
    slo = (r[:, None] > r[None, :])
    valid = r < 64
    same = (r[:, None] // 4 == r[None, :] // 4) & valid[:, None] & valid[None, :]
    tri = np.stack([up, up & same], 1).astype(np.float32)
    ones = np.stack([np.ones((128, 128), bool), same], 1).astype(np.float32)
    m5 = np.zeros((128, 2, 5, 128), np.float32)
    for v, extra in ((0, np.ones((128, 128), bool)), (1, same)):
        m5[:, v, 0] = sup & extra
        m5[:, v, 1] = slo & extra
        m5[:, v, 2] = sup & extra
        m5[:, v, 3] = up & extra
        m5[:, v, 4] = up & extra
    tm = ((r % 4 != 0) & valid).astype(np.float32)[:, None]
    return {'c_tri': tri, 'c_ones': ones, 'c_m5': m5, 'c_tm': tm}


def _route_consts():
    r = np.arange(128)
    slt = (r[:, None] < r[None, :]).astype(np.float32)
    ecap = np.broadcast_to((np.arange(32, dtype=np.float32) * CAP)[None, :], (128, 32)).copy()
    valid = np.stack([np.ones(128, np.float32), (r < 64).astype(np.float32)], 1)
    trash = (NSLOT + r).astype(np.float32)[:, None]
    return {'c_slt': slt, 'c_ecap': ecap, 'c_valid': valid, 'c_trash': trash}


def _rope_table():
    half = RDK // 2
    inv = (10000.0 ** (-np.arange(half, dtype=np.float32) / half)).astype(np.float32)
    pos = np.zeros((T,), np.float32)
    pos[:2048] = np.arange(2048, dtype=np.float32)
    pos[2048:2112] = np.tile(PAST + np.arange(4, dtype=np.float32), 16)
    ang = pos[:, None] * inv[None, :]
    tab = np.stack([np.cos(ang), np.sin(ang)], 1).astype(np.float32)
    return tab


def build(debug_stage=99):
    import os
    ST = os.environ.get('KSTAGES', 'B,C1,C2,D,E0,E,F').split(',')
    nc = bass.Bass("TRN2", target_bir_lowering=False)
    S = Sched(nc)
    CONSTS, CDEC, CDEC_S = _ret_consts()

    def din(name, shape, dt=F32):
        return nc.dram_tensor(name, list(shape), dt, kind="ExternalInput").ap()

    def dout(name, shape, dt=F32):
        return nc.dram_tensor(name, list(shape), dt, kind="ExternalOutput").ap()

    def dscr(name, shape, dt=F32):
        return nc.dram_tensor(name, list(shape), dt, kind="Internal").ap()

    stack = [None]

    def sb(name, shape, dt=F32):
        cm = nc.sbuf_tensor(name, list(shape), dt)
        if stack[0] is not None:
            return stack[0].enter_context(cm)
        return cm.__enter__()

    x = din("x", [T, D])
    st_ret = din("st_ret", [16, RH, RDK, RDV])
    g_mix = din("g_mix", [1, D])
    w_in = din("w_in", [D, IN_COLS])
    rope = din("rope", [T, 2, 64])
    ident_d = din("ident", [128, 128])
    cst = {k: din(k, v.shape) for k, v in CONSTS.items()}

    prm = din("prm", [1, RWKV_PROJ + 7168])
    st_shift = din("st_shift", [16, RWKV_PROJ])
    st_wkv = din("st_wkv", [16, WH, WN, WN])
    wkv_w2 = din("wkv_w2", [64, 1024])
    wkv_a2 = din("wkv_a2", [64, 1024])
    wkv_g2 = din("wkv_g2", [160, 1024])
    WCONSTS = _wkv_consts()
    wc = {k: din(k, v.shape) for k, v in WCONSTS.items()}
    w_oa = din("w_oa", [D, D])
    w_ob = din("w_ob", [1024, D])
    w_out = din("w_out", [D, D])
    g_ffn = din("g_ffn", [1, D])
    wr = din("wr", [D, 36])
    rbias = din("rbias", [1, 36])
    NE_D = NE if 'E' in ST else 1
    e_gate = din("e_gate", [NE_D, D, DE])
    e_up = din("e_up", [NE_D, D, DE])
    e_down = din("e_down", [NE_D, DE, D])
    g_ple = din("g_ple", [1, D])
    w_ple_gate = din("w_ple_gate", [D, D])
    w_ple_proj = din("w_ple_proj", [DPLE, D])
    g_final = din("g_final", [1, D])
    p_in = din("p_in", [T, DPLE])
    RCONSTS = _route_consts()
    rcd = {k: din(k, v.shape) for k, v in RCONSTS.items()}
    x1D = dscr("x1D", [T, D], F32)
    XeD = dscr("XeD", [NSLOT + 128, D], BF16)
    YeD = dscr("YeD", [NSLOT + 128, D], F32)
    sh_p = dout("sh_p", [1, RWKV_PROJ])
    sh_s = dout("sh_s", [16, RWKV_PROJ])
    wkv_p = dout("wkv_p", [WH, WN, WN])
    wkv_s = dout("wkv_s", [16, WH, WN, WN])
    uD = dscr("uD", [T, RWKV_PROJ], F32)
    obD = dscr("obD", [T, 1024], BF16)
    y_o = dout("y", [T, D])
    ret_p = dout("ret_p", [RH, RDK, RDV])
    ret_s = dout("ret_s", [16, RH, RDK, RDV])

    oD = dscr("oD", [T, D], BF16)

    w_in_v = w_in.rearrange("(k p) n -> p k n", p=128)

    identb = sb("identb", [128, 128], BF16)
    identf = sb("identf", [128, 128], F32)
    gbc = sb("gbc", [128, D], F32)
    hT_stack = ExitStack()
    hT = hT_stack.enter_context(nc.sbuf_tensor("hT", [128, KC, T], BF16))
    psF = nc.psum_tensor("psF", [128, 7, 512], F32).__enter__()
    psT = nc.psum_tensor("psT", [128, 1024], BF16).__enter__()

    S.dma('sp', identf[:], ident_d[:, :], W=['identf'])
    S.op('dve', lambda e: e.tensor_copy(out=identb[:], in_=identf[:]), R=['identf'], W=['identb'])
    S.dma('sp', gbc[:], g_mix[0:1, :].partition_broadcast(128), W=['gbc'])

    stack[0] = ExitStack()
    xt = [sb(f"xt{i}", [128, D], F32) for i in range(2)]
    hb = [sb(f"hb{i}", [128, D], BF16) for i in range(2)]
    junk = sb("junk", [128, D], BF16)
    ss = sb("ss", [128, 4], F32)

    def rmsnorm_tile(src, dst_bf, gtile, key_src, key_dst, u, gkey='gbc'):
        S.op('act', lambda e: e.activation(out=junk[:], in_=src, func=AF.Square, accum_out=ss[:, u:u + 1]),
             R=[key_src], W=['junk', ('ss', u)])
        S.op('dve', lambda e: e.tensor_scalar(out=ss[:, u:u + 1], in0=ss[:, u:u + 1], scalar1=1.0 / D, scalar2=EPS,
                                              op0=ALU.mult, op1=ALU.add), R=[('ss', u)], W=[('ss', u)])
        S.op('act', lambda e: e.sqrt(out=ss[:, u:u + 1], in_=ss[:, u:u + 1]), R=[('ss', u)], W=[('ss', u)])
        S.op('dve', lambda e: e.reciprocal(out=ss[:, u:u + 1], in_=ss[:, u:u + 1]), R=[('ss', u)], W=[('ss', u)])
        S.op('dve', lambda e: e.scalar_tensor_tensor(out=dst_bf, in0=src, scalar=ss[:, u:u + 1], in1=gtile,
                                                     op0=ALU.mult, op1=ALU.mult),
             R=[key_src, ('ss', u), gkey], W=[key_dst])

    def transpose_to(dstT, src_bf, key_src, key_dst, i, nk=KC):
        for half in range(0, nk, 8):
            n = min(8, nk - half)

            def f(e, half=half, n=n):
                ins = None
                for k in range(n):
                    ins = e.transpose(psT[:, k * 128:(k + 1) * 128], src_bf[:, (half + k) * 128:(half + k + 1) * 128], identb[:])
                return ins
            S.op('pe', f, R=[key_src, 'identb'], W=['psT'])
            S.op('act', lambda e, half=half, n=n: e.activation(
                out=dstT[:, half:half + n, i * 128:(i + 1) * 128],
                in_=psT[:, 0:n * 128].rearrange("p (k t) -> p k t", k=n), func=AF.Copy),
                R=['psT'], W=[(key_dst, i)])

    for i in range(NT):
        b = i % 2
        S.dma('sp', xt[b][:], x[i * 128:(i + 1) * 128, :], W=[('xt', b)])
        rmsnorm_tile(xt[b][:], hb[b][:], gbc[:], ('xt', b), ('hb', b), b)
        transpose_to(hT, hb[b], ('hb', b), 'hT', i)

    S.barrier()
    stack[0].close()
    if True:
        stack[0] = ExitStack()
        junk = sb("junkB", [128, 256], BF16)
        cs = {}
        for k, v in CONSTS.items():
            if k == 'c_smi':
                continue
            cs[k] = sb("s_" + k, list(v.shape), F32)
            S.dma('sp', cs[k][:], cst[k], W=[k])
        smi_b = sb("smi_b", [128, 16, 128], BF16)
        S.dma('pool', smi_b[:], cst['c_smi'], W=['smi_b'])
        ropet = sb("ropet", [128, NT, 2, 64], F32)
        S.dma('sp', ropet[:], rope.rearrange("(n p) a f -> p n a f", p=128), W=['rope'])

        Wqk = [sb(f"Wqk{i}", [128, KC, 256], BF16) for i in range(2)]
        Wvg = [sb(f"Wvg{i}", [128, KC, 512], BF16) for i in range(2)]
        qk_r = [sb(f"qk_r{i}", [128, 2, 128], BF16) for i in range(2)]
        rt = [sb(f"rt{i}", [128, 2, 2, 64], F32) for i in range(4)]
        v_bf = [sb(f"v_bf{i}", [128, 256], BF16) for i in range(2)]
        sg = [sb(f"sg{i}", [128, 256], F32) for i in range(2)]
        qkT = [sb(f"qkT{i}", [128, 2, 128], BF16) for i in range(2)]
        qTd = [sb(f"qTd{i}", [128, 128], BF16) for i in range(2)]
        kd = [sb(f"kd{i}", [128, 128], BF16) for i in range(2)]
        sT = [sb(f"sT{i}", [128, 128], BF16) for i in range(2)]
        Sf = sb("Sf", [128, 256], F32)
        Sb = [sb(f"Sb{i}", [128, 256], BF16) for i in range(2)]
        on = [sb(f"on{i}", [128, 256], BF16) for i in range(2)]
        oss = sb("oss", [128, 2], F32)
        S0f = sb("S0f", [128, 8, 256], F32)
        S0b = sb("S0b", [128, 16, 256], BF16)
        Snew = sb("Snew", [128, 8, 256], F32)
        qTd_m = sb("qTd_m", [128, 16, 128], BF16)
        kd_m = sb("kd_m", [128, 16, 128], BF16)

        for h in range(RH if 'B' in ST else 0):
            wb = h % 2
            S.dma('pool', Wqk[wb][:, :, 0:128], w_in_v[:, :, OFF_Q + h * 128:OFF_Q + (h + 1) * 128], W=[('Wqk', wb)])
            S.dma('pool', Wqk[wb][:, :, 128:256], w_in_v[:, :, OFF_K + h * 128:OFF_K + (h + 1) * 128], W=[('Wqk', wb)])
            S.dma('pool', Wvg[wb][:, :, 0:256], w_in_v[:, :, OFF_V + h * 256:OFF_V + (h + 1) * 256], W=[('Wvg', wb)])
            S.dma('pool', Wvg[wb][:, :, 256:512], w_in_v[:, :, OFF_GR + h * 256:OFF_GR + (h + 1) * 256], W=[('Wvg', wb)])
            S.dma('pool', S0b[:], st_ret[:, h, :, :].rearrange("s d e -> d s e"), W=['S0b'])
            S.op('dve', lambda e: e.memset(Sf[:], 0.0), W=['Sf'])
            S.op('dve', lambda e: e.memset(Sb[0][:], 0.0), W=[('Sb', 0)])

            for i in range(NT):
                p = i % 2
                smp = (i == NT - 1)
                bA, bB, bO = 3 * p, 3 * p + 1, 3 * p + 2
                psA = psF[:, bA, 0:256]
                psSC = psF[:, bA, 256:384]
                psB = psF[:, bB, :]
                psO = psF[:, bO, 0:256]
                psDS = psF[:, bO, 256:512]
                tsl = slice(i * 128, (i + 1) * 128)

                def fA(e):
                    ins = None
                    for k in range(KC):
                        ins = e.matmul(psA, lhsT=hT[:, k, tsl], rhs=Wqk[wb][:, k, :], start=(k == 0), stop=(k == KC - 1))
                    return ins
                S.op('pe', fA, R=[('hT', i), ('Wqk', wb)], W=[('bkA', p)])

                def fB(e):
                    ins = None
                    for k in range(KC):
                        ins = e.matmul(psB, lhsT=hT[:, k, tsl], rhs=Wvg[wb][:, k, :], start=(k == 0), stop=(k == KC - 1))
                    return ins
                S.op('pe', fB, R=[('hT', i), ('Wvg', wb)], W=[('psB', p)])

                A4 = psA.rearrange("p (a b f) -> p a b f", a=2, b=2)
                cosb = ropet[:, i, 0:1, :].to_broadcast([128, 2, 64])
                sinb = ropet[:, i, 1:2, :].to_broadcast([128, 2, 64])
                r0, r1, r2, r3 = rt[0], rt[1], rt[2], rt[3]
                S.op('dve', lambda e: e.tensor_tensor(out=r0[:, :, 0, :], in0=A4[:, :, 0, :], in1=cosb, op=ALU.mult),
                     R=[('bkA', p), 'rope'], W=['r0a'])
                S.op('dve', lambda e: e.tensor_tensor(out=r0[:, :, 1, :], in0=A4[:, :, 1, :], in1=sinb, op=ALU.mult),
                     R=[('bkA', p), 'rope'], W=['r0b'])
                S.op('dve', lambda e: e.tensor_tensor(out=r1[:, :, 0, :], in0=A4[:, :, 0, :], in1=sinb, op=ALU.mult),
                     R=[('bkA', p), 'rope'], W=['r1a'])
                S.op('dve', lambda e: e.tensor_tensor(out=r1[:, :, 1, :], in0=A4[:, :, 1, :], in1=cosb, op=ALU.mult),
                     R=[('bkA', p), 'rope'], W=['r1b'])
                qkr4 = qk_r[p][:].rearrange("p a (b f) -> p a b f", b=2)
                S.op('dve', lambda e: e.tensor_tensor(out=qkr4[:, :, 0, :], in0=r0[:, :, 0, :], in1=r0[:, :, 1, :], op=ALU.subtract),
                     R=['r0a', 'r0b'], W=[('qk_r', p, 0)])
                S.op('dve', lambda e: e.tensor_tensor(out=qkr4[:, :, 1, :], in0=r1[:, :, 0, :], in1=r1[:, :, 1, :], op=ALU.add),
                     R=['r1a', 'r1b'], W=[('qk_r', p, 1)])
                S.op('act', lambda e: e.activation(out=v_bf[p][:], in_=psB[:, 0:256], func=AF.Copy), R=[('psB', p)], W=[('v_bf', p)])
                S.op('act', lambda e: e.activation(out=sg[p][:], in_=psB[:, 256:512], func=AF.Silu), R=[('psB', p)], W=[('sg', p)])
                tT = psT[:, 512 + p * 256:512 + (p + 1) * 256]

                def fT(e):
                    e.transpose(tT[:, 0:128], qk_r[p][:, 0, :], identb[:])
                    return e.transpose(tT[:, 128:256], qk_r[p][:, 1, :], identb[:])
                S.op('pe', fT, R=[('qk_r', p, 0), ('qk_r', p, 1), 'identb'], W=['psT'])
                S.op('act', lambda e: e.activation(out=qkT[p][:].rearrange("p a t -> p (a t)"), in_=tT, func=AF.Copy),
                     R=['psT'], W=[('qkT', p)])
                qd = cs['c_qdec_s'] if smp else cs['c_qdec']
                kdc = cs['c_kdec_s'] if smp else cs['c_kdec']
                mk = cs['c_maskT_s'] if smp else cs['c_maskT']
                S.op('dve', lambda e: e.tensor_tensor(out=qTd[p][:], in0=qkT[p][:, 0, :], in1=qd[:, h, :], op=ALU.mult),
                     R=[('qkT', p), 'c_qdec', 'c_qdec_s'], W=[('qTd', p)])
                S.op('dve', lambda e: e.tensor_scalar(out=kd[p][:], in0=qk_r[p][:, 1, :], scalar1=kdc[:, h:h + 1], scalar2=None,
                                                      op0=ALU.mult), R=[('qk_r', p, 0), ('qk_r', p, 1), 'c_kdec', 'c_kdec_s'], W=[('kd', p)])
                S.op('pe', lambda e: e.matmul(psSC, lhsT=qkT[p][:, 1, :], rhs=qkT[p][:, 0, :], start=True, stop=True),
                     R=[('qkT', p)], W=[('bkA', p)])
                S.op('dve', lambda e: e.tensor_tensor(out=sT[p][:], in0=psSC, in1=mk[:, h, :], op=ALU.mult),
                     R=[('bkA', p), 'c_maskT', 'c_maskT_s'], W=[('sT', p)])
                if not smp:
                    sbp = i % 2

                    def fO(e):
                        e.matmul(psO, lhsT=sT[p][:], rhs=v_bf[p][:], start=True, stop=False)
                        return e.matmul(psO, lhsT=qTd[p][:], rhs=Sb[sbp][:], start=False, stop=True)
                    S.op('pe', fO, R=[('sT', p), ('v_bf', p), ('qTd', p), ('Sb', sbp)], W=[('bkO', p)])
                    S.op('pe', lambda e: e.matmul(psDS, lhsT=kd[p][:], rhs=v_bf[p][:], start=True, stop=True),
                         R=[('kd', p), ('v_bf', p)], W=[('bkO', p)])
                    S.op('dve', lambda e: e.scalar_tensor_tensor(out=Sf[:], in0=Sf[:], scalar=CDEC[h], in1=psDS,
                                                                 op0=ALU.mult, op1=ALU.add), R=['Sf', ('bkO', p)], W=['Sf'])
                    S.op('act', lambda e: e.activation(out=Sb[1 - sbp][:], in_=Sf[:], func=AF.Copy), R=['Sf'], W=[('Sb', 1 - sbp)])
                    if i == NT - 2:
                        S.dma('sp', ret_p[h, :, :], Sf[:], R=['Sf'], W=['ret_p'])
                else:
                    S.op('dve', lambda e: e.tensor_tensor(out=qTd_m[:], in0=qTd[p][:].unsqueeze(1).to_broadcast([128, 16, 128]),
                                                          in1=smi_b[:], op=ALU.mult), R=[('qTd', p), 'smi_b'], W=['qTd_m'])
                    S.op('dve', lambda e: e.tensor_tensor(out=kd_m[:], in0=kd[p][:].unsqueeze(1).to_broadcast([128, 16, 128]),
                                                          in1=cs['c_smj'][:].unsqueeze(2).to_broadcast([128, 16, 128]), op=ALU.mult),
                         R=[('kd', p), 'c_smj'], W=['kd_m'])

                    def fO(e):
                        e.matmul(psO, lhsT=sT[p][:], rhs=v_bf[p][:], start=True, stop=False)
                        ins = None
                        for s in range(16):
                            ins = e.matmul(psO, lhsT=qTd_m[:, s, :], rhs=S0b[:, s, :], start=False, stop=(s == 15))
                        return ins
                    S.op('pe', fO, R=[('sT', p), ('v_bf', p), 'qTd_m', 'S0b'], W=[('bkO', p)])
                    for s2 in range(8):
                        bank6 = psF[:, 6, :]
                        if s2 % 4 == 0:
                            hf = s2 // 4
                            S.dma('sp', S0f[:], st_ret[8 * hf:8 * hf + 8, h, :, :].rearrange("s d e -> d s e"), W=['S0f'])

                        def fD(e, s2=s2):
                            e.matmul(bank6[:, 0:256], lhsT=kd_m[:, 2 * s2, :], rhs=v_bf[p][:], start=True, stop=True)
                            return e.matmul(bank6[:, 256:512], lhsT=kd_m[:, 2 * s2 + 1, :], rhs=v_bf[p][:], start=True, stop=True)
                        S.op('pe', fD, R=['kd_m', ('v_bf', p)], W=['bank6'])
                        S.op('dve', lambda e, s2=s2: e.scalar_tensor_tensor(
                            out=Snew[:, 2 * (s2 % 4):2 * (s2 % 4) + 2, :], in0=S0f[:, 2 * (s2 % 4):2 * (s2 % 4) + 2, :], scalar=CDEC_S[h],
                            in1=bank6.rearrange("p (a b) -> p a b", a=2), op0=ALU.mult, op1=ALU.add),
                            R=['S0f', 'bank6'], W=['Snew'])
                        if s2 % 4 == 3:
                            hf = s2 // 4
                            S.dma('sp', ret_s[8 * hf:8 * hf + 8, h, :, :].rearrange("s d e -> d s e"), Snew[:], R=['Snew'], W=['ret_s'])
                u = p
                S.op('act', lambda e: e.activation(out=junk[:, 0:256], in_=psO, func=AF.Square, accum_out=oss[:, u:u + 1]),
                     R=[('bkO', p)], W=['junk', ('oss', u)])
                S.op('dve', lambda e: e.tensor_scalar(out=oss[:, u:u + 1], in0=oss[:, u:u + 1], scalar1=1.0 / RDV, scalar2=EPS,
                                                      op0=ALU.mult, op1=ALU.add), R=[('oss', u)], W=[('oss', u)])
                S.op('act', lambda e: e.sqrt(out=oss[:, u:u + 1], in_=oss[:, u:u + 1]), R=[('oss', u)], W=[('oss', u)])
                S.op('dve', lambda e: e.reciprocal(out=oss[:, u:u + 1], in_=oss[:, u:u + 1]), R=[('oss', u)], W=[('oss', u)])
                S.op('dve', lambda e: e.scalar_tensor_tensor(out=on[p][:], in0=psO, scalar=oss[:, u:u + 1], in1=sg[p][:],
                                                             op0=ALU.mult, op1=ALU.mult),
                     R=[('bkO', p), ('oss', u), ('sg', p)], W=[('on', p)])
                S.dma('sp', oD[tsl, h * 256:(h + 1) * 256], on[p][:], R=[('on', p)], W=[('oD', i)])

    S.barrier()
    stack[0].close()
    stack[0] = ExitStack()
    mu_bc = sb("mu_bc", [128, RWKV_PROJ], F32)
    S.dma('sp', mu_bc[:], prm[0:1, 0:RWKV_PROJ].partition_broadcast(128), W=['mu_bc'])
    shrows = sb("shrows", [128, RWKV_PROJ], F32)
    S.op('dve', lambda e: e.memset(shrows[:], 0.0), W=['shrows'])
    for s_ in range(16):
        S.dma('sp', shrows[4 * s_:4 * s_ + 1, :], st_shift[s_:s_ + 1, :], W=['shrows'])
    tmk = sb("tmk", [128, 1], F32)
    S.dma('sp', tmk[:], wc['c_tm'], W=['tmk'])
    Wz = [sb(f"Wz{i}", [128, KC, 512], BF16) for i in range(2)]
    hTs = [sb(f"hTs{i}", [128, KC, 128], BF16) for i in range(2)]
    zbs = [sb(f"zbs{i}", [128, 512], F32) for i in range(2)]
    dd = [sb(f"dd{i}", [128, 512], F32) for i in range(2)]
    uu = [sb(f"uu{i}", [128, 512], F32) for i in range(2)]
    cnt = 0
    for blk in range(7 if 'C1' in ST else 0):
        wd_ = min(512, RWKV_PROJ - blk * 512)
        c0 = OFF_ZB + blk * 512
        wb = blk % 2
        S.dma('pool', Wz[wb][:, :, 0:wd_], w_in_v[:, :, c0:c0 + wd_], W=[('Wz', wb)])
        for i in range(NT):
            p = cnt % 2
            cnt += 1
            tsl = slice(i * 128, (i + 1) * 128)
            ce = 'act' if p == 0 else 'pool'
            def cp(e, o, i_):
                if ce == 'act':
                    return e.activation(out=o, in_=i_, func=AF.Copy)
                return e.tensor_copy(out=o, in_=i_)
            if i == 0:
                S.op('pool', lambda e: e.memset(hTs[p][:, :, 0:1], 0.0), W=[('hTs', p)])
                S.op(ce, lambda e: cp(e, hTs[p][:, :, 1:128], hT[:, :, 0:127]), R=[('hT', 0)], W=[('hTs', p)])
            else:
                S.op(ce, lambda e: cp(e, hTs[p][:], hT[:, :, i * 128 - 1:i * 128 + 127]),
                     R=[('hT', i), ('hT', i - 1)], W=[('hTs', p)])
            psZ = psF[:, 2 * p, 0:wd_]
            psP = psF[:, 2 * p + 1, 0:wd_]

            def fZ(e):
                ins = None
                for k in range(KC):
                    ins = e.matmul(psZ, lhsT=hT[:, k, tsl], rhs=Wz[wb][:, k, 0:wd_], start=(k == 0), stop=(k == KC - 1))
                return ins
            S.op('pe', fZ, R=[('hT', i), ('Wz', wb)], W=[('psZ', p)])

            def fP(e):
                ins = None
                for k in range(KC):
                    ins = e.matmul(psP, lhsT=hTs[p][:, k, :], rhs=Wz[wb][:, k, 0:wd_], start=(k == 0), stop=(k == KC - 1))
                return ins
            S.op('pe', fP, R=[('hTs', p), ('Wz', wb)], W=[('psP', p)])
            S.op('act', lambda e: e.activation(out=zbs[p][:, 0:wd_], in_=psZ, func=AF.Copy), R=[('psZ', p)], W=[('zbs', p)])
            if i == NT - 1:
                S.op('dve', lambda e: e.scalar_tensor_tensor(out=dd[p][:, 0:wd_], in0=psP, scalar=tmk[:, 0:1],
                                                             in1=shrows[:, blk * 512:blk * 512 + wd_], op0=ALU.mult, op1=ALU.add),
                     R=[('psP', p), 'tmk', 'shrows'], W=[('dd', p)])
                S.op('dve', lambda e: e.tensor_tensor(out=dd[p][:, 0:wd_], in0=dd[p][:, 0:wd_], in1=zbs[p][:, 0:wd_], op=ALU.subtract),
                     R=[('dd', p), ('zbs', p)], W=[('dd', p)])
            else:
                S.op('dve', lambda e: e.tensor_tensor(out=dd[p][:, 0:wd_], in0=psP, in1=zbs[p][:, 0:wd_], op=ALU.subtract),
                     R=[('psP', p), ('zbs', p)], W=[('dd', p)])
            S.op('dve', lambda e: e.tensor_tensor(out=dd[p][:, 0:wd_], in0=dd[p][:, 0:wd_], in1=mu_bc[:, blk * 512:blk * 512 + wd_], op=ALU.mult),
                 R=[('dd', p), 'mu_bc'], W=[('dd', p)])
            S.op('dve', lambda e: e.tensor_tensor(out=uu[p][:, 0:wd_], in0=dd[p][:, 0:wd_], in1=zbs[p][:, 0:wd_], op=ALU.add),
                 R=[('dd', p), ('zbs', p)], W=[('uu', p)])
            S.dma('sp', uD[tsl, blk * 512:blk * 512 + wd_], uu[p][:, 0:wd_], R=[('uu', p)], W=[('uD', i)])
            if i == NT - 2:
                S.dma('sp', sh_p[0:1, blk * 512:blk * 512 + wd_], zbs[p][127:128, 0:wd_], R=[('zbs', p)], W=['sh_p'])
            if i == NT - 1:
                for s_ in range(16):
                    S.dma('sp', sh_s[s_:s_ + 1, blk * 512:blk * 512 + wd_], zbs[p][4 * s_ + 3:4 * s_ + 4, 0:wd_], R=[('zbs', p)], W=['sh_s'])

    S.barrier()
    stack[0].close()
    stack[0] = ExitStack()
    hT_stack.close()
    NP = 4096
    O_W0, O_A0, O_KK, O_KA, O_RK, O_LW, O_LB = [1024 * j for j in range(7)]
    pb = sb("pb", [128, NP], F32)
    S.dma('sp', pb[:], prm[0:1, RWKV_PROJ:RWKV_PROJ + NP].partition_broadcast(128), W=['pb'])
    pbx = sb("pbx", [128, 1024], F32)

    def ldrow(off):
        S.dma('sp', pbx[:], prm[0:1, RWKV_PROJ + off:RWKV_PROJ + off + 1024].partition_broadcast(128), W=['pbx'])
    loraW = sb("loraW", [128, 1024], BF16)
    g2b = sb("g2b", [128, 1024], BF16)
    g2c = sb("g2c", [32, 1024], BF16)
    S.dma('pool', loraW[0:64, :], wkv_w2[:, :], W=['loraW'])
    S.dma('pool', loraW[64:128, :], wkv_a2[:, :], W=['loraW'])
    S.dma('pool', g2b[:], wkv_g2[0:128, :], W=['g2b'])
    S.dma('pool', g2c[:], wkv_g2[128:160, :], W=['g2c'])
    wcs = {}
    for k in ('c_tri', 'c_ones', 'c_m5'):
        wcs[k] = sb("s_" + k, list(WCONSTS[k].shape), BF16 if k == 'c_m5' else F32)
        S.dma('pool' if k == 'c_m5' else 'sp', wcs[k][:], wc[k], W=[k])
    smj = sb("smj2", [128, 16], F32)
    S.dma('sp', smj[:], cst['c_smj'], W=['smj2'])
    smi2 = sb("smi2", [128, 16, 128], BF16)
    S.dma('pool', smi2[:], cst['c_smi'], W=['smi2'])
    ut = sb("ut", [128, RWKV_PROJ], F32)
    lt = sb("lt", [128, 288], BF16)
    ltT = sb("ltT", [128, 3, 128], BF16)
    FA = [sb(f"FA{j}", [128, 1024], F32) for j in range(8)]
    ggb = sb("ggb", [128, 1024], BF16)
    BQ = {n: sb("BQ_" + n, [128, 1024], BF16) for n in ('at', 'bt', 'kt', 'rt', 'bh', 'kh', 'v')}
    BQ['XT'], BQ['UT'], BQ['ob'] = BQ['at'], BQ['bt'], BQ['kt']
    TQ = {n: sb("TQ_" + n, [128, 8, 128], BF16) for n in ('at', 'bt', 'kt', 'rt')}
    MS = {n: sb("MS_" + n, [128, 16, 128], BF16) for n in ('A', 'AT', 'M', 'Mbr', 'Mkr', 'P', 'IpAT')}
    gCT = sb("gCT", [128, 8, 16], F32)
    STf = sb("STf", [128, 8, 64], F32)
    STb = sb("STb", [128, 8, 64], BF16)
    st16 = sb("st16", [128, 6, 16], F32)
    S0Tf = sb("S0Tf", [128, 16, 8, 64], F32)
    som = sb("som", [64, 8, 128], F32)
    S0in = som[:].rearrange("p a b -> p (a b)")
    qm = sb("qm", [128, 16, 128], F32)
    bm = [sb(f"bm{j}", [128, 16, 128], BF16) for j in range(2)]
    S.op('dve', lambda e: e.memset(STf[:], 0.0), W=['STf'])
    S.op('dve', lambda e: e.memset(STb[:], 0.0), W=['STb'])
    for s_ in range(16 if 'C2' in ST else 0):
        S.dma('sp', S0in.rearrange("p (h j) -> p h j", h=16), st_wkv[s_].rearrange("h i j -> i h j"), W=['som'])

        def fTs(e, s_=s_):
            ins = None
            for pr in range(8):
                ins = e.transpose(psF[:, 6, pr * 64:(pr + 1) * 64], S0in[0:64, pr * 128:(pr + 1) * 128], identf[0:64, 0:64])
            return ins
        S.op('pe', fTs, R=['som', 'identf'], W=['b6'])
        S.op('act', lambda e, s_=s_: e.activation(out=S0Tf[:, s_, :, :].rearrange("p a b -> p (a b)"), in_=psF[:, 6, :], func=AF.Copy),
             R=['b6'], W=['S0Tf'])

    def V(fn, R, W):
        S.op('dve', fn, R=R, W=W)

    def A(fn, R, W):
        S.op('act', fn, R=R, W=W)

    def bank2(b):
        return psF[:, b:b + 2, :].rearrange("p a c -> p (a c)")

    def h16(ap):
        return ap.rearrange("p (h j) -> p h j", h=16)

    def bc16(col_ap):
        return col_ap.unsqueeze(2).to_broadcast([128, 16, 64])

    NC2 = int(os.environ.get('KC2N', NT))
    for i in range(NC2 if 'C2' in ST else 0):
        smp = (i == NT - 1)
        vv = 1 if smp else 0
        tsl = slice(i * 128, (i + 1) * 128)
        S.dma('sp', ut[:], uD[tsl, :], R=[('uD', i)], W=['ut'])
        r_, kx, vx = ut[:, 0:1024], ut[:, 1024:2048], ut[:, 2048:3072]
        A(lambda e: e.activation(out=lt[:, 0:64], in_=ut[:, 3072:3136], func=AF.Tanh), ['ut'], ['lt0'])
        A(lambda e: e.activation(out=lt[:, 64:128], in_=ut[:, 3136:3200], func=AF.Copy), ['ut'], ['lt1'])
        A(lambda e: e.activation(out=lt[:, 128:288], in_=ut[:, 3200:3360], func=AF.Sigmoid), ['ut'], ['lt2'])

        def fLT(e):
            e.transpose(psT[:, 0:128], lt[:, 0:128], identb[:])
            e.transpose(psT[:, 128:256], lt[:, 128:256], identb[:])
            return e.transpose(psT[0:32, 256:384], lt[:, 256:288], identb[:])
        S.op('pe', fLT, R=['lt0', 'lt1', 'lt2', 'identb'], W=['psT'])
        A(lambda e: e.activation(out=ltT[:, 0:2, :].rearrange("p a t -> p (a t)"), in_=psT[:, 0:256], func=AF.Copy), ['psT'], ['ltTa'])
        A(lambda e: e.activation(out=ltT[0:32, 2, :], in_=psT[0:32, 256:384], func=AF.Copy), ['psT'], ['ltTb'])
        pLW, pLA, pLG = bank2(0), bank2(2), bank2(4)

        def fL(e):
            for hf in range(2):
                cs_ = slice(hf * 512, (hf + 1) * 512)
                e.matmul(pLW[:, cs_], lhsT=ltT[0:64, 0, :], rhs=loraW[0:64, cs_], start=True, stop=True)
                e.matmul(pLA[:, cs_], lhsT=ltT[64:128, 0, :], rhs=loraW[64:128, cs_], start=True, stop=True)
                e.matmul(pLG[:, cs_], lhsT=ltT[:, 1, :], rhs=g2b[:, cs_], start=True, stop=False)
                ins = e.matmul(pLG[:, cs_], lhsT=ltT[0:32, 2, :], rhs=g2c[0:32, cs_], start=False, stop=True)
            return ins
        S.op('pe', fL, R=['ltTa', 'ltTb', 'loraW', 'g2b', 'g2c'], W=['b0', 'b1', 'b2', 'b3', 'b4', 'b5'])
        logw, a_s, kkn, kmod, tmpA, tmpB, egi, etd = FA
        gg = ggb
        eg = kkn
        V(lambda e: e.tensor_tensor(out=tmpA[:], in0=pLW, in1=pb[:, O_W0:O_W0 + 1024], op=ALU.add), ['b0', 'b1', 'pb'], ['tmpA'])
        A(lambda e: e.activation(out=tmpA[:], in_=tmpA[:], func=AF.Sigmoid), ['tmpA'], ['tmpA'])
        V(lambda e: e.tensor_scalar(out=logw[:], in0=tmpA[:], scalar1=-0.6065306597126334, scalar2=None, op0=ALU.mult), ['tmpA'], ['logw'])
        V(lambda e: e.tensor_tensor(out=tmpB[:], in0=pLA, in1=pb[:, O_A0:O_A0 + 1024], op=ALU.add), ['b2', 'b3', 'pb'], ['tmpB'])
        A(lambda e: e.activation(out=a_s[:], in_=tmpB[:], func=AF.Sigmoid), ['tmpB'], ['a_s'])
        A(lambda e: e.activation(out=gg[:], in_=pLG, func=AF.Copy), ['b4', 'b5'], ['gg'])
        pC, pTt = bank2(0), bank2(2)

        def fC(e):
            for hf in range(2):
                cs_ = slice(hf * 512, (hf + 1) * 512)
                e.matmul(pC[:, cs_], lhsT=wcs['c_tri'][:, vv, :], rhs=logw[:, cs_], start=True, stop=True)
                ins = e.matmul(pTt[:, cs_], lhsT=wcs['c_ones'][:, vv, :], rhs=logw[:, cs_], start=True, stop=True)
            return ins
        S.op('pe', fC, R=['logw', 'c_tri', 'c_ones'], W=['b0', 'b1', 'b2', 'b3'])
        ncol = 16 if smp else 1

        def fG(e):
            ins = None
            for pr in range(8):
                ins = e.matmul(psF[:, 6, pr * 16:pr * 16 + ncol], lhsT=logw[:, pr * 128:(pr + 1) * 128],
                               rhs=(smj[:, 0:16] if smp else wcs['c_ones'][:, 0, 0:1]), start=True, stop=True)
            return ins
        S.op('pe', fG, R=['logw', 'smj2', 'c_ones'], W=['b6'])
        A(lambda e: e.activation(out=gCT[:, :, 0:ncol], in_=psF[:, 6, 0:128].rearrange("p (a b) -> p a b", a=8)[:, :, 0:ncol], func=AF.Exp),
          ['b6'], ['gCT'])
        A(lambda e: e.activation(out=eg[:], in_=pC, func=AF.Exp), ['b0', 'b1'], ['kkn'])
        V(lambda e: e.tensor_tensor(out=BQ['rt'][:], in0=r_, in1=eg[:], op=ALU.mult), ['ut', 'kkn'], ['q_rt'])
        V(lambda e: e.tensor_scalar(out=tmpA[:], in0=pC, scalar1=-1.0, scalar2=None, op0=ALU.mult), ['b0', 'b1'], ['tmpA'])
        A(lambda e: e.activation(out=egi[:], in_=tmpA[:], func=AF.Exp), ['tmpA'], ['egi'])
        V(lambda e: e.tensor_tensor(out=tmpB[:], in0=pTt, in1=tmpA[:], op=ALU.add), ['b2', 'b3', 'tmpA'], ['tmpB'])
        A(lambda e: e.activation(out=etd[:], in_=tmpB[:], func=AF.Exp), ['tmpB'], ['etd'])
        V(lambda e: e.tensor_tensor(out=tmpB[:], in0=tmpA[:], in1=logw[:], op=ALU.add), ['tmpA', 'logw'], ['tmpB'])
        A(lambda e: e.activation(out=tmpB[:], in_=tmpB[:], func=AF.Exp, scale=-1.0), ['tmpB'], ['tmpB'])
        V(lambda e: e.tensor_tensor(out=kkn[:], in0=kx, in1=pb[:, O_KK:O_KK + 1024], op=ALU.mult), ['ut', 'pb'], ['kkn'])
        A(lambda e: e.activation(out=tmpA[:], in_=kkn[:], func=AF.Square), ['kkn'], ['tmpA'])
        V(lambda e: e.tensor_reduce(out=st16[:, 0, :], in_=h16(tmpA[:]), axis=AX.X, op=ALU.add), ['tmpA'], ['st0'])
        A(lambda e: e.sqrt(out=st16[:, 0, :], in_=st16[:, 0, :]), ['st0'], ['st0'])
        V(lambda e: e.tensor_scalar(out=st16[:, 0, :], in0=st16[:, 0, :], scalar1=1e-12, scalar2=None, op0=ALU.max), ['st0'], ['st0'])
        V(lambda e: e.reciprocal(out=st16[:, 0, :], in_=st16[:, 0, :]), ['st0'], ['st0'])
        V(lambda e: e.tensor_tensor(out=h16(kkn[:]), in0=h16(kkn[:]), in1=bc16(st16[:, 0, :]), op=ALU.mult), ['kkn', 'st0'], ['kkn'])
        V(lambda e: e.scalar_tensor_tensor(out=kmod[:], in0=a_s[:], scalar=-1.0, in1=pb[:, O_KA:O_KA + 1024], op0=ALU.add, op1=ALU.mult),
          ['a_s', 'pb'], ['kmod'])
        V(lambda e: e.scalar_tensor_tensor(out=kmod[:], in0=kmod[:], scalar=1.0, in1=kx, op0=ALU.add, op1=ALU.mult), ['kmod', 'ut'], ['kmod'])
        V(lambda e: e.tensor_tensor(out=tmpA[:], in0=r_, in1=kmod[:], op=ALU.mult), ['ut', 'kmod'], ['tmpA'])
        ldrow(O_RK)
        V(lambda e: e.tensor_tensor(out=tmpA[:], in0=tmpA[:], in1=pbx[:], op=ALU.mult), ['tmpA', 'pbx'], ['tmpA'])
        V(lambda e: e.tensor_reduce(out=st16[:, 1, :], in_=h16(tmpA[:]), axis=AX.X, op=ALU.add), ['tmpA'], ['st1'])
        V(lambda e: e.scalar_tensor_tensor(out=BQ['at'][:], in0=kkn[:], scalar=-1.0, in1=tmpB[:], op0=ALU.mult, op1=ALU.mult),
          ['kkn', 'tmpB'], ['q_at'])
        V(lambda e: e.tensor_tensor(out=tmpA[:], in0=kkn[:], in1=a_s[:], op=ALU.mult), ['kkn', 'a_s'], ['tmpA'])
        V(lambda e: e.tensor_tensor(out=BQ['bt'][:], in0=tmpA[:], in1=egi[:], op=ALU.mult), ['tmpA', 'egi'], ['q_bt'])
        V(lambda e: e.tensor_tensor(out=BQ['bh'][:], in0=tmpA[:], in1=etd[:], op=ALU.mult), ['tmpA', 'etd'], ['q_bh'])
        V(lambda e: e.tensor_tensor(out=BQ['kt'][:], in0=kmod[:], in1=egi[:], op=ALU.mult), ['kmod', 'egi'], ['q_kt'])
        V(lambda e: e.tensor_tensor(out=BQ['kh'][:], in0=kmod[:], in1=etd[:], op=ALU.mult), ['kmod', 'etd'], ['q_kh'])
        A(lambda e: e.activation(out=BQ['v'][:], in_=vx, func=AF.Copy), ['ut'], ['q_v'])
        for n in ('at', 'bt', 'kt', 'rt'):
            def fTq(e, n=n):
                ins = None
                for pr in range(8):
                    ins = e.transpose(psT[:, pr * 128:(pr + 1) * 128], BQ[n][:, pr * 128:(pr + 1) * 128], identb[:])
                return ins
            S.op('pe', fTq, R=['q_' + n, 'identb'], W=['psT'])
            A(lambda e, n=n: e.activation(out=TQ[n][:].rearrange("p a t -> p (a t)"), in_=psT[:, :], func=AF.Copy), ['psT'], ['T_' + n])
        m5 = wcs['c_m5']
        for hd in range(16):
            pr, off = hd // 2, 64 * (hd % 2)
            sl = slice(off, off + 64)
            pb_ = 4 + (hd % 2)
            p3 = psF[:, pb_, 0:384]
            p2 = psF[:, pb_, 384:512]

            def f5(e):
                e.matmul(p3[:, 0:128], lhsT=TQ['bt'][sl, pr, :], rhs=TQ['at'][sl, pr, :], start=True, stop=True)
                e.matmul(p3[:, 128:256], lhsT=TQ['at'][sl, pr, :], rhs=TQ['bt'][sl, pr, :], start=True, stop=True)
                return e.matmul(p3[:, 256:384], lhsT=TQ['kt'][sl, pr, :], rhs=TQ['at'][sl, pr, :], start=True, stop=True)
            S.op('pe', f5, R=['T_at', 'T_bt', 'T_kt'], W=[f'b{pb_}'])
            V(lambda e: e.tensor_tensor(out=MS['A'][:, hd, :], in0=p3[:, 0:128], in1=m5[:, vv, 0, :], op=ALU.mult), [f'b{pb_}', 'c_m5'], [('A', hd)])
            V(lambda e: e.tensor_tensor(out=MS['AT'][:, hd, :], in0=p3[:, 128:256], in1=m5[:, vv, 1, :], op=ALU.mult), [f'b{pb_}', 'c_m5'], [('AT', hd)])
            V(lambda e: e.tensor_tensor(out=MS['M'][:, hd, :], in0=p3[:, 256:384], in1=m5[:, vv, 2, :], op=ALU.mult), [f'b{pb_}', 'c_m5'], [('M', hd)])
            pq = psF[:, 6, (hd % 2) * 256:(hd % 2) * 256 + 256]

            def f2(e):
                e.matmul(pq[:, 0:128], lhsT=TQ['bt'][sl, pr, :], rhs=TQ['rt'][sl, pr, :], start=True, stop=True)
                return e.matmul(pq[:, 128:256], lhsT=TQ['kt'][sl, pr, :], rhs=TQ['rt'][sl, pr, :], start=True, stop=True)
            S.op('pe', f2, R=['T_bt', 'T_kt', 'T_rt'], W=['b6'])
            V(lambda e: e.tensor_tensor(out=MS['Mbr'][:, hd, :], in0=pq[:, 0:128], in1=m5[:, vv, 3, :], op=ALU.mult), ['b6', 'c_m5'], [('Mbr', hd)])
            V(lambda e: e.tensor_tensor(out=MS['Mkr'][:, hd, :], in0=pq[:, 128:256], in1=m5[:, vv, 4, :], op=ALU.mult), ['b6', 'c_m5'], [('Mkr', hd)])
        identb4 = identb[:].unsqueeze(1).to_broadcast([128, 4, 128])
        for g in range(4):
            gs = slice(4 * g, 4 * g + 4)
            gk = [('A', hd) for hd in range(4 * g, 4 * g + 4)]
            gkT = [('AT', hd) for hd in range(4 * g, 4 * g + 4)]
            V(lambda e: e.tensor_tensor(out=MS['P'][:, gs, :], in0=MS['A'][:, gs, :], in1=identb4, op=ALU.add), gk + ['identb'], [('P', g)])
            nlev = 2 if smp else 7
            for lev in range(1, nlev):
                last = (lev == nlev - 1)
                pa = bank2(2 * (g % 2)).rearrange("p (h a t) -> p h a t", h=4, a=2)
                pbk = psF[:, 4 + (g % 2), :].rearrange("p (h t) -> p h t", h=4)

                def fa(e):
                    ins = None
                    for q in range(4):
                        hd = 4 * g + q
                        if not last:
                            e.matmul(pa[:, q, 0, :], lhsT=MS['AT'][:, hd, :], rhs=MS['A'][:, hd, :], start=True, stop=True)
                        ins = e.matmul(pa[:, q, 1, :], lhsT=MS['A'][:, hd, :], rhs=MS['AT'][:, hd, :], start=True, stop=True)
                    return ins
                S.op('pe', fa, R=gk + gkT, W=[f'b{2 * (g % 2)}', f'b{2 * (g % 2) + 1}'])
                V(lambda e: e.tensor_tensor(out=MS['IpAT'][:, gs, :], in0=pa[:, :, 1, :], in1=identb4, op=ALU.add),
                  [f'b{2 * (g % 2)}', f'b{2 * (g % 2) + 1}', 'identb'], [('IpAT', g)])
                if not last:
                    A(lambda e: e.activation(out=MS['A'][:, gs, :], in_=pa[:, :, 0, :], func=AF.Copy), [f'b{2 * (g % 2)}', f'b{2 * (g % 2) + 1}'], gk)
                    A(lambda e: e.activation(out=MS['AT'][:, gs, :], in_=pa[:, :, 1, :], func=AF.Copy), [f'b{2 * (g % 2)}', f'b{2 * (g % 2) + 1}'], gkT)

                def fb(e):
                    ins = None
                    for q in range(4):
                        hd = 4 * g + q
                        ins = e.matmul(pbk[:, q, :], lhsT=MS['IpAT'][:, hd, :], rhs=MS['P'][:, hd, :], start=True, stop=True)
                    return ins
                S.op('pe', fb, R=[('IpAT', g), ('P', g)], W=[f'b{4 + (g % 2)}'])
                A(lambda e: e.activation(out=MS['P'][:, gs, :], in_=pbk, func=AF.Copy), [f'b{4 + (g % 2)}'], [('P', g)])
        pX, pU, pY = bank2(0), bank2(2), bank2(4)
        Pk = [('P', g) for g in range(4)]
        Mk = [('M', hd) for hd in range(16)]
        if not smp:
            def fX(e):
                ins = None
                for hd in range(16):
                    pr, off = hd // 2, 64 * (hd % 2)
                    sl = slice(off, off + 64)
                    cs_ = slice(hd * 64, hd * 64 + 64)
                    e.matmul(pX[:, cs_], lhsT=TQ['at'][sl, pr, :], rhs=STb[sl, pr, :], start=True, stop=False)
                    ins = e.matmul(pX[:, cs_], lhsT=MS['M'][:, hd, :], rhs=BQ['v'][:, cs_], start=False, stop=True)
                return ins
            S.op('pe', fX, R=['T_at', 'STb', 'q_v'] + Mk, W=['b0', 'b1'])
        else:
            def fX(e):
                ins = None
                for hd in range(16):
                    pr, off = hd // 2, 64 * (hd % 2)
                    sl = slice(off, off + 64)
                    cs_ = slice(hd * 64, hd * 64 + 64)
                    S.op('dve', lambda e2: e2.tensor_tensor(out=qm[sl, :, :], in0=TQ['at'][sl, pr, :].unsqueeze(1).to_broadcast([64, 16, 128]),
                                                            in1=smi2[sl, :, :], op=ALU.mult), R=['T_at', 'smi2'], W=['qm'])

                    def fx1(e3):
                        for s_ in range(16):
                            e3.matmul(pX[:, cs_], lhsT=qm[sl, s_, :], rhs=S0Tf[sl, s_, pr, :], start=(s_ == 0), stop=False)
                        return e3.matmul(pX[:, cs_], lhsT=MS['M'][:, hd, :], rhs=BQ['v'][:, cs_], start=False, stop=True)
                    S.op('pe', fx1, R=['qm', 'S0Tf', 'q_v', ('M', hd)], W=['b0', 'b1'])
            fX(None)
        A(lambda e: e.activation(out=BQ['XT'][:], in_=pX, func=AF.Copy), ['b0', 'b1'], ['q_at'])

        def fU(e):
            ins = None
            for hd in range(16):
                cs_ = slice(hd * 64, hd * 64 + 64)
                ins = e.matmul(pU[:, cs_], lhsT=MS['P'][:, hd, :], rhs=BQ['XT'][:, cs_], start=True, stop=True)
            return ins
        S.op('pe', fU, R=['q_at'] + Pk, W=['b2', 'b3'])
        A(lambda e: e.activation(out=BQ['UT'][:], in_=pU, func=AF.Copy), ['b2', 'b3'], ['q_bt'])
        Mbk = [('Mbr', hd) for hd in range(16)] + [('Mkr', hd) for hd in range(16)]
        if not smp:
            def fY(e):
                ins = None
                for hd in range(16):
                    pr, off = hd // 2, 64 * (hd % 2)
                    sl = slice(off, off + 64)
                    cs_ = slice(hd * 64, hd * 64 + 64)
                    e.matmul(pY[:, cs_], lhsT=TQ['rt'][sl, pr, :], rhs=STb[sl, pr, :], start=True, stop=False)
                    e.matmul(pY[:, cs_], lhsT=MS['Mbr'][:, hd, :], rhs=BQ['UT'][:, cs_], start=False, stop=False)
                    ins = e.matmul(pY[:, cs_], lhsT=MS['Mkr'][:, hd, :], rhs=BQ['v'][:, cs_], start=False, stop=True)
                return ins
            S.op('pe', fY, R=['T_rt', 'STb', 'q_bt', 'q_v'] + Mbk, W=['b4', 'b5'])
        else:
            for hd in range(16):
                pr, off = hd // 2, 64 * (hd % 2)
                sl = slice(off, off + 64)
                cs_ = slice(hd * 64, hd * 64 + 64)
                V(lambda e: e.tensor_tensor(out=qm[sl, :, :], in0=TQ['rt'][sl, pr, :].unsqueeze(1).to_broadcast([64, 16, 128]),
                                            in1=smi2[sl, :, :], op=ALU.mult), ['T_rt', 'smi2'], ['qm'])

                def fy1(e):
                    for s_ in range(16):
                        e.matmul(pY[:, cs_], lhsT=qm[sl, s_, :], rhs=S0Tf[sl, s_, pr, :], start=(s_ == 0), stop=False)
                    e.matmul(pY[:, cs_], lhsT=MS['Mbr'][:, hd, :], rhs=BQ['UT'][:, cs_], start=False, stop=False)
                    return e.matmul(pY[:, cs_], lhsT=MS['Mkr'][:, hd, :], rhs=BQ['v'][:, cs_], start=False, stop=True)
                S.op('pe', fy1, R=['qm', 'S0Tf', 'q_bt', 'q_v', ('Mbr', hd), ('Mkr', hd)], W=['b4', 'b5'])
        if not smp:
            pS = bank2(0).rearrange("p (h c) -> p h c", h=16)

            def fS(e):
                ins = None
                for hd in range(16):
                    pr = hd // 2
                    cs_ = slice(hd * 64, hd * 64 + 64)
                    ps_ = slice(pr * 128, (pr + 1) * 128)
                    e.matmul(pS[:, hd, :], lhsT=BQ['bh'][:, ps_], rhs=BQ['UT'][:, cs_], start=True, stop=False)
                    ins = e.matmul(pS[:, hd, :], lhsT=BQ['kh'][:, ps_], rhs=BQ['v'][:, cs_], start=False, stop=True)
                return ins
            S.op('pe', fS, R=['q_bh', 'q_kh', 'q_bt', 'q_v', 'q_at'], W=['b0', 'b1'])
            for h2 in range(2):
                sl = slice(64 * h2, 64 * h2 + 64)
                V(lambda e: e.tensor_tensor(out=STf[sl, :, :], in0=STf[sl, :, :], in1=gCT[sl, :, 0:1].to_broadcast([64, 8, 64]), op=ALU.mult),
                  ['STf', 'gCT'], ['STf'])
                V(lambda e: e.tensor_tensor(out=STf[sl, :, :], in0=STf[sl, :, :],
                                            in1=pS.rearrange("p (a b) c -> p a b c", b=2)[sl, :, h2, :], op=ALU.add), ['STf', 'b0', 'b1'], ['STf'])
            A(lambda e: e.activation(out=STb[:].rearrange("p a b -> p (a b)"), in_=STf[:].rearrange("p a b -> p (a b)"), func=AF.Copy), ['STf'], ['STb'])
            if i == NT - 2:
                def fTo(e):
                    ins = None
                    for pr in range(8):
                        ins = e.transpose(bank2(2)[0:64, pr * 128:(pr + 1) * 128], STf[:, pr, :], identf[:])
                    return ins
                S.op('pe', fTo, R=['STf', 'identf'], W=['b2', 'b3'])
                A(lambda e: e.activation(out=som[:].rearrange("p a b -> p (a b)"), in_=bank2(2)[0:64, :], func=AF.Copy), ['b2', 'b3'], ['som'])
                S.dma('sp', wkv_p.rearrange("(a b) i j -> i a b j", b=2), som[:].rearrange("p a (b j) -> p a b j", b=2), R=['som'], W=['wkv_p'])
        else:
            for pr in range(8):
                ps_ = slice(pr * 128, (pr + 1) * 128)
                V(lambda e: e.tensor_tensor(out=bm[0][:], in0=BQ['bh'][:, ps_].unsqueeze(1).to_broadcast([128, 16, 128]),
                                            in1=smj[:].unsqueeze(2).to_broadcast([128, 16, 128]), op=ALU.mult), ['q_bh', 'smj2'], ['bm0'])
                V(lambda e: e.tensor_tensor(out=bm[1][:], in0=BQ['kh'][:, ps_].unsqueeze(1).to_broadcast([128, 16, 128]),
                                            in1=smj[:].unsqueeze(2).to_broadcast([128, 16, 128]), op=ALU.mult), ['q_kh', 'smj2'], ['bm1'])
                pS4 = psF[:, 0:4, :].rearrange("p a c -> p (a c)").rearrange("p (s b c) -> p s b c", s=16, b=2)

                def fSs(e):
                    ins = None
                    for s_ in range(16):
                        for h2 in range(2):
                            hd = 2 * pr + h2
                            cs_ = slice(hd * 64, hd * 64 + 64)
                            e.matmul(pS4[:, s_, h2, :], lhsT=bm[0][:, s_, :], rhs=BQ['UT'][:, cs_], start=True, stop=False)
                            ins = e.matmul(pS4[:, s_, h2, :], lhsT=bm[1][:, s_, :], rhs=BQ['v'][:, cs_], start=False, stop=True)
                    return ins
                S.op('pe', fSs, R=['bm0', 'bm1', 'q_bt', 'q_v', 'q_at'], W=['b0', 'b1', 'b2', 'b3'])
                for h2 in range(2):
                    sl = slice(64 * h2, 64 * h2 + 64)
                    V(lambda e: e.tensor_tensor(out=S0Tf[sl, :, pr, :], in0=S0Tf[sl, :, pr, :],
                                                in1=gCT[sl, pr, :].unsqueeze(2).to_broadcast([64, 16, 64]), op=ALU.mult),
                      ['S0Tf', 'gCT'], ['S0Tf'])
                    V(lambda e: e.tensor_tensor(out=S0Tf[sl, :, pr, :], in0=S0Tf[sl, :, pr, :], in1=pS4[sl, :, h2, :], op=ALU.add),
                      ['S0Tf', 'b0', 'b1', 'b2', 'b3'], ['S0Tf'])
            for s_ in range(16):
                def fTo(e):
                    ins = None
                    for pr in range(8):
                        ins = e.transpose(bank2(0)[0:64, pr * 128:(pr + 1) * 128], S0Tf[:, s_, pr, :], identf[:])
                    return ins
                S.op('pe', fTo, R=['S0Tf', 'identf'], W=['b0', 'b1'])
                A(lambda e: e.activation(out=som[:].rearrange("p a b -> p (a b)"), in_=bank2(0)[0:64, :], func=AF.Copy), ['b0', 'b1'], ['som'])
                S.dma('sp', wkv_s[s_].rearrange("(a b) i j -> i a b j", b=2), som[:].rearrange("p a (b j) -> p a b j", b=2), R=['som'], W=['wkv_s'])
        ysb, ysq = tmpA, tmpB
        A(lambda e: e.activation(out=ysb[:], in_=pY, func=AF.Copy), ['b4', 'b5'], ['tmpA'])
        A(lambda e: e.activation(out=ysq[:], in_=pY, func=AF.Square), ['b4', 'b5'], ['tmpB'])
        V(lambda e: e.tensor_reduce(out=st16[:, 2, :], in_=h16(ysb[:]), axis=AX.X, op=ALU.add), ['tmpA'], ['st2'])
        V(lambda e: e.tensor_reduce(out=st16[:, 3, :], in_=h16(ysq[:]), axis=AX.X, op=ALU.add), ['tmpB'], ['st3'])
        V(lambda e: e.tensor_scalar(out=st16[:, 2, :], in0=st16[:, 2, :], scalar1=1.0 / 64, scalar2=None, op0=ALU.mult), ['st2'], ['st2'])
        V(lambda e: e.tensor_tensor(out=st16[:, 4, :], in0=st16[:, 2, :], in1=st16[:, 2, :], op=ALU.mult), ['st2'], ['st4'])
        V(lambda e: e.scalar_tensor_tensor(out=st16[:, 3, :], in0=st16[:, 3, :], scalar=1.0 / 64, in1=st16[:, 4, :], op0=ALU.mult, op1=ALU.subtract),
          ['st3', 'st4'], ['st3'])
        V(lambda e: e.tensor_scalar(out=st16[:, 3, :], in0=st16[:, 3, :], scalar1=64e-5, scalar2=None, op0=ALU.add), ['st3'], ['st3'])
        A(lambda e: e.sqrt(out=st16[:, 3, :], in_=st16[:, 3, :]), ['st3'], ['st3'])
        V(lambda e: e.reciprocal(out=st16[:, 3, :], in_=st16[:, 3, :]), ['st3'], ['st3'])
        V(lambda e: e.tensor_tensor(out=h16(ysb[:]), in0=h16(ysb[:]), in1=bc16(st16[:, 2, :]), op=ALU.subtract), ['tmpA', 'st2'], ['tmpA'])
        V(lambda e: e.tensor_tensor(out=h16(ysb[:]), in0=h16(ysb[:]), in1=bc16(st16[:, 3, :]), op=ALU.mult), ['tmpA', 'st3'], ['tmpA'])
        ldrow(O_LW)
        V(lambda e: e.tensor_tensor(out=ysb[:], in0=ysb[:], in1=pbx[:], op=ALU.mult), ['tmpA', 'pbx'], ['tmpA'])
        ldrow(O_LB)
        V(lambda e: e.tensor_tensor(out=ysb[:], in0=ysb[:], in1=pbx[:], op=ALU.add), ['tmpA', 'pbx'], ['tmpA'])
        V(lambda e: e.tensor_tensor(out=h16(ysq[:]), in0=h16(vx), in1=bc16(st16[:, 1, :]), op=ALU.mult), ['ut', 'st1', 'tmpB'], ['tmpB'])
        V(lambda e: e.tensor_tensor(out=ysb[:], in0=ysb[:], in1=ysq[:], op=ALU.add), ['tmpA', 'tmpB'], ['tmpA'])
        V(lambda e: e.tensor_tensor(out=BQ['ob'][:], in0=ysb[:], in1=gg[:], op=ALU.mult), ['tmpA', 'gg'], ['q_kt'])
        S.dma('sp', obD[tsl, :], BQ['ob'][:], R=['q_kt'], W=[('obD', i)])

    S.barrier()
    stack[0].close()
    stack[0] = ExitStack()
    w_oa_v = w_oa.rearrange("(k p) n -> p k n", p=128)
    w_ob_v = w_ob.rearrange("(k p) n -> p k n", p=128)
    w_out_v = w_out.rearrange("(k p) n -> p k n", p=128)
    junk = sb("junkD", [128, D], BF16)
    ss = sb("ssD", [128, 4], F32)
    xg = [sb(f"xg{j}", [128, D], F32) for j in range(4)]
    hbD = [sb(f"hbD{j}", [128, D], BF16) for j in range(2)]
    ost = [sb(f"ost{j}", [128, D], BF16) for j in range(2)]
    hTg = sb("hTg", [128, KC, 512], BF16)
    oTg = sb("oTg", [128, KC, 512], BF16)
    obTg = sb("obTg", [128, 8, 512], BF16)
    mtile = [sb(f"mtile{j}", [128, D], BF16) for j in range(4)]
    WA = [sb(f"WA{j}", [128, KC, 512], BF16) for j in range(3)]
    WB = [sb(f"WB{j}", [128, 8, 512], BF16) for j in range(2)]
    sgA = [sb(f"sgA{j}", [128, 512], F32) for j in range(2)]
    sgB = [sb(f"sgB{j}", [128, 512], F32) for j in range(2)]
    wa_i = [0]
    wb_i = [0]

    def nextWA():
        j = wa_i[0] % 3
        wa_i[0] += 1
        return j

    def mm16(ps, lhsT_fn, W_, nk=KC):
        def f(e):
            ins = None
            for k in range(nk):
                ins = e.matmul(ps, lhsT=lhsT_fn(k), rhs=W_[:, k, :], start=(k == 0), stop=(k == nk - 1))
            return ins
        return f

    groups = [(0, 4), (4, 4), (8, 4), (12, 4), (16, 1)]
    for (i0, n) in (groups if 'D' in ST else []):
        for j in range(n):
            i = i0 + j
            S.dma('sp', xg[j][:], x[i * 128:(i + 1) * 128, :], W=[('xg', j)])
            rmsnorm_tile(xg[j][:], hbD[j % 2][:], gbc[:], ('xg', j), ('hbD', j % 2), j % 2)
            transpose_to(hTg, hbD[j % 2], ('hbD', j % 2), 'hTg', j)
            S.dma('sp', ost[0][:], oD[i * 128:(i + 1) * 128, :], R=[('oD', i)], W=[('ost', 0)])
            transpose_to(oTg, ost[0], ('ost', 0), 'oTg', j)
            S.dma('sp', ost[1][:, 0:1024], obD[i * 128:(i + 1) * 128, :], R=[('obD', i)], W=[('ost', 1)])
            transpose_to(obTg, ost[1], ('ost', 1), 'obTg', j, nk=8)
        for blk in range(4):
            cs_ = slice(blk * 512, (blk + 1) * 512)
            a1, a2, a3 = nextWA(), nextWA(), nextWA()
            b1 = wb_i[0] % 2
            wb_i[0] += 1
            S.dma('pool', WA[a1][:], w_oa_v[:, :, cs_], W=[('WA', a1)])
            S.dma('pool', WB[b1][:], w_ob_v[:, :, cs_], W=[('WB', b1)])
            S.dma('pool', WA[a2][:], w_in_v[:, :, OFF_G + blk * 512:OFF_G + (blk + 1) * 512], W=[('WA', a2)])
            S.dma('pool', WA[a3][:], w_in_v[:, :, OFF_G + D + blk * 512:OFF_G + D + (blk + 1) * 512], W=[('WA', a3)])
            for j in range(n):
                q = j % 2
                tj = slice(j * 128, (j + 1) * 128)
                pGA, pGB, pYA, pYB = psF[:, q, :], psF[:, 2 + q, :], psF[:, 4 + q, :], psF[:, 6, :]
                S.op('pe', mm16(pGA, lambda k: hTg[:, k, tj], WA[a2]), R=[('hTg', j), ('WA', a2)], W=[f'b{q}'])
                S.op('pe', mm16(pGB, lambda k: hTg[:, k, tj], WA[a3]), R=[('hTg', j), ('WA', a3)], W=[f'b{2 + q}'])
                S.op('pe', mm16(pYA, lambda k: oTg[:, k, tj], WA[a1]), R=[('oTg', j), ('WA', a1)], W=[f'b{4 + q}'])
                S.op('pe', mm16(pYB, lambda k: obTg[:, k, tj], WB[b1], nk=8), R=[('obTg', j), ('WB', b1)], W=['b6'])
                S.op('act', lambda e: e.activation(out=sgA[q][:], in_=pGA, func=AF.Sigmoid), R=[f'b{q}'], W=[('sgA', q)])
                S.op('act', lambda e: e.activation(out=sgB[q][:], in_=pGB, func=AF.Sigmoid), R=[f'b{2 + q}'], W=[('sgB', q)])
                S.op('dve', lambda e: e.tensor_tensor(out=sgA[q][:], in0=sgA[q][:], in1=pYA, op=ALU.mult), R=[('sgA', q), f'b{4 + q}'], W=[('sgA', q)])
                S.op('dve', lambda e: e.tensor_tensor(out=sgB[q][:], in0=sgB[q][:], in1=pYB, op=ALU.mult), R=[('sgB', q), 'b6'], W=[('sgB', q)])
                S.op('dve', lambda e: e.tensor_tensor(out=mtile[j][:, cs_], in0=sgA[q][:], in1=sgB[q][:], op=ALU.add),
                     R=[('sgA', q), ('sgB', q)], W=[('mtile', j)])
        for j in range(n):
            transpose_to(oTg, mtile[j], ('mtile', j), 'oTg', j)
        for blk in range(4):
            cs_ = slice(blk * 512, (blk + 1) * 512)
            a1 = nextWA()
            S.dma('pool', WA[a1][:], w_out_v[:, :, cs_], W=[('WA', a1)])
            for j in range(n):
                q = j % 2
                tj = slice(j * 128, (j + 1) * 128)
                S.op('pe', mm16(psF[:, q, :], lambda k: oTg[:, k, tj], WA[a1]), R=[('oTg', j), ('WA', a1)], W=[f'b{q}'])
                S.op('dve', lambda e: e.tensor_tensor(out=xg[j][:, cs_], in0=xg[j][:, cs_], in1=psF[:, q, :], op=ALU.add),
                     R=[('xg', j), f'b{q}'], W=[('xg', j)])
        for j in range(n):
            i = i0 + j
            S.dma('sp', x1D[i * 128:(i + 1) * 128, :], xg[j][:], R=[('xg', j)], W=[('x1D', i)])

    S.barrier()
    stack[0].close()
    stack[0] = None
    slots_i = sb("slots_i", [128, NT, 2], I32)
    wts = sb("wts", [128, NT, 2], F32)
    stack[0] = ExitStack()
    junk = sb("junkE", [128, D], BF16)
    ss = sb("ssE", [128, 4], F32)
    S.dma('sp', gbc[:], g_ffn[0:1, :].partition_broadcast(128), W=['gbc'])
    wr_sb = sb("wr_sb", [128, KC, 36], F32)
    S.dma('sp', wr_sb[:], wr.rearrange("(k p) n -> p k n", p=128), W=['wr_sb'])
    rb_bc = sb("rb_bc", [128, 36], F32)
    S.dma('sp', rb_bc[:], rbias[0:1, :].partition_broadcast(128), W=['rb_bc'])
    rc = {}
    for k in ('c_slt', 'c_ecap', 'c_valid', 'c_trash'):
        rc[k] = sb("s_" + k, list(RCONSTS[k].shape), F32)
        S.dma('sp', rc[k][:], rcd[k], W=[k])
    ones_f = sb("ones_f", [128, 128], F32)
    S.op('dve', lambda e: e.memset(ones_f[:], 1.0), W=['ones_f'])
    base = sb("base", [128, 32], F32)
    S.op('dve', lambda e: e.memset(base[:], 0.0), W=['base'])
    xr = [sb(f"xr{j}", [128, D], F32) for j in range(2)]
    h2f = sb("h2f", [128, D], F32)
    h2b = [sb(f"h2b{j}", [128, D], BF16) for j in range(2)]
    h2T = sb("h2T", [128, KC, 128], F32)
    lg = sb("lg", [128, 36], F32)
    sm = sb("sm", [128, 16], F32)
    gm = sb("gm", [128, 4], F32)
    elm = sb("elm", [128, 32], F32)
    elm2 = sb("elm2", [128, 32], F32)
    oh0 = sb("oh0", [128, 32], F32)
    oh1 = sb("oh1", [128, 32], F32)
    ohs = sb("ohs", [128, 32], F32)
    cb = sb("cb", [128, 32], F32)
    cb2 = sb("cb2", [128, 32], F32)
    t32 = sb("t32", [128, 32], F32)

    def V(fn, R, W):
        S.op('dve', fn, R=R, W=W)

    def A(fn, R, W):
        S.op('act', fn, R=R, W=W)

    for i in range(NT if 'E0' in ST else 0):
        q = i % 2
        S.dma('sp', xr[q][:], x1D[i * 128:(i + 1) * 128, :], R=[('x1D', i)], W=[('xr', q)])
        rmsnorm_tile(xr[q][:], h2f[:], gbc[:], ('xr', q), 'h2f', q)
        A(lambda e: e.activation(out=h2b[q][:], in_=h2f[:], func=AF.Copy), ['h2f'], [('h2b', q)])
        for half in range(4):
            def fT4(e, half=half):
                ins = None
                for k in range(4):
                    kk_ = half * 4 + k
                    ins = e.transpose(psF[:, half % 2, k * 128:(k + 1) * 128], h2f[:, kk_ * 128:(kk_ + 1) * 128], identf[:])
                return ins
            S.op('pe', fT4, R=['h2f', 'identf'], W=[f'b{half % 2}'])
            A(lambda e, half=half: e.activation(out=h2T[:, half * 4:half * 4 + 4, :].rearrange("p a t -> p (a t)"), in_=psF[:, half % 2, :], func=AF.Copy),
              [f'b{half % 2}'], ['h2T'])
        pR = psF[:, 2, 0:36]

        def fR(e):
            ins = None
            for k in range(KC):
                ins = e.matmul(pR, lhsT=h2T[:, k, :], rhs=wr_sb[:, k, :], start=(k == 0), stop=(k == KC - 1))
            return ins
        S.op('pe', fR, R=['h2T', 'wr_sb'], W=['b2'])
        V(lambda e: e.tensor_tensor(out=lg[:], in0=pR, in1=rb_bc[:], op=ALU.add), ['b2', 'rb_bc'], ['lg'])
        V(lambda e: e.tensor_reduce(out=sm[:, 0:1], in_=lg[:, 0:4], axis=AX.X, op=ALU.max), ['lg'], ['sm0'])
        V(lambda e: e.tensor_scalar(out=gm[:], in0=lg[:, 0:4], scalar1=sm[:, 0:1], scalar2=None, op0=ALU.subtract), ['lg', 'sm0'], ['gm'])
        A(lambda e: e.activation(out=t32[:, 0:4], in_=gm[:], func=AF.Exp), ['gm'], ['t32'])
        V(lambda e: e.tensor_reduce(out=sm[:, 1:2], in_=t32[:, 0:4], axis=AX.X, op=ALU.add), ['t32'], ['sm1'])
        V(lambda e: e.reciprocal(out=sm[:, 1:2], in_=sm[:, 1:2]), ['sm1'], ['sm1'])
        V(lambda e: e.tensor_scalar(out=gm[:], in0=gm[:], scalar1=0.0, scalar2=None, op0=ALU.is_equal), ['gm'], ['gm'])
        V(lambda e: e.tensor_scalar(out=gm[:], in0=gm[:], scalar1=-1.0, scalar2=1e30, op0=ALU.add, op1=ALU.mult), ['gm'], ['gm'])
        V(lambda e: e.tensor_tensor(out=elm[:].rearrange("p (g x) -> p g x", g=4), in0=lg[:, 4:36].rearrange("p (g x) -> p g x", g=4),
                                    in1=gm[:].unsqueeze(2).to_broadcast([128, 4, 8]), op=ALU.add), ['lg', 'gm'], ['elm'])
        V(lambda e: e.tensor_reduce(out=sm[:, 2:3], in_=elm[:], axis=AX.X, op=ALU.max), ['elm'], ['sm2'])
        V(lambda e: e.tensor_scalar(out=oh0[:], in0=elm[:], scalar1=sm[:, 2:3], scalar2=None, op0=ALU.is_equal), ['elm', 'sm2'], ['oh0'])
        V(lambda e: e.scalar_tensor_tensor(out=elm2[:], in0=oh0[:], scalar=-1e30, in1=elm[:], op0=ALU.mult, op1=ALU.add), ['oh0', 'elm'], ['elm2'])
        V(lambda e: e.tensor_reduce(out=sm[:, 3:4], in_=elm2[:], axis=AX.X, op=ALU.max), ['elm2'], ['sm3'])
        V(lambda e: e.tensor_scalar(out=oh1[:], in0=elm2[:], scalar1=sm[:, 3:4], scalar2=None, op0=ALU.is_equal), ['elm2', 'sm3'], ['oh1'])
        V(lambda e: e.tensor_tensor(out=sm[:, 4:5], in0=sm[:, 3:4], in1=sm[:, 2:3], op=ALU.subtract), ['sm2', 'sm3'], ['sm4'])
        A(lambda e: e.activation(out=sm[:, 4:5], in_=sm[:, 4:5], func=AF.Exp), ['sm4'], ['sm4'])
        V(lambda e: e.tensor_scalar(out=sm[:, 5:6], in0=sm[:, 4:5], scalar1=1.0, scalar2=None, op0=ALU.add), ['sm4'], ['sm5'])
        V(lambda e: e.reciprocal(out=sm[:, 5:6], in_=sm[:, 5:6]), ['sm5'], ['sm5'])
        V(lambda e: e.tensor_tensor(out=wts[:, i, 0:1], in0=sm[:, 5:6], in1=sm[:, 1:2], op=ALU.mult), ['sm5', 'sm1'], [('wts', i)])
        V(lambda e: e.tensor_tensor(out=wts[:, i, 1:2], in0=wts[:, i, 0:1], in1=sm[:, 4:5], op=ALU.mult), [('wts', i), 'sm4'], [('wts', i)])
        vcol = rc['c_valid'][:, 1:2] if i == NT - 1 else rc['c_valid'][:, 0:1]
        V(lambda e: e.tensor_tensor(out=ohs[:], in0=oh0[:], in1=oh1[:], op=ALU.add), ['oh0', 'oh1'], ['ohs'])
        V(lambda e: e.tensor_scalar(out=ohs[:], in0=ohs[:], scalar1=vcol, scalar2=None, op0=ALU.mult), ['ohs', 'c_valid'], ['ohs'])
        pP, pTot = psF[:, 3, 0:32], psF[:, 3, 64:96]

        def fP2(e):
            e.matmul(pP, lhsT=rc['c_slt'][:], rhs=ohs[:], start=True, stop=True)
            return e.matmul(pTot, lhsT=ones_f[:], rhs=ohs[:], start=True, stop=True)
        S.op('pe', fP2, R=['ohs', 'c_slt', 'ones_f'], W=['b3'])
        V(lambda e: e.tensor_tensor(out=cb[:], in0=pP, in1=base[:], op=ALU.add), ['b3', 'base'], ['cb'])
        V(lambda e: e.tensor_tensor(out=base[:], in0=base[:], in1=pTot, op=ALU.add), ['b3', 'base'], ['base'])
        V(lambda e: e.tensor_tensor(out=cb2[:], in0=cb[:], in1=rc['c_ecap'][:], op=ALU.add), ['cb', 'c_ecap'], ['cb2'])
        for k_, oh_ in ((0, oh0), (1, oh1)):
            c0_ = 6 + 4 * k_
            V(lambda e: e.tensor_tensor(out=t32[:], in0=oh_[:], in1=cb[:], op=ALU.mult), ['oh0', 'oh1', 'cb', 't32'], ['t32'])
            V(lambda e: e.tensor_reduce(out=sm[:, c0_:c0_ + 1], in_=t32[:], axis=AX.X, op=ALU.add), ['t32'], [('smk', k_)])
            V(lambda e: e.tensor_tensor(out=t32[:], in0=oh_[:], in1=cb2[:], op=ALU.mult), ['oh0', 'oh1', 'cb2', 't32'], ['t32'])
            V(lambda e: e.tensor_reduce(out=sm[:, c0_ + 1:c0_ + 2], in_=t32[:], axis=AX.X, op=ALU.add), ['t32'], [('smk', k_)])
            V(lambda e: e.tensor_scalar(out=sm[:, c0_:c0_ + 1], in0=sm[:, c0_:c0_ + 1], scalar1=float(CAP) - 0.5, scalar2=None,
                                        op0=ALU.is_lt), [('smk', k_)], [('smk', k_)])
            V(lambda e: e.tensor_tensor(out=sm[:, c0_:c0_ + 1], in0=sm[:, c0_:c0_ + 1], in1=vcol, op=ALU.mult), [('smk', k_), 'c_valid'], [('smk', k_)])
            V(lambda e: e.tensor_tensor(out=sm[:, c0_ + 1:c0_ + 2], in0=sm[:, c0_ + 1:c0_ + 2], in1=rc['c_trash'][:, 0:1], op=ALU.subtract),
              [('smk', k_), 'c_trash'], [('smk', k_)])
            V(lambda e: e.tensor_tensor(out=sm[:, c0_ + 1:c0_ + 2], in0=sm[:, c0_ + 1:c0_ + 2], in1=sm[:, c0_:c0_ + 1], op=ALU.mult), [('smk', k_)], [('smk', k_)])
            V(lambda e: e.tensor_tensor(out=sm[:, c0_ + 1:c0_ + 2], in0=sm[:, c0_ + 1:c0_ + 2], in1=rc['c_trash'][:, 0:1], op=ALU.add),
              [('smk', k_), 'c_trash'], [('smk', k_)])
            V(lambda e: e.tensor_copy(out=slots_i[:, i, k_:k_ + 1], in_=sm[:, c0_ + 1:c0_ + 2]), [('smk', k_)], [('slots', i, k_)])
            S.dma('pool', None, None, R=[('h2b', q), ('slots', i, k_)], W=['XeD'],
                  fn=lambda e: e.indirect_dma_start(out=XeD[:, :], out_offset=bass.IndirectOffsetOnAxis(ap=slots_i[:, i, k_:k_ + 1], axis=0),
                                                    in_=h2b[q][:, :], in_offset=None))

    S.barrier()
    eg_v = e_gate.rearrange("e (k p) n -> e p k n", p=128)
    eu_v = e_up.rearrange("e (k p) n -> e p k n", p=128)
    ed_v = e_down.rearrange("e (k p) n -> e p k n", p=128)
    WE = [sb(f"WE{j}", [128, KC, 512], BF16) for j in range(6)]
    xe = [sb(f"xe{j}", [128, D], BF16) for j in range(2)]
    XeT = sb("XeT", [128, KC, 256], BF16)
    actT = sb("actT", [128, 8, 256], BF16)
    sl_ = [sb(f"sl{j}", [128, 256], F32) for j in range(2)]
    ye = [sb(f"ye{j}", [128, D], F32) for j in range(2)]
    we_i = [0]

    def nextWE():
        j = we_i[0] % 6
        we_i[0] += 1
        return j

    cnt = 0
    for ex in range(NE if 'E' in ST else 0):
        for t2 in range(2):
            S.dma('sp', xe[t2][:], XeD[ex * CAP + t2 * 128:ex * CAP + (t2 + 1) * 128, :], R=['XeD'], W=[('xe', t2)])
            transpose_to(XeT, xe[t2], ('xe', t2), 'XeT', t2)
        for fh in range(2):
            wg, wu = nextWE(), nextWE()
            S.dma('pool', WE[wg][:], eg_v[ex, :, :, fh * 512:(fh + 1) * 512], W=[('WE', wg)])
            S.dma('pool', WE[wu][:], eu_v[ex, :, :, fh * 512:(fh + 1) * 512], W=[('WE', wu)])
            for fc in range(4):
                q = cnt % 2
                cnt += 1
                pG, pU_ = psF[:, q, 0:256], psF[:, q, 256:512]

                def fGU(e):
                    for k in range(KC):
                        e.matmul(pG, lhsT=WE[wg][:, k, fc * 128:(fc + 1) * 128], rhs=XeT[:, k, :], start=(k == 0), stop=(k == KC - 1))
                    ins = None
                    for k in range(KC):
                        ins = e.matmul(pU_, lhsT=WE[wu][:, k, fc * 128:(fc + 1) * 128], rhs=XeT[:, k, :], start=(k == 0), stop=(k == KC - 1))
                    return ins
                S.op('pe', fGU, R=[('XeT', 0), ('XeT', 1), ('WE', wg), ('WE', wu)], W=[f'b{q}'])
                A(lambda e: e.activation(out=sl_[q][:], in_=pG, func=AF.Silu), [f'b{q}'], [('sl', q)])
                V(lambda e: e.tensor_tensor(out=actT[:, fh * 4 + fc, :], in0=sl_[q][:], in1=pU_, op=ALU.mult), [('sl', q), f'b{q}'], ['actT'])
        wd0, wd1 = nextWE(), nextWE()
        S.dma('pool', WE[wd0][:, 0:8, :], ed_v[ex, :, :, 0:512], W=[('WE', wd0)])
        S.dma('pool', WE[wd0][:, 8:16, :], ed_v[ex, :, :, 512:1024], W=[('WE', wd0)])
        S.dma('pool', WE[wd1][:, 0:8, :], ed_v[ex, :, :, 1024:1536], W=[('WE', wd1)])
        S.dma('pool', WE[wd1][:, 8:16, :], ed_v[ex, :, :, 1536:2048], W=[('WE', wd1)])
        for t2 in range(2):
            for cbk in range(4):
                wsel = WE[wd0] if cbk < 2 else WE[wd1]
                wkey = ('WE', wd0) if cbk < 2 else ('WE', wd1)
                ko = 8 * (cbk % 2)
                q = cnt % 2
                cnt += 1
                pD = psF[:, 2 + q, :]

                def fDn(e):
                    ins = None
                    for k in range(8):
                        ins = e.matmul(pD, lhsT=actT[:, k, t2 * 128:(t2 + 1) * 128], rhs=wsel[:, ko + k, :], start=(k == 0), stop=(k == 7))
                    return ins
                S.op('pe', fDn, R=['actT', wkey], W=[f'b{2 + q}'])
                A(lambda e: e.activation(out=ye[t2][:, cbk * 512:(cbk + 1) * 512], in_=pD, func=AF.Copy), [f'b{2 + q}'], [('ye', t2)])
            S.dma('sp', YeD[ex * CAP + t2 * 128:ex * CAP + (t2 + 1) * 128, :], ye[t2][:], R=[('ye', t2)], W=['YeD'])

    S.barrier()
    stack[0].close()
    stack[0] = ExitStack()
    junk = sb("junkF", [128, D], BF16)
    ss = sb("ssF", [128, 4], F32)
    gfin = sb("gfin", [128, D], F32)
    S.dma('sp', gbc[:], g_ple[0:1, :].partition_broadcast(128), W=['gbc'])
    S.dma('sp', gfin[:], g_final[0:1, :].partition_broadcast(128), W=['gfin'])
    wp_v = w_ple_gate.rearrange("(k p) n -> p k n", p=128)
    wpp_v = w_ple_proj.rearrange("(k p) n -> p k n", p=128)
    xg = [sb(f"xf{j}", [128, D], F32) for j in range(4)]
    yg = [sb(f"yg{j}", [128, D], F32) for j in range(2)]
    hbF = [sb(f"hbF{j}", [128, D], BF16) for j in range(2)]
    h3T = sb("h3T", [128, KC, 512], BF16)
    pt = sb("pt", [128, DPLE], F32)
    ptb = sb("ptb", [128, DPLE], BF16)
    pT = sb("pT", [128, 2, 512], BF16)
    WP = [sb(f"WP{j}", [128, KC, 512], BF16) for j in range(2)]
    WPP = [sb(f"WPP{j}", [128, 2, 512], BF16) for j in range(2)]
    sgF = [sb(f"sgF{j}", [128, 512], F32) for j in range(2)]
    yout = [sb(f"yout{j}", [128, D], F32) for j in range(2)]
    wcnt = 0
    V(lambda e: e.memset(yg[0][:], 0.0), [], [('yg', 0)])
    S.dma('sp', YeD[NSLOT:NSLOT + 128, :], yg[0][:], R=[('yg', 0)], W=['YeD'])
    for (i0, n) in (groups if 'F' in ST else []):
        for j in range(n):
            i = i0 + j
            S.dma('sp', xg[j][:], x1D[i * 128:(i + 1) * 128, :], R=[('x1D', i)], W=[('xf', j)])
            for k_ in range(2):
                V(lambda e: e.memset(yg[k_][:], 0.0), [], [('yg', k_)])
                S.dma('pool', None, None, R=['YeD', ('slots', i, k_)], W=[('yg', k_)],
                      fn=lambda e: e.indirect_dma_start(out=yg[k_][:, :], out_offset=None, in_=YeD[:, :],
                                                        in_offset=bass.IndirectOffsetOnAxis(ap=slots_i[:, i, k_:k_ + 1], axis=0)))
                V(lambda e: e.scalar_tensor_tensor(out=xg[j][:], in0=yg[k_][:], scalar=wts[:, i, k_:k_ + 1], in1=xg[j][:],
                                                   op0=ALU.mult, op1=ALU.add), [('yg', k_), ('wts', i), ('xf', j)], [('xf', j)])
            rmsnorm_tile(xg[j][:], hbF[j % 2][:], gbc[:], ('xf', j), ('hbF', j % 2), j % 2)
            transpose_to(h3T, hbF[j % 2], ('hbF', j % 2), 'h3T', j)
            S.dma('sp', pt[:], p_in[i * 128:(i + 1) * 128, :], W=['pt'])
            A(lambda e: e.activation(out=ptb[:], in_=pt[:], func=AF.Copy), ['pt'], ['ptb'])
            transpose_to(pT, ptb, 'ptb', 'pT', j, nk=2)
        for blk in range(4):
            cs_ = slice(blk * 512, (blk + 1) * 512)
            w1 = wcnt % 2
            wcnt += 1
            S.dma('pool', WP[w1][:], wp_v[:, :, cs_], W=[('WP', w1)])
            S.dma('pool', WPP[w1][:], wpp_v[:, :, cs_], W=[('WPP', w1)])
            for j in range(n):
                q = j % 2
                tj = slice(j * 128, (j + 1) * 128)
                S.op('pe', mm16(psF[:, q, :], lambda k: h3T[:, k, tj], WP[w1]), R=[('h3T', j), ('WP', w1)], W=[f'b{q}'])
                S.op('pe', mm16(psF[:, 2 + q, :], lambda k: pT[:, k, tj], WPP[w1], nk=2), R=[('pT', j), ('WPP', w1)], W=[f'b{2 + q}'])
                A(lambda e: e.activation(out=sgF[q][:], in_=psF[:, q, :], func=AF.Sigmoid), [f'b{q}'], [('sgF', q)])
                V(lambda e: e.tensor_tensor(out=sgF[q][:], in0=sgF[q][:], in1=psF[:, 2 + q, :], op=ALU.mult), [('sgF', q), f'b{2 + q}'], [('sgF', q)])
                V(lambda e: e.tensor_tensor(out=xg[j][:, cs_], in0=xg[j][:, cs_], in1=sgF[q][:], op=ALU.add), [('xf', j), ('sgF', q)], [('xf', j)])
        for j in range(n):
            i = i0 + j
            rmsnorm_tile(xg[j][:], yout[j % 2][:], gfin[:], ('xf', j), ('yout', j % 2), j % 2, gkey='gfin')
            S.dma('sp', y_o[i * 128:(i + 1) * 128, :], yout[j % 2][:], R=[('yout', j % 2)], W=[('y', i)])
    S.finish()
    CONSTS = dict(CONSTS)
    CONSTS.update(WCONSTS)
    CONSTS.update(RCONSTS)
    return nc, CONSTS


_CACHE = {}


def _prep(inp, CONSTS, cores=range(8)):
    f32 = np.float32
    rope = _rope_table()
    ident = np.eye(128, dtype=f32)
    xp = np.asarray(inp['x_prompt'], f32)
    xs = np.asarray(inp['x_sample'], f32)
    prm = np.concatenate([np.asarray(inp[k], f32).reshape(-1) for k in
                          ('wkv_mu', 'wkv_w0', 'wkv_a0', 'wkv_k_k', 'wkv_k_a', 'wkv_r_k', 'wkv_ln_w', 'wkv_ln_b')])[None, :]
    W = {k: np.ascontiguousarray(np.asarray(inp[k], f32)[0]) for k in
         ('w_oa', 'w_ob', 'w_out', 'e_gate', 'e_up', 'e_down', 'w_ple_gate', 'w_ple_proj')}
    for k in ('g_ffn', 'g_ple'):
        W[k] = np.ascontiguousarray(np.asarray(inp[k], f32).reshape(1, D))
    W['g_final'] = np.ascontiguousarray(np.asarray(inp['g_final'], f32).reshape(1, D))
    W['wr'] = np.ascontiguousarray(np.concatenate([np.asarray(inp['router_g_w'], f32)[0], np.asarray(inp['router_e_w'], f32)[0]], 1))
    W['rbias'] = np.ascontiguousarray(np.concatenate([np.asarray(inp['router_g_b'], f32)[0], np.asarray(inp['router_e_b'], f32)[0]])[None, :])
    pp_ = np.asarray(inp['p_prompt'], f32)[0]
    ps_ = np.asarray(inp['p_sample'], f32)[0]
    import os
    if 'E' not in os.environ.get('KSTAGES', 'E').split(','):
        for k in ('e_gate', 'e_up', 'e_down'):
            W[k] = W[k][0:1]
    in_maps = []
    for c in cores:
        pc = np.zeros((T, DPLE), f32)
        pc[:2048] = pp_[c % 4]
        pc[2048:2112] = ps_[16 * c:16 * c + 16].reshape(64, DPLE)
        xc = np.zeros((T, D), f32)
        xc[:2048] = xp[c % 4]
        xc[2048:2112] = xs[16 * c:16 * c + 16].reshape(64, D)
        m = {
            'x': xc,
            'st_ret': np.ascontiguousarray(inp['state_ret'][0, 16 * c:16 * c + 16]),
            'g_mix': np.ascontiguousarray(inp['g_mix']),
            'w_in': np.ascontiguousarray(inp['w_in'][0]),
            'rope': rope,
            'ident': ident,
            'prm': prm,
            'st_shift': np.ascontiguousarray(inp['state_shift'][0, 16 * c:16 * c + 16]),
            'st_wkv': np.ascontiguousarray(inp['state_wkv'][0, 16 * c:16 * c + 16]),
            'wkv_w2': np.ascontiguousarray(inp['wkv_w2'][0]),
            'wkv_a2': np.ascontiguousarray(inp['wkv_a2'][0]),
            'wkv_g2': np.ascontiguousarray(inp['wkv_g2'][0]),
            'w_oa': W['w_oa'], 'w_ob': W['w_ob'], 'w_out': W['w_out'], 'g_ffn': W['g_ffn'], 'wr': W['wr'], 'rbias': W['rbias'],
            'e_gate': W['e_gate'], 'e_up': W['e_up'], 'e_down': W['e_down'], 'g_ple': W['g_ple'],
            'w_ple_gate': W['w_ple_gate'], 'w_ple_proj': W['w_ple_proj'], 'g_final': W['g_final'],
            'p_in': pc,
        }
        m.update(CONSTS)
        in_maps.append(m)
    return in_maps


def kernel(**inp):
    f32 = np.float32
    nc, CONSTS = build()
    in_maps = _prep(inp, CONSTS)
    res = run_bass_kernel_spmd(nc, in_maps, core_ids=list(range(8)))
    R = res.results
    y_p = np.stack([R[c]['y'][:2048] for c in range(4)], 0)
    y_s = np.concatenate([R[c]['y'][2048:2112].reshape(16, 4, D) for c in range(8)], 0)
    ret_p = np.stack([R[c]['ret_p'] for c in range(4)], 0)[None]
    ret_s = np.concatenate([R[c]['ret_s'] for c in range(8)], 0)[None]
    wkv_p = np.stack([R[c]['wkv_p'] for c in range(4)], 0)[None]
    sh_p = np.stack([R[c]['sh_p'][0] for c in range(4)], 0)[None]
    wkv_s = np.concatenate([R[c]['wkv_s'] for c in range(8)], 0)[None]
    sh_s = np.concatenate([R[c]['sh_s'] for c in range(8)], 0)[None]
    return (y_p, y_s, ret_p, wkv_p, sh_p, ret_s, wkv_s, sh_s)
```

```python
import numpy as np
from contextlib import ExitStack
import ml_dtypes
import concourse.bass as bass
import concourse.mybir as mybir
from concourse.bass_utils import run_bass_kernel_spmd

F32 = mybir.dt.float32
BF16 = mybir.dt.bfloat16
I32 = mybir.dt.int32
AF = mybir.ActivationFunctionType
ALU = mybir.AluOpType
AX = mybir.AxisListType

D = 2048
NT = 17
T = NT * 128
KC = 16
RH, RDK, RDV = 8, 128, 256
WH, WN = 16, 64
RWKV_PROJ = 3360
IN_COLS = 13600
OFF_Q, OFF_K, OFF_V, OFF_GR, OFF_ZB, OFF_G = 0, 1024, 2048, 4096, 6144, 9504
NE, EPG, DE = 32, 8, 1024
DPLE = 256
PAST = 16384
EPS = 1e-6
CAP = 128
NSLOT = NE * CAP
NTL = 9
TL = NTL * 128


class Sched:
    def __init__(self, nc):
        self.nc = nc
        self.eng = {'pe': nc.tensor, 'dve': nc.vector, 'act': nc.scalar, 'pool': nc.gpsimd, 'sp': nc.sync}
        self.sem = {}
        self.cnt = {}
        self.nsem = 0
        for e in self.eng:
            self._newsem(e)
        self.waited = {e: {} for e in self.eng}
        self.lastw = {}
        self.readers = {}
        self.RING = 8
        self.ring = {}
        for q in ('sp', 'pool', 'act'):
            self.ring[q] = [[self._mksem(), 0] for _ in range(self.RING)]
        self.ring_i = {q: 0 for q in self.ring}
        self.nins = 0

    def _mksem(self):
        self.nsem += 1
        return (self.nc.semaphore(f"sm{self.nsem}").__enter__(), self.nsem)

    def _newsem(self, e):
        self.sem[e] = self._mksem()
        self.cnt[e] = 0

    def _wait(self, e, tickets):
        w = self.waited[e]
        for (sem, sid, val) in tickets:
            if w.get(sid, 0) >= val:
                continue
            self.eng[e].wait_ge(sem, val)
            w[sid] = val

    @staticmethod
    def _is_psum(k):
        n = k[0] if isinstance(k, tuple) else k
        return isinstance(n, str) and (n in ('psT', 'bkA', 'bkO', 'psZ', 'psP', 'psB') or (len(n) == 2 and n[0] == 'b' and n[1].isdigit()))

    def _deps(self, R, W, me=None):
        t = []
        for k in R:
            if k in self.lastw:
                t.append(self.lastw[k])
            if self._is_psum(k):
                r = self.readers.get(k)
                if r:
                    t.extend(v for s_, v in r.items() if s_ != me)
        for k in W:
            if k in self.lastw:
                t.append(self.lastw[k])
            r = self.readers.get(k)
            if r:
                t.extend(r.values())
        return t

    def _record(self, tk, slot, R, W):
        for k in R:
            self.readers.setdefault(k, {})[slot] = tk
        for k in W:
            self.lastw[k] = tk
            self.readers[k] = {}

    def op(self, e, fn, R=(), W=()):
        deps = self._deps(R, W, e)
        if e == 'pe':
            pesid = self.sem['pe'][1]
            deps = [d for d in deps if d[1] != pesid]
        self._wait(e, deps)
        ins = fn(self.eng[e])
        if self.cnt[e] >= 30000:
            self._newsem(e)
        self.cnt[e] += 1
        sem, sid = self.sem[e]
        ins.then_inc(sem, 1)
        self.nins += 1
        self._record((sem, sid, self.cnt[e]), e, R, W)

    def dma(self, q, out, in_, R=(), W=(), fn=None):
        deps = self._deps(R, W)
        i = self.ring_i[q]
        self.ring_i[q] = (i + 1) % self.RING
        slot = self.ring[q][i]
        (sem, sid), val = slot
        if val > 0:
            deps.append((sem, sid, val))
        self._wait(q, deps)
        if fn is None:
            ins = self.eng[q].dma_start(out=out, in_=in_)
        else:
            ins = fn(self.eng[q])
        slot[1] = val + 16
        ins.then_inc(sem, 16)
        self.nins += 1
        self._record((sem, sid, val + 16), (q, i), R, W)

    def barrier(self):
        tk = []
        for q in self.ring:
            for (sem, sid), val in self.ring[q]:
                if val > 0:
                    tk.append((sem, sid, val))
        for e in ('pe', 'dve', 'act', 'pool'):
            sem, sid = self.sem[e]
            if self.cnt[e] > 0:
                tk.append((sem, sid, self.cnt[e]))
        for e in ('pe', 'dve', 'act', 'pool', 'sp'):
            self._wait(e, tk)

    def finish(self):
        tk = []
        for q in self.ring:
            for (sem, sid), val in self.ring[q]:
                if val > 0:
                    tk.append((sem, sid, val))
        for e in ('pe', 'dve', 'act', 'pool'):
            sem, sid = self.sem[e]
            if self.cnt[e] > 0:
                tk.append((sem, sid, self.cnt[e]))
        self._wait('sp', tk)


def _ret_consts():
    h = np.arange(RH, dtype=np.float64)
    log_g = np.log1p(-np.exp2(-5.0 - h))
    C = 128
    idx = np.arange(C, dtype=np.float64)
    diff = idx[:, None] - idx[None, :]
    scale = RDK ** -0.5
    mask = np.where(diff >= 0, np.exp(log_g[:, None, None] * np.maximum(diff, 0.0)), 0.0)
    maskT = mask.transpose(0, 2, 1) * scale
    qdec = np.exp(log_g[:, None] * (idx + 1.0))
    kdec = np.exp(log_g[:, None] * (C - 1.0 - idx)) * scale
    cdec = np.exp(log_g * C)
    r = np.arange(128)
    s_id = r // 4
    t_id = (r % 4).astype(np.float64)
    valid = r < 64
    same = (s_id[:, None] == s_id[None, :]) & valid[:, None] & valid[None, :]
    dd = t_id[:, None] - t_id[None, :]
    mask_s = np.where(same & (dd >= 0), np.exp(log_g[:, None, None] * np.maximum(dd, 0.0)), 0.0)
    maskT_s = mask_s.transpose(0, 2, 1) * scale
    qdec_s = np.exp(log_g[:, None] * (t_id + 1.0)) * valid
    kdec_s = np.exp(log_g[:, None] * (3.0 - t_id)) * scale * valid
    cdec_s = np.exp(log_g * 4.0)
    out = {
        'c_maskT': np.ascontiguousarray(maskT.transpose(1, 0, 2)).astype(np.float32),
        'c_maskT_s': np.ascontiguousarray(maskT_s.transpose(1, 0, 2)).astype(np.float32),
        'c_qdec': np.broadcast_to(qdec[None], (128, RH, 128)).astype(np.float32).copy(),
        'c_qdec_s': np.broadcast_to(qdec_s[None], (128, RH, 128)).astype(np.float32).copy(),
        'c_kdec': np.ascontiguousarray(kdec.T).astype(np.float32),
        'c_kdec_s': np.ascontiguousarray(kdec_s.T).astype(np.float32),
    }
    smi = np.zeros((128, 16, 128), np.float32)
    for s in range(16):
        smi[:, s, 4 * s:4 * s + 4] = 1.0
    smj = np.zeros((128, 16), np.float32)
    for s in range(16):
        smj[4 * s:4 * s + 4, s] = 1.0
    out['c_smi'] = smi
    out['c_smj'] = smj
    return out, [float(x) for x in cdec], [float(x) for x in cdec_s]


def _wkv_consts():
    r = np.arange(128)
    up = (r[:, None] <= r[None, :])
    sup = (r[:, None] < r[None, :])
    slo = (r[:, None] > r[None, :])
    valid = r < 64
    same = (r[:, None] // 4 == r[None, :] // 4) & valid[:, None] & valid[None, :]
    tri = np.stack([up, up & same], 1).astype(np.float32)
    ones = np.stack([np.ones((128, 128), bool), same], 1).astype(np.float32)
    m5 = np.zeros((128, 2, 5, 128), np.float32)
    for v, extra in ((0, np.ones((128, 128), bool)), (1, same)):
        m5[:, v, 0] = sup & extra
        m5[:, v, 1] = slo & extra
        m5[:, v, 2] = sup & extra
        m5[:, v, 3] = up & extra
        m5[:, v, 4] = up & extra
    tm = ((r % 4 != 0) & valid).astype(np.float32)[:, None]
    shift = np.zeros((128, 3, 128), np.float32)
    shift[:, 0] = (r[:, None] == r[None, :] - 1)
    shift[:, 1] = (r[:, None] == r[None, :] - 1) & same & (r[None, :] % 4 != 0)
    shift[127, 2, 0] = 1.0
    return {'c_tri': tri, 'c_ones': ones, 'c_m5': m5, 'c_tm': tm, 'c_shift': shift}


def _route_consts():
    r = np.arange(128)
    slt = (r[:, None] < r[None, :]).astype(np.float32)
    ecap = np.broadcast_to((np.arange(32, dtype=np.float32) * CAP)[None, :], (128, 32)).copy()
    valid = np.stack([np.ones(128, np.float32), (r < 64).astype(np.float32)], 1)
    trash = (NSLOT + r).astype(np.float32)[:, None]
    return {'c_slt': slt, 'c_ecap': ecap, 'c_valid': valid, 'c_trash': trash}


def _rope_table():
    half = RDK // 2
    inv = (10000.0 ** (-np.arange(half, dtype=np.float32) / half)).astype(np.float32)
    pos = np.zeros((T,), np.float32)
    pos[:2048] = np.arange(2048, dtype=np.float32)
    pos[2048:2112] = np.tile(PAST + np.arange(4, dtype=np.float32), 16)
    ang = pos[:, None] * inv[None, :]
    tab = np.stack([np.cos(ang), np.sin(ang)], 1).astype(np.float32)
    return tab


def build(debug_stage=99):
    import os
    ST = os.environ.get('KSTAGES', 'B,C1,C2,D,E0,E,F').split(',')
    nc = bass.Bass("TRN2", target_bir_lowering=False)
    S = Sched(nc)
    CONSTS, CDEC, CDEC_S = _ret_consts()

    def din(name, shape, dt=F32):
        return nc.dram_tensor(name, list(shape), dt, kind="ExternalInput").ap()

    def dout(name, shape, dt=F32):
        return nc.dram_tensor(name, list(shape), dt, kind="ExternalOutput").ap()

    def dscr(name, shape, dt=F32):
        return nc.dram_tensor(name, list(shape), dt, kind="Internal").ap()

    stack = [None]

    def sb(name, shape, dt=F32):
        cm = nc.sbuf_tensor(name, list(shape), dt)
        if stack[0] is not None:
            return stack[0].enter_context(cm)
        return cm.__enter__()

    x = din("x", [T, D])
    st_ret = din("st_ret", [16, RH, RDK, RDV])
    g_mix = din("g_mix", [1, D])
    w_in = din("w_in", [D, IN_COLS])
    rope = din("rope", [T, 2, 64])
    ident_d = din("ident", [128, 128])
    cst = {k: din(k, v.shape) for k, v in CONSTS.items()}

    prm = din("prm", [1, RWKV_PROJ + 7168])
    st_shift = din("st_shift", [16, RWKV_PROJ])
    st_wkv = din("st_wkv", [16, WH, WN, WN])
    wkv_w2 = din("wkv_w2", [64, 1024])
    wkv_a2 = din("wkv_a2", [64, 1024])
    wkv_g2 = din("wkv_g2", [160, 1024])
    WCONSTS = _wkv_consts()
    wc = {k: din(k, v.shape) for k, v in WCONSTS.items()}
    w_oa = din("w_oa", [D, D])
    w_ob = din("w_ob", [1024, D])
    w_out = din("w_out", [D, D])
    g_ffn = din("g_ffn", [1, D])
    wr = din("wr", [D, 36])
    rbias = din("rbias", [1, 36])
    NE_D = NE if 'E' in ST else 1
    e_gate = din("e_gate", [NE_D, D, DE])
    e_up = din("e_up", [NE_D, D, DE])
    e_down = din("e_down", [NE_D, DE, D])
    g_ple = din("g_ple", [1, D])
    w_ple_gate = din("w_ple_gate", [D, D])
    w_ple_proj = din("w_ple_proj", [DPLE, D])
    g_final = din("g_final", [1, D])
    p_in = din("p_in", [TL, DPLE])
    x_h = din("x_h", [TL, D])
    ridx = din("ridx", [128, NTL], I32)
    RCONSTS = _route_consts()
    rcd = {k: din(k, v.shape) for k, v in RCONSTS.items()}
    x1D = dscr("x1D", [TL, D], F32)
    XeD = dscr("XeD", [NSLOT + 128, D], BF16)
    YeD = dscr("YeD", [NSLOT + 128, D], F32)
    sh_p = dout("sh_p", [1, RWKV_PROJ])
    sh_s = dout("sh_s", [16, RWKV_PROJ])
    wkv_p = dout("wkv_p", [WH, WN, WN])
    wkv_s = dout("wkv_s", [16, WH, WN, WN])
    uD = dscr("uD", [T, RWKV_PROJ], F32)
    obD = dscr("obD", [T, 1024], BF16)
    y_o = dout("y", [TL, D])
    ret_p = dout("ret_p", [RH, RDK, RDV])
    ret_s = dout("ret_s", [16, RH, RDK, RDV])

    oD = dscr("oD", [T, D], BF16)

    w_in_v = w_in.rearrange("(k p) n -> p k n", p=128)

    identb = sb("identb", [128, 128], BF16)
    identf = sb("identf", [128, 128], F32)
    gbc = sb("gbc", [128, D], F32)
    hT_stack = ExitStack()
    hT = hT_stack.enter_context(nc.sbuf_tensor("hT", [128, KC, T], BF16))
    psF = nc.psum_tensor("psF", [128, 7, 512], F32).__enter__()
    psT = nc.psum_tensor("psT", [128, 1024], BF16).__enter__()

    S.dma('sp', identf[:], ident_d[:, :], W=['identf'])
    S.op('dve', lambda e: e.tensor_copy(out=identb[:], in_=identf[:]), R=['identf'], W=['identb'])
    S.dma('sp', gbc[:], g_mix[0:1, :].partition_broadcast(128), W=['gbc'])

    stack[0] = ExitStack()
    xt = [sb(f"xt{i}", [128, D], F32) for i in range(2)]
    hb = [sb(f"hb{i}", [128, D], BF16) for i in range(2)]
    junk = sb("junk", [128, D], BF16)
    ss = sb("ss", [128, 4], F32)

    def rmsnorm_tile(src, dst_bf, gtile, key_src, key_dst, u, gkey='gbc'):
        S.op('act', lambda e: e.activation(out=junk[:], in_=src, func=AF.Square, accum_out=ss[:, u:u + 1]),
             R=[key_src], W=['junk', ('ss', u)])
        S.op('dve', lambda e: e.tensor_scalar(out=ss[:, u:u + 1], in0=ss[:, u:u + 1], scalar1=1.0 / D, scalar2=EPS,
                                              op0=ALU.mult, op1=ALU.add), R=[('ss', u)], W=[('ss', u)])
        S.op('act', lambda e: e.sqrt(out=ss[:, u:u + 1], in_=ss[:, u:u + 1]), R=[('ss', u)], W=[('ss', u)])
        S.op('dve', lambda e: e.reciprocal(out=ss[:, u:u + 1], in_=ss[:, u:u + 1]), R=[('ss', u)], W=[('ss', u)])
        S.op('dve', lambda e: e.scalar_tensor_tensor(out=dst_bf, in0=src, scalar=ss[:, u:u + 1], in1=gtile,
                                                     op0=ALU.mult, op1=ALU.mult),
             R=[key_src, ('ss', u), gkey], W=[key_dst])

    def transpose_to(dstT, src_bf, key_src, key_dst, i, nk=KC):
        for half in range(0, nk, 8):
            n = min(8, nk - half)

            def f(e, half=half, n=n):
                ins = None
                for k in range(n):
                    ins = e.transpose(psT[:, k * 128:(k + 1) * 128], src_bf[:, (half + k) * 128:(half + k + 1) * 128], identb[:])
                return ins
            S.op('pe', f, R=[key_src, 'identb'], W=['psT'])
            S.op('act', lambda e, half=half, n=n: e.activation(
                out=dstT[:, half:half + n, i * 128:(i + 1) * 128],
                in_=psT[:, 0:n * 128].rearrange("p (k t) -> p k t", k=n), func=AF.Copy),
                R=['psT'], W=[(key_dst, i)])

    for i in range(NT):
        b = i % 2
        S.dma('sp', xt[b][:], x[i * 128:(i + 1) * 128, :], W=[('xt', b)])
        rmsnorm_tile(xt[b][:], hb[b][:], gbc[:], ('xt', b), ('hb', b), b)
        transpose_to(hT, hb[b], ('hb', b), 'hT', i)

    S.barrier()
    stack[0].close()
    if True:
        stack[0] = ExitStack()
        junk = sb("junkB", [128, 256], BF16)
        cs = {}
        for k, v in CONSTS.items():
            if k == 'c_smi':
                continue
            cs[k] = sb("s_" + k, list(v.shape), F32)
            S.dma('sp', cs[k][:], cst[k], W=[k])
        smi_b = sb("smi_b", [128, 16, 128], BF16)
        S.dma('pool', smi_b[:], cst['c_smi'], W=['smi_b'])
        ropet = sb("ropet", [128, NT, 2, 64], F32)
        S.dma('sp', ropet[:], rope.rearrange("(n p) a f -> p n a f", p=128), W=['rope'])

        Wqk = [sb(f"Wqk{i}", [128, KC, 256], BF16) for i in range(2)]
        Wvg = [sb(f"Wvg{i}", [128, KC, 512], BF16) for i in range(2)]
        qk_r = [sb(f"qk_r{i}", [128, 2, 128], BF16) for i in range(2)]
        rt = [sb(f"rt{i}", [128, 2, 2, 64], F32) for i in range(4)]
        v_bf = [sb(f"v_bf{i}", [128, 256], BF16) for i in range(2)]
        sg = [sb(f"sg{i}", [128, 256], F32) for i in range(2)]
        qkT = [sb(f"qkT{i}", [128, 2, 128], BF16) for i in range(2)]
        qTd = [sb(f"qTd{i}", [128, 128], BF16) for i in range(2)]
        kd = [sb(f"kd{i}", [128, 128], BF16) for i in range(2)]
        sT = [sb(f"sT{i}", [128, 128], BF16) for i in range(2)]
        Sf = sb("Sf", [128, 256], F32)
        Sb = [sb(f"Sb{i}", [128, 256], BF16) for i in range(2)]
        on = [sb(f"on{i}", [128, 256], BF16) for i in range(2)]
        oss = sb("oss", [128, 2], F32)
        S0f = sb("S0f", [128, 8, 256], F32)
        S0b = sb("S0b", [128, 16, 256], BF16)
        Snew = sb("Snew", [128, 8, 256], F32)
        qTd_m = sb("qTd_m", [128, 16, 128], BF16)
        kd_m = sb("kd_m", [128, 16, 128], BF16)

        for h in range(RH if 'B' in ST else 0):
            wb = h % 2
            S.dma('pool', Wqk[wb][:, :, 0:128], w_in_v[:, :, OFF_Q + h * 128:OFF_Q + (h + 1) * 128], W=[('Wqk', wb)])
            S.dma('pool', Wqk[wb][:, :, 128:256], w_in_v[:, :, OFF_K + h * 128:OFF_K + (h + 1) * 128], W=[('Wqk', wb)])
            S.dma('pool', Wvg[wb][:, :, 0:256], w_in_v[:, :, OFF_V + h * 256:OFF_V + (h + 1) * 256], W=[('Wvg', wb)])
            S.dma('pool', Wvg[wb][:, :, 256:512], w_in_v[:, :, OFF_GR + h * 256:OFF_GR + (h + 1) * 256], W=[('Wvg', wb)])
            S.dma('pool', S0b[:], st_ret[:, h, :, :].rearrange("s d e -> d s e"), W=['S0b'])
            S.op('dve', lambda e: e.memset(Sf[:], 0.0), W=['Sf'])
            S.op('dve', lambda e: e.memset(Sb[0][:], 0.0), W=[('Sb', 0)])

            def tileB(i, phase):
                p = i % 2
                smp = (i == NT - 1)
                bA, bB, bO = 3 * p, 3 * p + 1, 3 * p + 2
                psA = psF[:, bA, 0:256]
                psSC = psF[:, bA, 256:384]
                psB = psF[:, bB, :]
                psO = psF[:, bO, 0:256]
                psDS = psF[:, bO, 256:512]
                tsl = slice(i * 128, (i + 1) * 128)

                if phase == 1:
                    def fA(e):
                        ins = None
                        for k in range(KC):
                            ins = e.matmul(psA, lhsT=hT[:, k, tsl], rhs=Wqk[wb][:, k, :], start=(k == 0), stop=(k == KC - 1))
                        return ins
                    S.op('pe', fA, R=[('hT', i), ('Wqk', wb)], W=[('bkA', p)])

                    def fB(e):
                        ins = None
                        for k in range(KC):
                            ins = e.matmul(psB, lhsT=hT[:, k, tsl], rhs=Wvg[wb][:, k, :], start=(k == 0), stop=(k == KC - 1))
                        return ins
                    S.op('pe', fB, R=[('hT', i), ('Wvg', wb)], W=[('psB', p)])

                    return
                A4 = psA.rearrange("p (a b f) -> p a b f", a=2, b=2)
                cosb = ropet[:, i, 0:1, :].to_broadcast([128, 2, 64])
                sinb = ropet[:, i, 1:2, :].to_broadcast([128, 2, 64])
                r0, r1, r2, r3 = rt[0], rt[1], rt[2], rt[3]
                S.op('dve', lambda e: e.tensor_tensor(out=r0[:, :, 0, :], in0=A4[:, :, 0, :], in1=cosb, op=ALU.mult),
                     R=[('bkA', p), 'rope'], W=['r0a'])
                S.op('dve', lambda e: e.tensor_tensor(out=r0[:, :, 1, :], in0=A4[:, :, 1, :], in1=sinb, op=ALU.mult),
                     R=[('bkA', p), 'rope'], W=['r0b'])
                S.op('dve', lambda e: e.tensor_tensor(out=r1[:, :, 0, :], in0=A4[:, :, 0, :], in1=sinb, op=ALU.mult),
                     R=[('bkA', p), 'rope'], W=['r1a'])
                S.op('dve', lambda e: e.tensor_tensor(out=r1[:, :, 1, :], in0=A4[:, :, 1, :], in1=cosb, op=ALU.mult),
                     R=[('bkA', p), 'rope'], W=['r1b'])
                qkr4 = qk_r[p][:].rearrange("p a (b f) -> p a b f", b=2)
                S.op('dve', lambda e: e.tensor_tensor(out=qkr4[:, :, 0, :], in0=r0[:, :, 0, :], in1=r0[:, :, 1, :], op=ALU.subtract),
                     R=['r0a', 'r0b'], W=[('qk_r', p, 0)])
                S.op('dve', lambda e: e.tensor_tensor(out=qkr4[:, :, 1, :], in0=r1[:, :, 0, :], in1=r1[:, :, 1, :], op=ALU.add),
                     R=['r1a', 'r1b'], W=[('qk_r', p, 1)])
                S.op('act', lambda e: e.activation(out=v_bf[p][:], in_=psB[:, 0:256], func=AF.Copy), R=[('psB', p)], W=[('v_bf', p)])
                S.op('act', lambda e: e.activation(out=sg[p][:], in_=psB[:, 256:512], func=AF.Silu), R=[('psB', p)], W=[('sg', p)])
                tT = psT[:, 512 + p * 256:512 + (p + 1) * 256]

                def fT(e):
                    e.transpose(tT[:, 0:128], qk_r[p][:, 0, :], identb[:])
                    return e.transpose(tT[:, 128:256], qk_r[p][:, 1, :], identb[:])
                S.op('pe', fT, R=[('qk_r', p, 0), ('qk_r', p, 1), 'identb'], W=['psT'])
                S.op('act', lambda e: e.activation(out=qkT[p][:].rearrange("p a t -> p (a t)"), in_=tT, func=AF.Copy),
                     R=['psT'], W=[('qkT', p)])
                qd = cs['c_qdec_s'] if smp else cs['c_qdec']
                kdc = cs['c_kdec_s'] if smp else cs['c_kdec']
                mk = cs['c_maskT_s'] if smp else cs['c_maskT']
                S.op('dve', lambda e: e.tensor_tensor(out=qTd[p][:], in0=qkT[p][:, 0, :], in1=qd[:, h, :], op=ALU.mult),
                     R=[('qkT', p), 'c_qdec', 'c_qdec_s'], W=[('qTd', p)])
                S.op('dve', lambda e: e.tensor_scalar(out=kd[p][:], in0=qk_r[p][:, 1, :], scalar1=kdc[:, h:h + 1], scalar2=None,
                                                      op0=ALU.mult), R=[('qk_r', p, 0), ('qk_r', p, 1), 'c_kdec', 'c_kdec_s'], W=[('kd', p)])
                S.op('pe', lambda e: e.matmul(psSC, lhsT=qkT[p][:, 1, :], rhs=qkT[p][:, 0, :], start=True, stop=True),
                     R=[('qkT', p)], W=[('bkA', p)])
                S.op('dve', lambda e: e.tensor_tensor(out=sT[p][:], in0=psSC, in1=mk[:, h, :], op=ALU.mult),
                     R=[('bkA', p), 'c_maskT', 'c_maskT_s'], W=[('sT', p)])
                if not smp:
                    sbp = i % 2

                    def fO(e):
                        e.matmul(psO, lhsT=sT[p][:], rhs=v_bf[p][:], start=True, stop=False)
                        return e.matmul(psO, lhsT=qTd[p][:], rhs=Sb[sbp][:], start=False, stop=True)
                    S.op('pe', fO, R=[('sT', p), ('v_bf', p), ('qTd', p), ('Sb', sbp)], W=[('bkO', p)])
                    S.op('pe', lambda e: e.matmul(psDS, lhsT=kd[p][:], rhs=v_bf[p][:], start=True, stop=True),
                         R=[('kd', p), ('v_bf', p)], W=[('bkO', p)])
                    S.op('dve', lambda e: e.scalar_tensor_tensor(out=Sf[:], in0=Sf[:], scalar=CDEC[h], in1=psDS,
                                                                 op0=ALU.mult, op1=ALU.add), R=['Sf', ('bkO', p)], W=['Sf'])
                    S.op('act', lambda e: e.activation(out=Sb[1 - sbp][:], in_=Sf[:], func=AF.Copy), R=['Sf'], W=[('Sb', 1 - sbp)])
                    if i == NT - 2:
                        S.dma('sp', ret_p[h, :, :], Sf[:], R=['Sf'], W=['ret_p'])
                else:
                    S.op('dve', lambda e: e.tensor_tensor(out=qTd_m[:], in0=qTd[p][:].unsqueeze(1).to_broadcast([128, 16, 128]),
                                                          in1=smi_b[:], op=ALU.mult), R=[('qTd', p), 'smi_b'], W=['qTd_m'])
                    S.op('dve', lambda e: e.tensor_tensor(out=kd_m[:], in0=kd[p][:].unsqueeze(1).to_broadcast([128, 16, 128]),
                                                          in1=cs['c_smj'][:].unsqueeze(2).to_broadcast([128, 16, 128]), op=ALU.mult),
                         R=[('kd', p), 'c_smj'], W=['kd_m'])

                    def fO(e):
                        e.matmul(psO, lhsT=sT[p][:], rhs=v_bf[p][:], start=True, stop=False)
                        ins = None
                        for s in range(16):
                            ins = e.matmul(psO, lhsT=qTd_m[:, s, :], rhs=S0b[:, s, :], start=False, stop=(s == 15))
                        return ins
                    S.op('pe', fO, R=[('sT', p), ('v_bf', p), 'qTd_m', 'S0b'], W=[('bkO', p)])
                    for s2 in range(8):
                        bank6 = psF[:, 6, :]
                        if s2 % 4 == 0:
                            hf = s2 // 4
                            S.dma('sp', S0f[:], st_ret[8 * hf:8 * hf + 8, h, :, :].rearrange("s d e -> d s e"), W=['S0f'])

                        def fD(e, s2=s2):
                            e.matmul(bank6[:, 0:256], lhsT=kd_m[:, 2 * s2, :], rhs=v_bf[p][:], start=True, stop=True)
                            return e.matmul(bank6[:, 256:512], lhsT=kd_m[:, 2 * s2 + 1, :], rhs=v_bf[p][:], start=True, stop=True)
                        S.op('pe', fD, R=['kd_m', ('v_bf', p)], W=['bank6'])
                        S.op('dve', lambda e, s2=s2: e.scalar_tensor_tensor(
                            out=Snew[:, 2 * (s2 % 4):2 * (s2 % 4) + 2, :], in0=S0f[:, 2 * (s2 % 4):2 * (s2 % 4) + 2, :], scalar=CDEC_S[h],
                            in1=bank6.rearrange("p (a b) -> p a b", a=2), op0=ALU.mult, op1=ALU.add),
                            R=['S0f', 'bank6'], W=['Snew'])
                        if s2 % 4 == 3:
                            hf = s2 // 4
                            S.dma('sp', ret_s[8 * hf:8 * hf + 8, h, :, :].rearrange("s d e -> d s e"), Snew[:], R=['Snew'], W=['ret_s'])
                u = p
                S.op('act', lambda e: e.activation(out=junk[:, 0:256], in_=psO, func=AF.Square, accum_out=oss[:, u:u + 1]),
                     R=[('bkO', p)], W=['junk', ('oss', u)])
                S.op('dve', lambda e: e.tensor_scalar(out=oss[:, u:u + 1], in0=oss[:, u:u + 1], scalar1=1.0 / RDV, scalar2=EPS,
                                                      op0=ALU.mult, op1=ALU.add), R=[('oss', u)], W=[('oss', u)])
                S.op('act', lambda e: e.sqrt(out=oss[:, u:u + 1], in_=oss[:, u:u + 1]), R=[('oss', u)], W=[('oss', u)])
                S.op('dve', lambda e: e.reciprocal(out=oss[:, u:u + 1], in_=oss[:, u:u + 1]), R=[('oss', u)], W=[('oss', u)])
                S.op('dve', lambda e: e.scalar_tensor_tensor(out=on[p][:], in0=psO, scalar=oss[:, u:u + 1], in1=sg[p][:],
                                                             op0=ALU.mult, op1=ALU.mult),
                     R=[('bkO', p), ('oss', u), ('sg', p)], W=[('on', p)])
                S.dma('sp', oD[tsl, h * 256:(h + 1) * 256], on[p][:], R=[('on', p)], W=[('oD', i)])

            tileB(0, 1)
            for i in range(NT):
                if i + 1 < NT:
                    tileB(i + 1, 1)
                tileB(i, 2)

    S.barrier()
    stack[0].close()
    stack[0] = ExitStack()
    mu_bc = sb("mu_bc", [128, RWKV_PROJ], F32)
    S.dma('sp', mu_bc[:], prm[0:1, 0:RWKV_PROJ].partition_broadcast(128), W=['mu_bc'])
    shrows = sb("shrows", [128, RWKV_PROJ], F32)
    S.op('dve', lambda e: e.memset(shrows[:], 0.0), W=['shrows'])
    for s_ in range(16):
        S.dma('sp', shrows[4 * s_:4 * s_ + 1, :], st_shift[s_:s_ + 1, :], W=['shrows'])
    tmk = sb("tmk", [128, 1], F32)
    S.dma('sp', tmk[:], wc['c_tm'], W=['tmk'])
    Wz = [sb(f"Wz{i}", [128, KC, 512], BF16) for i in range(2)]
    shm = sb("shm", [128, 3, 128], F32)
    S.dma('sp', shm[:], wc['c_shift'], W=['shm'])
    zbs = [sb(f"zbs{i}", [128, 512], F32) for i in range(2)]
    dd = [sb(f"dd{i}", [128, 512], F32) for i in range(2)]
    uu = [sb(f"uu{i}", [128, 512], F32) for i in range(2)]
    c1_items = [(blk, i) for blk in range(7 if 'C1' in ST else 0) for i in range(NT)]

    def itemC1(idx, phase):
        blk, i = c1_items[idx]
        p = idx % 2
        wd_ = min(512, RWKV_PROJ - blk * 512)
        c0 = OFF_ZB + blk * 512
        wb = blk % 2
        if phase == 1 and i == 0:
            S.dma('pool', Wz[wb][:, :, 0:wd_], w_in_v[:, :, c0:c0 + wd_], W=[('Wz', wb)])
        tsl = slice(i * 128, (i + 1) * 128)
        psZ = psF[:, 2 * p, 0:wd_]
        psP = psF[:, 2 * p + 1, 0:wd_]

        if phase == 1:
            def fZ(e):
                ins = None
                for k in range(KC):
                    ins = e.matmul(psZ, lhsT=hT[:, k, tsl], rhs=Wz[wb][:, k, 0:wd_], start=(k == 0), stop=(k == KC - 1))
                return ins
            S.op('pe', fZ, R=[('hT', i), ('Wz', wb)], W=[('psZ', p)])
            return


        S.op('act', lambda e: e.activation(out=zbs[p][:, 0:wd_], in_=psZ, func=AF.Copy), R=[('psZ', p)], W=[('zbs', p)])
        carry = (0 < i < NT - 1)

        def fP(e):
            ins = e.matmul(psP, lhsT=shm[:, 1 if i == NT - 1 else 0, :], rhs=zbs[p][:, 0:wd_], start=True, stop=not carry)
            if carry:
                ins = e.matmul(psP, lhsT=shm[:, 2, :], rhs=zbs[1 - p][:, 0:wd_], start=False, stop=True)
            return ins
        S.op('pe', fP, R=[('zbs', p), ('zbs', 1 - p), 'shm'], W=[('psP', p)])
        if i == NT - 1:
            S.op('dve', lambda e: e.tensor_tensor(out=dd[p][:, 0:wd_], in0=psP, in1=shrows[:, blk * 512:blk * 512 + wd_], op=ALU.add),
                 R=[('psP', p), 'shrows'], W=[('dd', p)])
            S.op('dve', lambda e: e.tensor_tensor(out=dd[p][:, 0:wd_], in0=dd[p][:, 0:wd_], in1=zbs[p][:, 0:wd_], op=ALU.subtract),
                 R=[('dd', p), ('zbs', p)], W=[('dd', p)])
        else:
            S.op('dve', lambda e: e.tensor_tensor(out=dd[p][:, 0:wd_], in0=psP, in1=zbs[p][:, 0:wd_], op=ALU.subtract),
                 R=[('psP', p), ('zbs', p)], W=[('dd', p)])
        S.op('dve', lambda e: e.tensor_tensor(out=dd[p][:, 0:wd_], in0=dd[p][:, 0:wd_], in1=mu_bc[:, blk * 512:blk * 512 + wd_], op=ALU.mult),
             R=[('dd', p), 'mu_bc'], W=[('dd', p)])
        S.op('dve', lambda e: e.tensor_tensor(out=uu[p][:, 0:wd_], in0=dd[p][:, 0:wd_], in1=zbs[p][:, 0:wd_], op=ALU.add),
             R=[('dd', p), ('zbs', p)], W=[('uu', p)])
        S.dma('sp', uD[tsl, blk * 512:blk * 512 + wd_], uu[p][:, 0:wd_], R=[('uu', p)], W=[('uD', i)])
        if i == NT - 2:
            S.dma('sp', sh_p[0:1, blk * 512:blk * 512 + wd_], zbs[p][127:128, 0:wd_], R=[('zbs', p)], W=['sh_p'])
        if i == NT - 1:
            for s_ in range(16):
                S.dma('sp', sh_s[s_:s_ + 1, blk * 512:blk * 512 + wd_], zbs[p][4 * s_ + 3:4 * s_ + 4, 0:wd_], R=[('zbs', p)], W=['sh_s'])

    if c1_items:
        itemC1(0, 1)
    for idx in range(len(c1_items)):
        if idx + 1 < len(c1_items):
            itemC1(idx + 1, 1)
        itemC1(idx, 2)

    S.barrier()
    stack[0].close()
    stack[0] = ExitStack()
    hT_stack.close()
    NP = 4096
    O_W0, O_A0, O_KK, O_KA, O_RK, O_LW, O_LB = [1024 * j for j in range(7)]
    pb = sb("pb", [128, NP], F32)
    S.dma('sp', pb[:], prm[0:1, RWKV_PROJ:RWKV_PROJ + NP].partition_broadcast(128), W=['pb'])
    pbx = sb("pbx", [128, 1024], F32)

    def ldrow(off):
        S.dma('sp', pbx[:], prm[0:1, RWKV_PROJ + off:RWKV_PROJ + off + 1024].partition_broadcast(128), W=['pbx'])
    loraW = sb("loraW", [128, 1024], BF16)
    g2b = sb("g2b", [128, 1024], BF16)
    g2c = sb("g2c", [32, 1024], BF16)
    S.dma('pool', loraW[0:64, :], wkv_w2[:, :], W=['loraW'])
    S.dma('pool', loraW[64:128, :], wkv_a2[:, :], W=['loraW'])
    S.dma('pool', g2b[:], wkv_g2[0:128, :], W=['g2b'])
    S.dma('pool', g2c[:], wkv_g2[128:160, :], W=['g2c'])
    wcs = {}
    for k in ('c_tri', 'c_ones', 'c_m5'):
        wcs[k] = sb("s_" + k, list(WCONSTS[k].shape), BF16 if k == 'c_m5' else F32)
        S.dma('pool' if k == 'c_m5' else 'sp', wcs[k][:], wc[k], W=[k])
    smj = sb("smj2", [128, 16], F32)
    S.dma('sp', smj[:], cst['c_smj'], W=['smj2'])
    smi2 = sb("smi2", [128, 16, 128], BF16)
    S.dma('pool', smi2[:], cst['c_smi'], W=['smi2'])
    ut = sb("ut", [128, RWKV_PROJ], F32)
    lt = sb("lt", [128, 288], BF16)
    ltT = sb("ltT", [128, 3, 128], BF16)
    FA = [sb(f"FA{j}", [128, 1024], F32) for j in range(8)]
    ggb = sb("ggb", [128, 1024], BF16)
    BQ = {n: sb("BQ_" + n, [128, 1024], BF16) for n in ('at', 'bt', 'kt', 'rt', 'bh', 'kh', 'v')}
    BQ['XT'], BQ['UT'], BQ['ob'] = BQ['at'], BQ['bt'], BQ['kt']
    TQ = {n: sb("TQ_" + n, [128, 8, 128], BF16) for n in ('at', 'bt', 'kt', 'rt')}
    MS = {n: sb("MS_" + n, [128, 16, 128], BF16) for n in ('A', 'AT', 'M', 'Mbr', 'Mkr', 'P', 'IpAT')}
    gCT = sb("gCT", [128, 8, 16], F32)
    STf = sb("STf", [128, 8, 64], F32)
    STb = sb("STb", [128, 8, 64], BF16)
    st16 = sb("st16", [128, 6, 16], F32)
    S0Tf = sb("S0Tf", [128, 16, 8, 64], F32)
    som = sb("som", [64, 8, 128], F32)
    S0in = som[:].rearrange("p a b -> p (a b)")
    qm = sb("qm", [128, 16, 128], F32)
    bm = [sb(f"bm{j}", [128, 16, 128], BF16) for j in range(2)]
    S.op('dve', lambda e: e.memset(STf[:], 0.0), W=['STf'])
    S.op('dve', lambda e: e.memset(STb[:], 0.0), W=['STb'])
    for s_ in range(16 if 'C2' in ST else 0):
        S.dma('sp', S0in.rearrange("p (h j) -> p h j", h=16), st_wkv[s_].rearrange("h i j -> i h j"), W=['som'])

        def fTs(e, s_=s_):
            ins = None
            for pr in range(8):
                ins = e.transpose(psF[:, 6, pr * 64:(pr + 1) * 64], S0in[0:64, pr * 128:(pr + 1) * 128], identf[0:64, 0:64])
            return ins
        S.op('pe', fTs, R=['som', 'identf'], W=['b6'])
        S.op('act', lambda e, s_=s_: e.activation(out=S0Tf[:, s_, :, :].rearrange("p a b -> p (a b)"), in_=psF[:, 6, :], func=AF.Copy),
             R=['b6'], W=['S0Tf'])

    def V(fn, R, W):
        S.op('dve', fn, R=R, W=W)

    def A(fn, R, W):
        S.op('act', fn, R=R, W=W)

    def bank2(b):
        return psF[:, b:b + 2, :].rearrange("p a c -> p (a c)")

    def h16(ap):
        return ap.rearrange("p (h j) -> p h j", h=16)

    def bc16(col_ap):
        return col_ap.unsqueeze(2).to_broadcast([128, 16, 64])

    NC2 = int(os.environ.get('KC2N', NT))
    for i in range(NC2 if 'C2' in ST else 0):
        smp = (i == NT - 1)
        vv = 1 if smp else 0
        tsl = slice(i * 128, (i + 1) * 128)
        S.dma('sp', ut[:], uD[tsl, :], R=[('uD', i)], W=['ut'])
        r_, kx, vx = ut[:, 0:1024], ut[:, 1024:2048], ut[:, 2048:3072]
        A(lambda e: e.activation(out=lt[:, 0:64], in_=ut[:, 3072:3136], func=AF.Tanh), ['ut'], ['lt0'])
        A(lambda e: e.activation(out=lt[:, 64:128], in_=ut[:, 3136:3200], func=AF.Copy), ['ut'], ['lt1'])
        A(lambda e: e.activation(out=lt[:, 128:288], in_=ut[:, 3200:3360], func=AF.Sigmoid), ['ut'], ['lt2'])

        def fLT(e):
            e.transpose(psT[:, 0:128], lt[:, 0:128], identb[:])
            e.transpose(psT[:, 128:256], lt[:, 128:256], identb[:])
            return e.transpose(psT[0:32, 256:384], lt[:, 256:288], identb[:])
        S.op('pe', fLT, R=['lt0', 'lt1', 'lt2', 'identb'], W=['psT'])
        A(lambda e: e.activation(out=ltT[:, 0:2, :].rearrange("p a t -> p (a t)"), in_=psT[:, 0:256], func=AF.Copy), ['psT'], ['ltTa'])
        A(lambda e: e.activation(out=ltT[0:32, 2, :], in_=psT[0:32, 256:384], func=AF.Copy), ['psT'], ['ltTb'])
        pLW, pLA, pLG = bank2(0), bank2(2), bank2(4)

        def fL(e):
            for hf in range(2):
                cs_ = slice(hf * 512, (hf + 1) * 512)
                e.matmul(pLW[:, cs_], lhsT=ltT[0:64, 0, :], rhs=loraW[0:64, cs_], start=True, stop=True)
                e.matmul(pLA[:, cs_], lhsT=ltT[64:128, 0, :], rhs=loraW[64:128, cs_], start=True, stop=True)
                e.matmul(pLG[:, cs_], lhsT=ltT[:, 1, :], rhs=g2b[:, cs_], start=True, stop=False)
                ins = e.matmul(pLG[:, cs_], lhsT=ltT[0:32, 2, :], rhs=g2c[0:32, cs_], start=False, stop=True)
            return ins
        S.op('pe', fL, R=['ltTa', 'ltTb', 'loraW', 'g2b', 'g2c'], W=['b0', 'b1', 'b2', 'b3', 'b4', 'b5'])
        logw, a_s, kkn, kmod, tmpA, tmpB, egi, etd = FA
        gg = ggb
        eg = kkn
        V(lambda e: e.tensor_tensor(out=tmpA[:], in0=pLW, in1=pb[:, O_W0:O_W0 + 1024], op=ALU.add), ['b0', 'b1', 'pb'], ['tmpA'])
        A(lambda e: e.activation(out=tmpA[:], in_=tmpA[:], func=AF.Sigmoid), ['tmpA'], ['tmpA'])
        V(lambda e: e.tensor_scalar(out=logw[:], in0=tmpA[:], scalar1=-0.6065306597126334, scalar2=None, op0=ALU.mult), ['tmpA'], ['logw'])
        V(lambda e: e.tensor_tensor(out=tmpB[:], in0=pLA, in1=pb[:, O_A0:O_A0 + 1024], op=ALU.add), ['b2', 'b3', 'pb'], ['tmpB'])
        A(lambda e: e.activation(out=a_s[:], in_=tmpB[:], func=AF.Sigmoid), ['tmpB'], ['a_s'])
        A(lambda e: e.activation(out=gg[:], in_=pLG, func=AF.Copy), ['b4', 'b5'], ['gg'])
        pC, pTt = bank2(0), bank2(2)

        def fC(e):
            for hf in range(2):
                cs_ = slice(hf * 512, (hf + 1) * 512)
                e.matmul(pC[:, cs_], lhsT=wcs['c_tri'][:, vv, :], rhs=logw[:, cs_], start=True, stop=True)
                ins = e.matmul(pTt[:, cs_], lhsT=wcs['c_ones'][:, vv, :], rhs=logw[:, cs_], start=True, stop=True)
            return ins
        S.op('pe', fC, R=['logw', 'c_tri', 'c_ones'], W=['b0', 'b1', 'b2', 'b3'])
        ncol = 16 if smp else 1

        def fG(e):
            ins = None
            for pr in range(8):
                ins = e.matmul(psF[:, 6, pr * 16:pr * 16 + ncol], lhsT=logw[:, pr * 128:(pr + 1) * 128],
                               rhs=(smj[:, 0:16] if smp else wcs['c_ones'][:, 0, 0:1]), start=True, stop=True)
            return ins
        S.op('pe', fG, R=['logw', 'smj2', 'c_ones'], W=['b6'])
        A(lambda e: e.activation(out=gCT[:, :, 0:ncol], in_=psF[:, 6, 0:128].rearrange("p (a b) -> p a b", a=8)[:, :, 0:ncol], func=AF.Exp),
          ['b6'], ['gCT'])
        A(lambda e: e.activation(out=eg[:], in_=pC, func=AF.Exp), ['b0', 'b1'], ['kkn'])
        V(lambda e: e.tensor_tensor(out=BQ['rt'][:], in0=r_, in1=eg[:], op=ALU.mult), ['ut', 'kkn'], ['q_rt'])
        V(lambda e: e.tensor_scalar(out=tmpA[:], in0=pC, scalar1=-1.0, scalar2=None, op0=ALU.mult), ['b0', 'b1'], ['tmpA'])
        A(lambda e: e.activation(out=egi[:], in_=tmpA[:], func=AF.Exp), ['tmpA'], ['egi'])
        V(lambda e: e.tensor_tensor(out=tmpB[:], in0=pTt, in1=tmpA[:], op=ALU.add), ['b2', 'b3', 'tmpA'], ['tmpB'])
        A(lambda e: e.activation(out=etd[:], in_=tmpB[:], func=AF.Exp), ['tmpB'], ['etd'])
        V(lambda e: e.tensor_tensor(out=tmpB[:], in0=tmpA[:], in1=logw[:], op=ALU.add), ['tmpA', 'logw'], ['tmpB'])
        A(lambda e: e.activation(out=tmpB[:], in_=tmpB[:], func=AF.Exp, scale=-1.0), ['tmpB'], ['tmpB'])
        V(lambda e: e.tensor_tensor(out=kkn[:], in0=kx, in1=pb[:, O_KK:O_KK + 1024], op=ALU.mult), ['ut', 'pb'], ['kkn'])
        A(lambda e: e.activation(out=tmpA[:], in_=kkn[:], func=AF.Square), ['kkn'], ['tmpA'])
        V(lambda e: e.tensor_reduce(out=st16[:, 0, :], in_=h16(tmpA[:]), axis=AX.X, op=ALU.add), ['tmpA'], ['st0'])
        A(lambda e: e.sqrt(out=st16[:, 0, :], in_=st16[:, 0, :]), ['st0'], ['st0'])
        V(lambda e: e.tensor_scalar(out=st16[:, 0, :], in0=st16[:, 0, :], scalar1=1e-12, scalar2=None, op0=ALU.max), ['st0'], ['st0'])
        V(lambda e: e.reciprocal(out=st16[:, 0, :], in_=st16[:, 0, :]), ['st0'], ['st0'])
        V(lambda e: e.tensor_tensor(out=h16(kkn[:]), in0=h16(kkn[:]), in1=bc16(st16[:, 0, :]), op=ALU.mult), ['kkn', 'st0'], ['kkn'])
        V(lambda e: e.scalar_tensor_tensor(out=kmod[:], in0=a_s[:], scalar=-1.0, in1=pb[:, O_KA:O_KA + 1024], op0=ALU.add, op1=ALU.mult),
          ['a_s', 'pb'], ['kmod'])
        V(lambda e: e.scalar_tensor_tensor(out=kmod[:], in0=kmod[:], scalar=1.0, in1=kx, op0=ALU.add, op1=ALU.mult), ['kmod', 'ut'], ['kmod'])
        V(lambda e: e.tensor_tensor(out=tmpA[:], in0=r_, in1=kmod[:], op=ALU.mult), ['ut', 'kmod'], ['tmpA'])
        ldrow(O_RK)
        V(lambda e: e.tensor_tensor(out=tmpA[:], in0=tmpA[:], in1=pbx[:], op=ALU.mult), ['tmpA', 'pbx'], ['tmpA'])
        V(lambda e: e.tensor_reduce(out=st16[:, 1, :], in_=h16(tmpA[:]), axis=AX.X, op=ALU.add), ['tmpA'], ['st1'])
        V(lambda e: e.scalar_tensor_tensor(out=BQ['at'][:], in0=kkn[:], scalar=-1.0, in1=tmpB[:], op0=ALU.mult, op1=ALU.mult),
          ['kkn', 'tmpB'], ['q_at'])
        V(lambda e: e.tensor_tensor(out=tmpA[:], in0=kkn[:], in1=a_s[:], op=ALU.mult), ['kkn', 'a_s'], ['tmpA'])
        V(lambda e: e.tensor_tensor(out=BQ['bt'][:], in0=tmpA[:], in1=egi[:], op=ALU.mult), ['tmpA', 'egi'], ['q_bt'])
        V(lambda e: e.tensor_tensor(out=BQ['bh'][:], in0=tmpA[:], in1=etd[:], op=ALU.mult), ['tmpA', 'etd'], ['q_bh'])
        V(lambda e: e.tensor_tensor(out=BQ['kt'][:], in0=kmod[:], in1=egi[:], op=ALU.mult), ['kmod', 'egi'], ['q_kt'])
        V(lambda e: e.tensor_tensor(out=BQ['kh'][:], in0=kmod[:], in1=etd[:], op=ALU.mult), ['kmod', 'etd'], ['q_kh'])
        A(lambda e: e.activation(out=BQ['v'][:], in_=vx, func=AF.Copy), ['ut'], ['q_v'])
        for n in ('at', 'bt', 'kt', 'rt'):
            def fTq(e, n=n):
                ins = None
                for pr in range(8):
                    ins = e.transpose(psT[:, pr * 128:(pr + 1) * 128], BQ[n][:, pr * 128:(pr + 1) * 128], identb[:])
                return ins
            S.op('pe', fTq, R=['q_' + n, 'identb'], W=['psT'])
            A(lambda e, n=n: e.activation(out=TQ[n][:].rearrange("p a t -> p (a t)"), in_=psT[:, :], func=AF.Copy), ['psT'], ['T_' + n])
        m5 = wcs['c_m5']
        for hd in range(16):
            pr, off = hd // 2, 64 * (hd % 2)
            sl = slice(off, off + 64)
            pb_ = 4 + (hd % 2)
            p3 = psF[:, pb_, 0:384]
            p2 = psF[:, pb_, 384:512]

            def f5(e):
                e.matmul(p3[:, 0:128], lhsT=TQ['bt'][sl, pr, :], rhs=TQ['at'][sl, pr, :], start=True, stop=True)
                e.matmul(p3[:, 128:256], lhsT=TQ['at'][sl, pr, :], rhs=TQ['bt'][sl, pr, :], start=True, stop=True)
                return e.matmul(p3[:, 256:384], lhsT=TQ['kt'][sl, pr, :], rhs=TQ['at'][sl, pr, :], start=True, stop=True)
            S.op('pe', f5, R=['T_at', 'T_bt', 'T_kt'], W=[f'b{pb_}'])
            V(lambda e: e.tensor_tensor(out=MS['A'][:, hd, :], in0=p3[:, 0:128], in1=m5[:, vv, 0, :], op=ALU.mult), [f'b{pb_}', 'c_m5'], [('A', hd)])
            V(lambda e: e.tensor_tensor(out=MS['AT'][:, hd, :], in0=p3[:, 128:256], in1=m5[:, vv, 1, :], op=ALU.mult), [f'b{pb_}', 'c_m5'], [('AT', hd)])
            V(lambda e: e.tensor_tensor(out=MS['M'][:, hd, :], in0=p3[:, 256:384], in1=m5[:, vv, 2, :], op=ALU.mult), [f'b{pb_}', 'c_m5'], [('M', hd)])
            pq = psF[:, 6, (hd % 2) * 256:(hd % 2) * 256 + 256]

            def f2(e):
                e.matmul(pq[:, 0:128], lhsT=TQ['bt'][sl, pr, :], rhs=TQ['rt'][sl, pr, :], start=True, stop=True)
                return e.matmul(pq[:, 128:256], lhsT=TQ['kt'][sl, pr, :], rhs=TQ['rt'][sl, pr, :], start=True, stop=True)
            S.op('pe', f2, R=['T_bt', 'T_kt', 'T_rt'], W=['b6'])
            V(lambda e: e.tensor_tensor(out=MS['Mbr'][:, hd, :], in0=pq[:, 0:128], in1=m5[:, vv, 3, :], op=ALU.mult), ['b6', 'c_m5'], [('Mbr', hd)])
            V(lambda e: e.tensor_tensor(out=MS['Mkr'][:, hd, :], in0=pq[:, 128:256], in1=m5[:, vv, 4, :], op=ALU.mult), ['b6', 'c_m5'], [('Mkr', hd)])
        identb4 = identb[:].unsqueeze(1).to_broadcast([128, 4, 128])
        GS = [slice(4 * g, 4 * g + 4) for g in range(4)]
        GK = [[('A', hd) for hd in range(4 * g, 4 * g + 4)] for g in range(4)]
        GKT = [[('AT', hd) for hd in range(4 * g, 4 * g + 4)] for g in range(4)]
        for g in range(4):
            V(lambda e: e.tensor_tensor(out=MS['P'][:, GS[g], :], in0=MS['A'][:, GS[g], :], in1=identb4, op=ALU.add), GK[g] + ['identb'], [('P', g)])
        nlev = 2 if smp else 7
        for lev in range(1, nlev):
            last = (lev == nlev - 1)
            for g in range(4):
                gs, gk, gkT = GS[g], GK[g], GKT[g]
                bks = [f'b{2 * (g % 2)}', f'b{2 * (g % 2) + 1}']
                pa = bank2(2 * (g % 2)).rearrange("p (h a t) -> p h a t", h=4, a=2)

                def fa(e):
                    ins = None
                    for q in range(4):
                        hd = 4 * g + q
                        if not last:
                            e.matmul(pa[:, q, 0, :], lhsT=MS['AT'][:, hd, :], rhs=MS['A'][:, hd, :], start=True, stop=True)
                        ins = e.matmul(pa[:, q, 1, :], lhsT=MS['A'][:, hd, :], rhs=MS['AT'][:, hd, :], start=True, stop=True)
                    return ins
                S.op('pe', fa, R=gk + gkT, W=bks)
                V(lambda e: e.tensor_tensor(out=MS['IpAT'][:, gs, :], in0=pa[:, :, 1, :], in1=identb4, op=ALU.add), bks + ['identb'], [('IpAT', g)])
                if not last:
                    A(lambda e: e.activation(out=MS['A'][:, gs, :], in_=pa[:, :, 0, :], func=AF.Copy), bks, gk)
                    A(lambda e: e.activation(out=MS['AT'][:, gs, :], in_=pa[:, :, 1, :], func=AF.Copy), bks, gkT)
            for g in range(4):
                gs = GS[g]
                pbk = psF[:, 4 + (g % 2), :].rearrange("p (h t) -> p h t", h=4)

                def fb(e):
                    ins = None
                    for q in range(4):
                        hd = 4 * g + q
                        ins = e.matmul(pbk[:, q, :], lhsT=MS['IpAT'][:, hd, :], rhs=MS['P'][:, hd, :], start=True, stop=True)
                    return ins
                S.op('pe', fb, R=[('IpAT', g), ('P', g)], W=[f'b{4 + (g % 2)}'])
                A(lambda e: e.activation(out=MS['P'][:, gs, :], in_=pbk, func=AF.Copy), [f'b{4 + (g % 2)}'], [('P', g)])
        pX, pU, pY = bank2(0), bank2(2), bank2(4)
        Pk = [('P', g) for g in range(4)]
        Mk = [('M', hd) for hd in range(16)]
        if not smp:
            def fX(e):
                ins = None
                for hd in range(16):
                    pr, off = hd // 2, 64 * (hd % 2)
                    sl = slice(off, off + 64)
                    cs_ = slice(hd * 64, hd * 64 + 64)
                    e.matmul(pX[:, cs_], lhsT=TQ['at'][sl, pr, :], rhs=STb[sl, pr, :], start=True, stop=False)
                    ins = e.matmul(pX[:, cs_], lhsT=MS['M'][:, hd, :], rhs=BQ['v'][:, cs_], start=False, stop=True)
                return ins
            S.op('pe', fX, R=['T_at', 'STb', 'q_v'] + Mk, W=['b0', 'b1'])
        else:
            def fX(e):
                ins = None
                for hd in range(16):
                    pr, off = hd // 2, 64 * (hd % 2)
                    sl = slice(off, off + 64)
                    cs_ = slice(hd * 64, hd * 64 + 64)
                    S.op('dve', lambda e2: e2.tensor_tensor(out=qm[sl, :, :], in0=TQ['at'][sl, pr, :].unsqueeze(1).to_broadcast([64, 16, 128]),
                                                            in1=smi2[sl, :, :], op=ALU.mult), R=['T_at', 'smi2'], W=['qm'])

                    def fx1(e3):
                        for s_ in range(16):
                            e3.matmul(pX[:, cs_], lhsT=qm[sl, s_, :], rhs=S0Tf[sl, s_, pr, :], start=(s_ == 0), stop=False)
                        return e3.matmul(pX[:, cs_], lhsT=MS['M'][:, hd, :], rhs=BQ['v'][:, cs_], start=False, stop=True)
                    S.op('pe', fx1, R=['qm', 'S0Tf', 'q_v', ('M', hd)], W=['b0', 'b1'])
            fX(None)
        A(lambda e: e.activation(out=BQ['XT'][:], in_=pX, func=AF.Copy), ['b0', 'b1'], ['q_at'])

        def fU(e):
            ins = None
            for hd in range(16):
                cs_ = slice(hd * 64, hd * 64 + 64)
                ins = e.matmul(pU[:, cs_], lhsT=MS['P'][:, hd, :], rhs=BQ['XT'][:, cs_], start=True, stop=True)
            return ins
        S.op('pe', fU, R=['q_at'] + Pk, W=['b2', 'b3'])
        A(lambda e: e.activation(out=BQ['UT'][:], in_=pU, func=AF.Copy), ['b2', 'b3'], ['q_bt'])
        Mbk = [('Mbr', hd) for hd in range(16)] + [('Mkr', hd) for hd in range(16)]
        if not smp:
            def fY(e):
                ins = None
                for hd in range(16):
                    pr, off = hd // 2, 64 * (hd % 2)
                    sl = slice(off, off + 64)
                    cs_ = slice(hd * 64, hd * 64 + 64)
                    e.matmul(pY[:, cs_], lhsT=TQ['rt'][sl, pr, :], rhs=STb[sl, pr, :], start=True, stop=False)
                    e.matmul(pY[:, cs_], lhsT=MS['Mbr'][:, hd, :], rhs=BQ['UT'][:, cs_], start=False, stop=False)
                    ins = e.matmul(pY[:, cs_], lhsT=MS['Mkr'][:, hd, :], rhs=BQ['v'][:, cs_], start=False, stop=True)
                return ins
            S.op('pe', fY, R=['T_rt', 'STb', 'q_bt', 'q_v'] + Mbk, W=['b4', 'b5'])
        else:
            for hd in range(16):
                pr, off = hd // 2, 64 * (hd % 2)
                sl = slice(off, off + 64)
                cs_ = slice(hd * 64, hd * 64 + 64)
                V(lambda e: e.tensor_tensor(out=qm[sl, :, :], in0=TQ['rt'][sl, pr, :].unsqueeze(1).to_broadcast([64, 16, 128]),
                                            in1=smi2[sl, :, :], op=ALU.mult), ['T_rt', 'smi2'], ['qm'])

                def fy1(e):
                    for s_ in range(16):
                        e.matmul(pY[:, cs_], lhsT=qm[sl, s_, :], rhs=S0Tf[sl, s_, pr, :], start=(s_ == 0), stop=False)
                    e.matmul(pY[:, cs_], lhsT=MS['Mbr'][:, hd, :], rhs=BQ['UT'][:, cs_], start=False, stop=False)
                    return e.matmul(pY[:, cs_], lhsT=MS['Mkr'][:, hd, :], rhs=BQ['v'][:, cs_], start=False, stop=True)
                S.op('pe', fy1, R=['qm', 'S0Tf', 'q_bt', 'q_v', ('Mbr', hd), ('Mkr', hd)], W=['b4', 'b5'])
        if not smp:
            pS = bank2(0).rearrange("p (h c) -> p h c", h=16)

            def fS(e):
                ins = None
                for hd in range(16):
                    pr = hd // 2
                    cs_ = slice(hd * 64, hd * 64 + 64)
                    ps_ = slice(pr * 128, (pr + 1) * 128)
                    e.matmul(pS[:, hd, :], lhsT=BQ['bh'][:, ps_], rhs=BQ['UT'][:, cs_], start=True, stop=False)
                    ins = e.matmul(pS[:, hd, :], lhsT=BQ['kh'][:, ps_], rhs=BQ['v'][:, cs_], start=False, stop=True)
                return ins
            S.op('pe', fS, R=['q_bh', 'q_kh', 'q_bt', 'q_v', 'q_at'], W=['b0', 'b1'])
            for h2 in range(2):
                sl = slice(64 * h2, 64 * h2 + 64)
                V(lambda e: e.tensor_tensor(out=STf[sl, :, :], in0=STf[sl, :, :], in1=gCT[sl, :, 0:1].to_broadcast([64, 8, 64]), op=ALU.mult),
                  ['STf', 'gCT'], ['STf'])
                V(lambda e: e.tensor_tensor(out=STf[sl, :, :], in0=STf[sl, :, :],
                                            in1=pS.rearrange("p (a b) c -> p a b c", b=2)[sl, :, h2, :], op=ALU.add), ['STf', 'b0', 'b1'], ['STf'])
            A(lambda e: e.activation(out=STb[:].rearrange("p a b -> p (a b)"), in_=STf[:].rearrange("p a b -> p (a b)"), func=AF.Copy), ['STf'], ['STb'])
            if i == NT - 2:
                def fTo(e):
                    ins = None
                    for pr in range(8):
                        ins = e.transpose(bank2(2)[0:64, pr * 128:(pr + 1) * 128], STf[:, pr, :], identf[:])
                    return ins
                S.op('pe', fTo, R=['STf', 'identf'], W=['b2', 'b3'])
                A(lambda e: e.activation(out=som[:].rearrange("p a b -> p (a b)"), in_=bank2(2)[0:64, :], func=AF.Copy), ['b2', 'b3'], ['som'])
                S.dma('sp', wkv_p.rearrange("(a b) i j -> i a b j", b=2), som[:].rearrange("p a (b j) -> p a b j", b=2), R=['som'], W=['wkv_p'])
        else:
            for pr in range(8):
                ps_ = slice(pr * 128, (pr + 1) * 128)
                V(lambda e: e.tensor_tensor(out=bm[0][:], in0=BQ['bh'][:, ps_].unsqueeze(1).to_broadcast([128, 16, 128]),
                                            in1=smj[:].unsqueeze(2).to_broadcast([128, 16, 128]), op=ALU.mult), ['q_bh', 'smj2'], ['bm0'])
                V(lambda e: e.tensor_tensor(out=bm[1][:], in0=BQ['kh'][:, ps_].unsqueeze(1).to_broadcast([128, 16, 128]),
                                            in1=smj[:].unsqueeze(2).to_broadcast([128, 16, 128]), op=ALU.mult), ['q_kh', 'smj2'], ['bm1'])
                pS4 = psF[:, 0:4, :].rearrange("p a c -> p (a c)").rearrange("p (s b c) -> p s b c", s=16, b=2)

                def fSs(e):
                    ins = None
                    for s_ in range(16):
                        for h2 in range(2):
                            hd = 2 * pr + h2
                            cs_ = slice(hd * 64, hd * 64 + 64)
                            e.matmul(pS4[:, s_, h2, :], lhsT=bm[0][:, s_, :], rhs=BQ['UT'][:, cs_], start=True, stop=False)
                            ins = e.matmul(pS4[:, s_, h2, :], lhsT=bm[1][:, s_, :], rhs=BQ['v'][:, cs_], start=False, stop=True)
                    return ins
                S.op('pe', fSs, R=['bm0', 'bm1', 'q_bt', 'q_v', 'q_at'], W=['b0', 'b1', 'b2', 'b3'])
                for h2 in range(2):
                    sl = slice(64 * h2, 64 * h2 + 64)
                    V(lambda e: e.tensor_tensor(out=S0Tf[sl, :, pr, :], in0=S0Tf[sl, :, pr, :],
                                                in1=gCT[sl, pr, :].unsqueeze(2).to_broadcast([64, 16, 64]), op=ALU.mult),
                      ['S0Tf', 'gCT'], ['S0Tf'])
                    V(lambda e: e.tensor_tensor(out=S0Tf[sl, :, pr, :], in0=S0Tf[sl, :, pr, :], in1=pS4[sl, :, h2, :], op=ALU.add),
                      ['S0Tf', 'b0', 'b1', 'b2', 'b3'], ['S0Tf'])
            for s_ in range(16):
                def fTo(e):
                    ins = None
                    for pr in range(8):
                        ins = e.transpose(bank2(0)[0:64, pr * 128:(pr + 1) * 128], S0Tf[:, s_, pr, :], identf[:])
                    return ins
                S.op('pe', fTo, R=['S0Tf', 'identf'], W=['b0', 'b1'])
                A(lambda e: e.activation(out=som[:].rearrange("p a b -> p (a b)"), in_=bank2(0)[0:64, :], func=AF.Copy), ['b0', 'b1'], ['som'])
                S.dma('sp', wkv_s[s_].rearrange("(a b) i j -> i a b j", b=2), som[:].rearrange("p a (b j) -> p a b j", b=2), R=['som'], W=['wkv_s'])
        ysb, ysq = tmpA, tmpB
        A(lambda e: e.activation(out=ysb[:], in_=pY, func=AF.Copy), ['b4', 'b5'], ['tmpA'])
        A(lambda e: e.activation(out=ysq[:], in_=pY, func=AF.Square), ['b4', 'b5'], ['tmpB'])
        V(lambda e: e.tensor_reduce(out=st16[:, 2, :], in_=h16(ysb[:]), axis=AX.X, op=ALU.add), ['tmpA'], ['st2'])
        V(lambda e: e.tensor_reduce(out=st16[:, 3, :], in_=h16(ysq[:]), axis=AX.X, op=ALU.add), ['tmpB'], ['st3'])
        V(lambda e: e.tensor_scalar(out=st16[:, 2, :], in0=st16[:, 2, :], scalar1=1.0 / 64, scalar2=None, op0=ALU.mult), ['st2'], ['st2'])
        V(lambda e: e.tensor_tensor(out=st16[:, 4, :], in0=st16[:, 2, :], in1=st16[:, 2, :], op=ALU.mult), ['st2'], ['st4'])
        V(lambda e: e.scalar_tensor_tensor(out=st16[:, 3, :], in0=st16[:, 3, :], scalar=1.0 / 64, in1=st16[:, 4, :], op0=ALU.mult, op1=ALU.subtract),
          ['st3', 'st4'], ['st3'])
        V(lambda e: e.tensor_scalar(out=st16[:, 3, :], in0=st16[:, 3, :], scalar1=64e-5, scalar2=None, op0=ALU.add), ['st3'], ['st3'])
        A(lambda e: e.sqrt(out=st16[:, 3, :], in_=st16[:, 3, :]), ['st3'], ['st3'])
        V(lambda e: e.reciprocal(out=st16[:, 3, :], in_=st16[:, 3, :]), ['st3'], ['st3'])
        V(lambda e: e.tensor_tensor(out=h16(ysb[:]), in0=h16(ysb[:]), in1=bc16(st16[:, 2, :]), op=ALU.subtract), ['tmpA', 'st2'], ['tmpA'])
        V(lambda e: e.tensor_tensor(out=h16(ysb[:]), in0=h16(ysb[:]), in1=bc16(st16[:, 3, :]), op=ALU.mult), ['tmpA', 'st3'], ['tmpA'])
        ldrow(O_LW)
        V(lambda e: e.tensor_tensor(out=ysb[:], in0=ysb[:], in1=pbx[:], op=ALU.mult), ['tmpA', 'pbx'], ['tmpA'])
        ldrow(O_LB)
        V(lambda e: e.tensor_tensor(out=ysb[:], in0=ysb[:], in1=pbx[:], op=ALU.add), ['tmpA', 'pbx'], ['tmpA'])
        V(lambda e: e.tensor_tensor(out=h16(ysq[:]), in0=h16(vx), in1=bc16(st16[:, 1, :]), op=ALU.mult), ['ut', 'st1', 'tmpB'], ['tmpB'])
        V(lambda e: e.tensor_tensor(out=ysb[:], in0=ysb[:], in1=ysq[:], op=ALU.add), ['tmpA', 'tmpB'], ['tmpA'])
        V(lambda e: e.tensor_tensor(out=BQ['ob'][:], in0=ysb[:], in1=gg[:], op=ALU.mult), ['tmpA', 'gg'], ['q_kt'])
        S.dma('sp', obD[tsl, :], BQ['ob'][:], R=['q_kt'], W=[('obD', i)])

    S.barrier()
    stack[0].close()
    stack[0] = ExitStack()
    w_oa_v = w_oa.rearrange("(k p) n -> p k n", p=128)
    w_ob_v = w_ob.rearrange("(k p) n -> p k n", p=128)
    w_out_v = w_out.rearrange("(k p) n -> p k n", p=128)
    junk = sb("junkD", [128, D], BF16)
    ss = sb("ssD", [128, 4], F32)
    xg = [sb(f"xg{j}", [128, D], F32) for j in range(4)]
    hbD = [sb(f"hbD{j}", [128, D], BF16) for j in range(2)]
    ost = [sb(f"ost{j}", [128, D], BF16) for j in range(2)]
    hTg = sb("hTg", [128, KC, 512], BF16)
    oTg = sb("oTg", [128, KC, 512], BF16)
    obTg = sb("obTg", [128, 8, 512], BF16)
    mtile = [sb(f"mtile{j}", [128, D], BF16) for j in range(4)]
    WA = [sb(f"WA{j}", [128, KC, 512], BF16) for j in range(4)]
    WB = [sb(f"WB{j}", [128, 8, 512], BF16) for j in range(2)]
    sgA = [sb(f"sgA{j}", [128, 512], F32) for j in range(2)]
    sgB = [sb(f"sgB{j}", [128, 512], F32) for j in range(2)]
    ridx_sb = sb("ridx_sb", [128, NTL], I32)
    S.dma('sp', ridx_sb[:], ridx[:, :], W=['ridx_sb'])
    wa_i = [0]
    wb_i = [0]

    def nextWA():
        j = wa_i[0] % 4
        wa_i[0] += 1
        return j

    def mm16(ps, lhsT_fn, W_, nk=KC):
        def f(e):
            ins = None
            for k in range(nk):
                ins = e.matmul(ps, lhsT=lhsT_fn(k), rhs=W_[:, k, :], start=(k == 0), stop=(k == nk - 1))
            return ins
        return f

    groups = [(0, 4), (4, 4), (8, 1)]
    for (i0, n) in (groups if 'D' in ST else []):
        for j in range(n):
            i = i0 + j
            S.dma('sp', xg[j][:], x_h[i * 128:(i + 1) * 128, :], W=[('xg', j)])
            rmsnorm_tile(xg[j][:], hbD[j % 2][:], gbc[:], ('xg', j), ('hbD', j % 2), j % 2)
            transpose_to(hTg, hbD[j % 2], ('hbD', j % 2), 'hTg', j)
            S.dma('pool', None, None, R=['ridx_sb'], W=[('ost', 0)],
                  fn=lambda e: e.indirect_dma_start(out=ost[0][:, :], out_offset=None, in_=oD[:, :],
                                                    in_offset=bass.IndirectOffsetOnAxis(ap=ridx_sb[:, i:i + 1], axis=0)))
            transpose_to(oTg, ost[0], ('ost', 0), 'oTg', j)
            S.dma('pool', None, None, R=['ridx_sb'], W=[('ost', 1)],
                  fn=lambda e: e.indirect_dma_start(out=ost[1][:, 0:1024], out_offset=None, in_=obD[:, :],
                                                    in_offset=bass.IndirectOffsetOnAxis(ap=ridx_sb[:, i:i + 1], axis=0)))
            transpose_to(obTg, ost[1], ('ost', 1), 'obTg', j, nk=8)
        for blk in range(4):
            cs_ = slice(blk * 512, (blk + 1) * 512)
            a1, a2, a3 = nextWA(), nextWA(), nextWA()
            b1 = wb_i[0] % 2
            wb_i[0] += 1
            S.dma('pool', WA[a1][:], w_oa_v[:, :, cs_], W=[('WA', a1)])
            S.dma('pool', WB[b1][:], w_ob_v[:, :, cs_], W=[('WB', b1)])
            S.dma('pool', WA[a2][:], w_in_v[:, :, OFF_G + blk * 512:OFF_G + (blk + 1) * 512], W=[('WA', a2)])
            S.dma('pool', WA[a3][:], w_in_v[:, :, OFF_G + D + blk * 512:OFF_G + D + (blk + 1) * 512], W=[('WA', a3)])
            for j in range(n):
                q = j % 2
                tj = slice(j * 128, (j + 1) * 128)
                pGA, pGB, pYA, pYB = psF[:, q, :], psF[:, 2 + q, :], psF[:, 4 + q, :], psF[:, 6, :]
                S.op('pe', mm16(pGA, lambda k: hTg[:, k, tj], WA[a2]), R=[('hTg', j), ('WA', a2)], W=[f'b{q}'])
                S.op('pe', mm16(pGB, lambda k: hTg[:, k, tj], WA[a3]), R=[('hTg', j), ('WA', a3)], W=[f'b{2 + q}'])
                S.op('pe', mm16(pYA, lambda k: oTg[:, k, tj], WA[a1]), R=[('oTg', j), ('WA', a1)], W=[f'b{4 + q}'])
                S.op('pe', mm16(pYB, lambda k: obTg[:, k, tj], WB[b1], nk=8), R=[('obTg', j), ('WB', b1)], W=['b6'])
                S.op('act', lambda e: e.activation(out=sgA[q][:], in_=pGA, func=AF.Sigmoid), R=[f'b{q}'], W=[('sgA', q)])
                S.op('act', lambda e: e.activation(out=sgB[q][:], in_=pGB, func=AF.Sigmoid), R=[f'b{2 + q}'], W=[('sgB', q)])
                S.op('dve', lambda e: e.tensor_tensor(out=sgA[q][:], in0=sgA[q][:], in1=pYA, op=ALU.mult), R=[('sgA', q), f'b{4 + q}'], W=[('sgA', q)])
                S.op('dve', lambda e: e.tensor_tensor(out=sgB[q][:], in0=sgB[q][:], in1=pYB, op=ALU.mult), R=[('sgB', q), 'b6'], W=[('sgB', q)])
                S.op('dve', lambda e: e.tensor_tensor(out=mtile[j][:, cs_], in0=sgA[q][:], in1=sgB[q][:], op=ALU.add),
                     R=[('sgA', q), ('sgB', q)], W=[('mtile', j)])
        for j in range(n):
            transpose_to(oTg, mtile[j], ('mtile', j), 'oTg', j)
        for blk in range(4):
            cs_ = slice(blk * 512, (blk + 1) * 512)
            a1 = nextWA()
            S.dma('pool', WA[a1][:], w_out_v[:, :, cs_], W=[('WA', a1)])
            for j in range(n):
                q = j % 2
                tj = slice(j * 128, (j + 1) * 128)
                S.op('pe', mm16(psF[:, q, :], lambda k: oTg[:, k, tj], WA[a1]), R=[('oTg', j), ('WA', a1)], W=[f'b{q}'])
                S.op('dve', lambda e: e.tensor_tensor(out=xg[j][:, cs_], in0=xg[j][:, cs_], in1=psF[:, q, :], op=ALU.add),
                     R=[('xg', j), f'b{q}'], W=[('xg', j)])
        for j in range(n):
            i = i0 + j
            S.dma('sp', x1D[i * 128:(i + 1) * 128, :], xg[j][:], R=[('xg', j)], W=[('x1D', i)])

    S.barrier()
    stack[0].close()
    stack[0] = None
    slots_i = sb("slots_i", [128, NTL, 2], I32)
    wts = sb("wts", [128, NTL, 2], F32)
    stack[0] = ExitStack()
    junk = sb("junkE", [128, D], BF16)
    ss = sb("ssE", [128, 4], F32)
    S.dma('sp', gbc[:], g_ffn[0:1, :].partition_broadcast(128), W=['gbc'])
    wr_sb = sb("wr_sb", [128, KC, 36], F32)
    S.dma('sp', wr_sb[:], wr.rearrange("(k p) n -> p k n", p=128), W=['wr_sb'])
    rb_bc = sb("rb_bc", [128, 36], F32)
    S.dma('sp', rb_bc[:], rbias[0:1, :].partition_broadcast(128), W=['rb_bc'])
    rc = {}
    for k in ('c_slt', 'c_ecap', 'c_valid', 'c_trash'):
        rc[k] = sb("s_" + k, list(RCONSTS[k].shape), F32)
        S.dma('sp', rc[k][:], rcd[k], W=[k])
    ones_f = sb("ones_f", [128, 128], F32)
    S.op('dve', lambda e: e.memset(ones_f[:], 1.0), W=['ones_f'])
    base = sb("base", [128, 32], F32)
    S.op('dve', lambda e: e.memset(base[:], 0.0), W=['base'])
    xr = [sb(f"xr{j}", [128, D], F32) for j in range(2)]
    h2f = sb("h2f", [128, D], F32)
    h2b = [sb(f"h2b{j}", [128, D], BF16) for j in range(2)]
    h2T = sb("h2T", [128, KC, 128], F32)
    lg = sb("lg", [128, 36], F32)
    sm = sb("sm", [128, 16], F32)
    gm = sb("gm", [128, 4], F32)
    elm = sb("elm", [128, 32], F32)
    elm2 = sb("elm2", [128, 32], F32)
    oh0 = sb("oh0", [128, 32], F32)
    oh1 = sb("oh1", [128, 32], F32)
    ohs = sb("ohs", [128, 32], F32)
    cb = sb("cb", [128, 32], F32)
    cb2 = sb("cb2", [128, 32], F32)
    t32 = sb("t32", [128, 32], F32)

    def V(fn, R, W):
        S.op('dve', fn, R=R, W=W)

    def A(fn, R, W):
        S.op('act', fn, R=R, W=W)

    S.op('dve', lambda e: e.memset(h2b[0][:], 0.0), W=[('h2b', 0)])
    XeD_v = XeD.rearrange("(n p) d -> p n d", p=128)
    for z0 in range(0, NE * CAP // 128 + 1, 11):
        z1 = min(z0 + 11, NE * CAP // 128 + 1)
        S.dma('sp', XeD_v[:, z0:z1, :], h2b[0][:].unsqueeze(1).to_broadcast([128, z1 - z0, D]), R=[('h2b', 0)], W=['XeD'])
    for i in range(NTL if 'E0' in ST else 0):
        q = i % 2
        S.dma('sp', xr[q][:], x1D[i * 128:(i + 1) * 128, :], R=[('x1D', i)], W=[('xr', q)])
        rmsnorm_tile(xr[q][:], h2f[:], gbc[:], ('xr', q), 'h2f', q)
        A(lambda e: e.activation(out=h2b[q][:], in_=h2f[:], func=AF.Copy), ['h2f'], [('h2b', q)])
        for half in range(4):
            def fT4(e, half=half):
                ins = None
                for k in range(4):
                    kk_ = half * 4 + k
                    ins = e.transpose(psF[:, half % 2, k * 128:(k + 1) * 128], h2f[:, kk_ * 128:(kk_ + 1) * 128], identf[:])
                return ins
            S.op('pe', fT4, R=['h2f', 'identf'], W=[f'b{half % 2}'])
            A(lambda e, half=half: e.activation(out=h2T[:, half * 4:half * 4 + 4, :].rearrange("p a t -> p (a t)"), in_=psF[:, half % 2, :], func=AF.Copy),
              [f'b{half % 2}'], ['h2T'])
        pR = psF[:, 2, 0:36]

        def fR(e):
            ins = None
            for k in range(KC):
                ins = e.matmul(pR, lhsT=h2T[:, k, :], rhs=wr_sb[:, k, :], start=(k == 0), stop=(k == KC - 1))
            return ins
        S.op('pe', fR, R=['h2T', 'wr_sb'], W=['b2'])
        V(lambda e: e.tensor_tensor(out=lg[:], in0=pR, in1=rb_bc[:], op=ALU.add), ['b2', 'rb_bc'], ['lg'])
        V(lambda e: e.tensor_reduce(out=sm[:, 0:1], in_=lg[:, 0:4], axis=AX.X, op=ALU.max), ['lg'], ['sm0'])
        V(lambda e: e.tensor_scalar(out=gm[:], in0=lg[:, 0:4], scalar1=sm[:, 0:1], scalar2=None, op0=ALU.subtract), ['lg', 'sm0'], ['gm'])
        A(lambda e: e.activation(out=t32[:, 0:4], in_=gm[:], func=AF.Exp), ['gm'], ['t32'])
        V(lambda e: e.tensor_reduce(out=sm[:, 1:2], in_=t32[:, 0:4], axis=AX.X, op=ALU.add), ['t32'], ['sm1'])
        V(lambda e: e.reciprocal(out=sm[:, 1:2], in_=sm[:, 1:2]), ['sm1'], ['sm1'])
        V(lambda e: e.tensor_scalar(out=gm[:], in0=gm[:], scalar1=0.0, scalar2=None, op0=ALU.is_equal), ['gm'], ['gm'])
        V(lambda e: e.tensor_scalar(out=gm[:], in0=gm[:], scalar1=-1.0, scalar2=1e30, op0=ALU.add, op1=ALU.mult), ['gm'], ['gm'])
        V(lambda e: e.tensor_tensor(out=elm[:].rearrange("p (g x) -> p g x", g=4), in0=lg[:, 4:36].rearrange("p (g x) -> p g x", g=4),
                                    in1=gm[:].unsqueeze(2).to_broadcast([128, 4, 8]), op=ALU.add), ['lg', 'gm'], ['elm'])
        V(lambda e: e.tensor_reduce(out=sm[:, 2:3], in_=elm[:], axis=AX.X, op=ALU.max), ['elm'], ['sm2'])
        V(lambda e: e.tensor_scalar(out=oh0[:], in0=elm[:], scalar1=sm[:, 2:3], scalar2=None, op0=ALU.is_equal), ['elm', 'sm2'], ['oh0'])
        V(lambda e: e.scalar_tensor_tensor(out=elm2[:], in0=oh0[:], scalar=-1e30, in1=elm[:], op0=ALU.mult, op1=ALU.add), ['oh0', 'elm'], ['elm2'])
        V(lambda e: e.tensor_reduce(out=sm[:, 3:4], in_=elm2[:], axis=AX.X, op=ALU.max), ['elm2'], ['sm3'])
        V(lambda e: e.tensor_scalar(out=oh1[:], in0=elm2[:], scalar1=sm[:, 3:4], scalar2=None, op0=ALU.is_equal), ['elm2', 'sm3'], ['oh1'])
        V(lambda e: e.tensor_tensor(out=sm[:, 4:5], in0=sm[:, 3:4], in1=sm[:, 2:3], op=ALU.subtract), ['sm2', 'sm3'], ['sm4'])
        A(lambda e: e.activation(out=sm[:, 4:5], in_=sm[:, 4:5], func=AF.Exp), ['sm4'], ['sm4'])
        V(lambda e: e.tensor_scalar(out=sm[:, 5:6], in0=sm[:, 4:5], scalar1=1.0, scalar2=None, op0=ALU.add), ['sm4'], ['sm5'])
        V(lambda e: e.reciprocal(out=sm[:, 5:6], in_=sm[:, 5:6]), ['sm5'], ['sm5'])
        V(lambda e: e.tensor_tensor(out=wts[:, i, 0:1], in0=sm[:, 5:6], in1=sm[:, 1:2], op=ALU.mult), ['sm5', 'sm1'], [('wts', i)])
        V(lambda e: e.tensor_tensor(out=wts[:, i, 1:2], in0=wts[:, i, 0:1], in1=sm[:, 4:5], op=ALU.mult), [('wts', i), 'sm4'], [('wts', i)])
        vcol = rc['c_valid'][:, 1:2] if i == NTL - 1 else rc['c_valid'][:, 0:1]
        V(lambda e: e.tensor_tensor(out=ohs[:], in0=oh0[:], in1=oh1[:], op=ALU.add), ['oh0', 'oh1'], ['ohs'])
        V(lambda e: e.tensor_scalar(out=ohs[:], in0=ohs[:], scalar1=vcol, scalar2=None, op0=ALU.mult), ['ohs', 'c_valid'], ['ohs'])
        pP, pTot = psF[:, 3, 0:32], psF[:, 3, 64:96]

        def fP2(e):
            e.matmul(pP, lhsT=rc['c_slt'][:], rhs=ohs[:], start=True, stop=True)
            return e.matmul(pTot, lhsT=ones_f[:], rhs=ohs[:], start=True, stop=True)
        S.op('pe', fP2, R=['ohs', 'c_slt', 'ones_f'], W=['b3'])
        V(lambda e: e.tensor_tensor(out=cb[:], in0=pP, in1=base[:], op=ALU.add), ['b3', 'base'], ['cb'])
        V(lambda e: e.tensor_tensor(out=base[:], in0=base[:], in1=pTot, op=ALU.add), ['b3', 'base'], ['base'])
        V(lambda e: e.tensor_tensor(out=cb2[:], in0=cb[:], in1=rc['c_ecap'][:], op=ALU.add), ['cb', 'c_ecap'], ['cb2'])
        for k_, oh_ in ((0, oh0), (1, oh1)):
            c0_ = 6 + 4 * k_
            V(lambda e: e.tensor_tensor(out=t32[:], in0=oh_[:], in1=cb[:], op=ALU.mult), ['oh0', 'oh1', 'cb', 't32'], ['t32'])
            V(lambda e: e.tensor_reduce(out=sm[:, c0_:c0_ + 1], in_=t32[:], axis=AX.X, op=ALU.add), ['t32'], [('smk', k_)])
            V(lambda e: e.tensor_tensor(out=t32[:], in0=oh_[:], in1=cb2[:], op=ALU.mult), ['oh0', 'oh1', 'cb2', 't32'], ['t32'])
            V(lambda e: e.tensor_reduce(out=sm[:, c0_ + 1:c0_ + 2], in_=t32[:], axis=AX.X, op=ALU.add), ['t32'], [('smk', k_)])
            V(lambda e: e.tensor_scalar(out=sm[:, c0_:c0_ + 1], in0=sm[:, c0_:c0_ + 1], scalar1=float(CAP) - 0.5, scalar2=None,
                                        op0=ALU.is_lt), [('smk', k_)], [('smk', k_)])
            V(lambda e: e.tensor_tensor(out=sm[:, c0_:c0_ + 1], in0=sm[:, c0_:c0_ + 1], in1=vcol, op=ALU.mult), [('smk', k_), 'c_valid'], [('smk', k_)])
            V(lambda e: e.tensor_tensor(out=sm[:, c0_ + 1:c0_ + 2], in0=sm[:, c0_ + 1:c0_ + 2], in1=rc['c_trash'][:, 0:1], op=ALU.subtract),
              [('smk', k_), 'c_trash'], [('smk', k_)])
            V(lambda e: e.tensor_tensor(out=sm[:, c0_ + 1:c0_ + 2], in0=sm[:, c0_ + 1:c0_ + 2], in1=sm[:, c0_:c0_ + 1], op=ALU.mult), [('smk', k_)], [('smk', k_)])
            V(lambda e: e.tensor_tensor(out=sm[:, c0_ + 1:c0_ + 2], in0=sm[:, c0_ + 1:c0_ + 2], in1=rc['c_trash'][:, 0:1], op=ALU.add),
              [('smk', k_), 'c_trash'], [('smk', k_)])
            V(lambda e: e.tensor_copy(out=slots_i[:, i, k_:k_ + 1], in_=sm[:, c0_ + 1:c0_ + 2]), [('smk', k_)], [('slots', i, k_)])
            S.dma('pool', None, None, R=[('h2b', q), ('slots', i, k_)], W=['XeD'],
                  fn=lambda e: e.indirect_dma_start(out=XeD[:, :], out_offset=bass.IndirectOffsetOnAxis(ap=slots_i[:, i, k_:k_ + 1], axis=0),
                                                    in_=h2b[q][:, :], in_offset=None))

    S.barrier()
    stack[0].close()
    stack[0] = ExitStack()
    eg_v = e_gate.rearrange("e (k p) n -> e p k n", p=128)
    eu_v = e_up.rearrange("e (k p) n -> e p k n", p=128)
    ed_v = e_down.rearrange("e (k p) n -> e p k n", p=128)
    WE = [sb(f"WE{j}", [128, KC, 512], BF16) for j in range(9)]
    xe = [sb(f"xe{j}", [128, D], BF16) for j in range(2)]
    XeT = sb("XeT", [128, KC, CAP], BF16)
    actT = sb("actT", [128, 8, CAP], BF16)
    sl_ = [sb(f"sl{j}", [128, CAP], F32) for j in range(2)]
    ye = [sb(f"ye{j}", [128, D], F32) for j in range(2)]
    we_i = [0]

    def nextWE():
        j = we_i[0] % 9
        we_i[0] += 1
        return j

    cnt = 0
    for ex in range(NE if 'E' in ST else 0):
        for t2 in range(CAP // 128):
            S.dma('sp', xe[t2][:], XeD[ex * CAP + t2 * 128:ex * CAP + (t2 + 1) * 128, :], R=['XeD'], W=[('xe', t2)])
            transpose_to(XeT, xe[t2], ('xe', t2), 'XeT', t2)
        for fh in range(2):
            wg, wu = nextWE(), nextWE()
            S.dma('pool', WE[wg][:], eg_v[ex, :, :, fh * 512:(fh + 1) * 512], W=[('WE', wg)])
            S.dma('pool', WE[wu][:], eu_v[ex, :, :, fh * 512:(fh + 1) * 512], W=[('WE', wu)])
            for fc in range(4):
                q = cnt % 2
                cnt += 1
                pG, pU_ = psF[:, q, 0:CAP], psF[:, q, 256:256 + CAP]

                def fGU(e):
                    for k in range(KC):
                        e.matmul(pG, lhsT=WE[wg][:, k, fc * 128:(fc + 1) * 128], rhs=XeT[:, k, :], start=(k == 0), stop=(k == KC - 1))
                    ins = None
                    for k in range(KC):
                        ins = e.matmul(pU_, lhsT=WE[wu][:, k, fc * 128:(fc + 1) * 128], rhs=XeT[:, k, :], start=(k == 0), stop=(k == KC - 1))
                    return ins
                S.op('pe', fGU, R=[('XeT', t_) for t_ in range(CAP // 128)] + [('WE', wg), ('WE', wu)], W=[f'b{q}'])
                A(lambda e: e.activation(out=sl_[q][:], in_=pG, func=AF.Silu), [f'b{q}'], [('sl', q)])
                V(lambda e: e.tensor_tensor(out=actT[:, fh * 4 + fc, :], in0=sl_[q][:], in1=pU_, op=ALU.mult), [('sl', q), f'b{q}'], ['actT'])
        wd0, wd1 = nextWE(), nextWE()
        S.dma('pool', WE[wd0][:, 0:8, :], ed_v[ex, :, :, 0:512], W=[('WE', wd0)])
        S.dma('pool', WE[wd0][:, 8:16, :], ed_v[ex, :, :, 512:1024], W=[('WE', wd0)])
        S.dma('pool', WE[wd1][:, 0:8, :], ed_v[ex, :, :, 1024:1536], W=[('WE', wd1)])
        S.dma('pool', WE[wd1][:, 8:16, :], ed_v[ex, :, :, 1536:2048], W=[('WE', wd1)])
        for t2 in range(CAP // 128):
            for cbk in range(4):
                wsel = WE[wd0] if cbk < 2 else WE[wd1]
                wkey = ('WE', wd0) if cbk < 2 else ('WE', wd1)
                ko = 8 * (cbk % 2)
                q = cnt % 2
                cnt += 1
                pD = psF[:, 2 + q, :]

                def fDn(e):
                    ins = None
                    for k in range(8):
                        ins = e.matmul(pD, lhsT=actT[:, k, t2 * 128:(t2 + 1) * 128], rhs=wsel[:, ko + k, :], start=(k == 0), stop=(k == 7))
                    return ins
                S.op('pe', fDn, R=['actT', wkey], W=[f'b{2 + q}'])
                A(lambda e: e.activation(out=ye[t2][:, cbk * 512:(cbk + 1) * 512], in_=pD, func=AF.Copy), [f'b{2 + q}'], [('ye', t2)])
            S.dma('sp', YeD[ex * CAP + t2 * 128:ex * CAP + (t2 + 1) * 128, :], ye[t2][:], R=[('ye', t2)], W=['YeD'])

    S.barrier()
    stack[0].close()
    stack[0] = ExitStack()
    junk = sb("junkF", [128, D], BF16)
    ss = sb("ssF", [128, 4], F32)
    gfin = sb("gfin", [128, D], F32)
    S.dma('sp', gbc[:], g_ple[0:1, :].partition_broadcast(128), W=['gbc'])
    S.dma('sp', gfin[:], g_final[0:1, :].partition_broadcast(128), W=['gfin'])
    wp_v = w_ple_gate.rearrange("(k p) n -> p k n", p=128)
    wpp_v = w_ple_proj.rearrange("(k p) n -> p k n", p=128)
    xg = [sb(f"xf{j}", [128, D], F32) for j in range(4)]
    yg = [sb(f"yg{j}", [128, D], F32) for j in range(2)]
    hbF = [sb(f"hbF{j}", [128, D], BF16) for j in range(2)]
    h3T = sb("h3T", [128, KC, 512], BF16)
    pt = sb("pt", [128, DPLE], F32)
    ptb = sb("ptb", [128, DPLE], BF16)
    pT = sb("pT", [128, 2, 512], BF16)
    WP = [sb(f"WP{j}", [128, KC, 512], BF16) for j in range(2)]
    WPP = [sb(f"WPP{j}", [128, 2, 512], BF16) for j in range(2)]
    sgF = [sb(f"sgF{j}", [128, 512], F32) for j in range(2)]
    yout = [sb(f"yout{j}", [128, D], F32) for j in range(2)]
    wcnt = 0
    V(lambda e: e.memset(yg[0][:], 0.0), [], [('yg', 0)])
    S.dma('sp', YeD[NSLOT:NSLOT + 128, :], yg[0][:], R=[('yg', 0)], W=['YeD'])
    for (i0, n) in (groups if 'F' in ST else []):
        for j in range(n):
            i = i0 + j
            S.dma('sp', xg[j][:], x1D[i * 128:(i + 1) * 128, :], R=[('x1D', i)], W=[('xf', j)])
            for k_ in range(2):
                V(lambda e: e.memset(yg[k_][:], 0.0), [], [('yg', k_)])
                S.dma('pool', None, None, R=['YeD', ('slots', i, k_)], W=[('yg', k_)],
                      fn=lambda e: e.indirect_dma_start(out=yg[k_][:, :], out_offset=None, in_=YeD[:, :],
                                                        in_offset=bass.IndirectOffsetOnAxis(ap=slots_i[:, i, k_:k_ + 1], axis=0)))
                V(lambda e: e.scalar_tensor_tensor(out=xg[j][:], in0=yg[k_][:], scalar=wts[:, i, k_:k_ + 1], in1=xg[j][:],
                                                   op0=ALU.mult, op1=ALU.add), [('yg', k_), ('wts', i), ('xf', j)], [('xf', j)])
            rmsnorm_tile(xg[j][:], hbF[j % 2][:], gbc[:], ('xf', j), ('hbF', j % 2), j % 2)
            transpose_to(h3T, hbF[j % 2], ('hbF', j % 2), 'h3T', j)
            S.dma('sp', pt[:], p_in[i * 128:(i + 1) * 128, :], W=['pt'])
            A(lambda e: e.activation(out=ptb[:], in_=pt[:], func=AF.Copy), ['pt'], ['ptb'])
            transpose_to(pT, ptb, 'ptb', 'pT', j, nk=2)
        for blk in range(4):
            cs_ = slice(blk * 512, (blk + 1) * 512)
            w1 = wcnt % 2
            wcnt += 1
            S.dma('pool', WP[w1][:], wp_v[:, :, cs_], W=[('WP', w1)])
            S.dma('pool', WPP[w1][:], wpp_v[:, :, cs_], W=[('WPP', w1)])
            for j in range(n):
                q = j % 2
                tj = slice(j * 128, (j + 1) * 128)
                S.op('pe', mm16(psF[:, q, :], lambda k: h3T[:, k, tj], WP[w1]), R=[('h3T', j), ('WP', w1)], W=[f'b{q}'])
                S.op('pe', mm16(psF[:, 2 + q, :], lambda k: pT[:, k, tj], WPP[w1], nk=2), R=[('pT', j), ('WPP', w1)], W=[f'b{2 + q}'])
                A(lambda e: e.activation(out=sgF[q][:], in_=psF[:, q, :], func=AF.Sigmoid), [f'b{q}'], [('sgF', q)])
                V(lambda e: e.tensor_tensor(out=sgF[q][:], in0=sgF[q][:], in1=psF[:, 2 + q, :], op=ALU.mult), [('sgF', q), f'b{2 + q}'], [('sgF', q)])
                V(lambda e: e.tensor_tensor(out=xg[j][:, cs_], in0=xg[j][:, cs_], in1=sgF[q][:], op=ALU.add), [('xf', j), ('sgF', q)], [('xf', j)])
        for j in range(n):
            i = i0 + j
            rmsnorm_tile(xg[j][:], yout[j % 2][:], gfin[:], ('xf', j), ('yout', j % 2), j % 2, gkey='gfin')
            S.dma('sp', y_o[i * 128:(i + 1) * 128, :], yout[j % 2][:], R=[('yout', j % 2)], W=[('y', i)])
    S.finish()
    CONSTS = dict(CONSTS)
    CONSTS.update(WCONSTS)
    CONSTS.update(RCONSTS)
    return nc, CONSTS


_CACHE = {}


def _prep(inp, CONSTS, cores=range(8)):
    f32 = np.float32
    rope = _rope_table()
    ident = np.eye(128, dtype=f32)
    xp = np.asarray(inp['x_prompt'], f32)
    xs = np.asarray(inp['x_sample'], f32)
    prm = np.concatenate([np.asarray(inp[k], f32).reshape(-1) for k in
                          ('wkv_mu', 'wkv_w0', 'wkv_a0', 'wkv_k_k', 'wkv_k_a', 'wkv_r_k', 'wkv_ln_w', 'wkv_ln_b')])[None, :]
    W = {k: np.ascontiguousarray(np.asarray(inp[k], f32)[0]) for k in
         ('w_oa', 'w_ob', 'w_out', 'e_gate', 'e_up', 'e_down', 'w_ple_gate', 'w_ple_proj')}
    for k in ('g_ffn', 'g_ple'):
        W[k] = np.ascontiguousarray(np.asarray(inp[k], f32).reshape(1, D))
    W['g_final'] = np.ascontiguousarray(np.asarray(inp['g_final'], f32).reshape(1, D))
    W['wr'] = np.ascontiguousarray(np.concatenate([np.asarray(inp['router_g_w'], f32)[0], np.asarray(inp['router_e_w'], f32)[0]], 1))
    W['rbias'] = np.ascontiguousarray(np.concatenate([np.asarray(inp['router_g_b'], f32)[0], np.asarray(inp['router_e_b'], f32)[0]])[None, :])
    pp_ = np.asarray(inp['p_prompt'], f32)[0]
    ps_ = np.asarray(inp['p_sample'], f32)[0]
    import os
    if 'E' not in os.environ.get('KSTAGES', 'E').split(','):
        for k in ('e_gate', 'e_up', 'e_down'):
            W[k] = W[k][0:1]
    in_maps = []
    for c in cores:
        hf = c // 4
        pc = np.zeros((TL, DPLE), f32)
        pc[:1024] = pp_[c % 4][1024 * hf:1024 * hf + 1024]
        pc[1024:1088] = ps_[16 * c:16 * c + 16].reshape(64, DPLE)
        xh = np.zeros((TL, D), f32)
        xh[:1024] = xp[c % 4][1024 * hf:1024 * hf + 1024]
        xh[1024:1088] = xs[16 * c:16 * c + 16].reshape(64, D)
        ridx = np.zeros((128, NTL), np.int32)
        for l in range(NTL):
            gt = 8 * hf + l if l < 8 else 16
            ridx[:, l] = gt * 128 + np.arange(128)
        xc = np.zeros((T, D), f32)
        xc[:2048] = xp[c % 4]
        xc[2048:2112] = xs[16 * c:16 * c + 16].reshape(64, D)
        m = {
            'x': xc,
            'st_ret': np.ascontiguousarray(inp['state_ret'][0, 16 * c:16 * c + 16]),
            'g_mix': np.ascontiguousarray(inp['g_mix']),
            'w_in': np.ascontiguousarray(inp['w_in'][0]),
            'rope': rope,
            'ident': ident,
            'prm': prm,
            'st_shift': np.ascontiguousarray(inp['state_shift'][0, 16 * c:16 * c + 16]),
            'st_wkv': np.ascontiguousarray(inp['state_wkv'][0, 16 * c:16 * c + 16]),
            'wkv_w2': np.ascontiguousarray(inp['wkv_w2'][0]),
            'wkv_a2': np.ascontiguousarray(inp['wkv_a2'][0]),
            'wkv_g2': np.ascontiguousarray(inp['wkv_g2'][0]),
            'w_oa': W['w_oa'], 'w_ob': W['w_ob'], 'w_out': W['w_out'], 'g_ffn': W['g_ffn'], 'wr': W['wr'], 'rbias': W['rbias'],
            'e_gate': W['e_gate'], 'e_up': W['e_up'], 'e_down': W['e_down'], 'g_ple': W['g_ple'],
            'w_ple_gate': W['w_ple_gate'], 'w_ple_proj': W['w_ple_proj'], 'g_final': W['g_final'],
            'p_in': pc, 'x_h': xh, 'ridx': ridx,
        }
        m.update(CONSTS)
        in_maps.append(m)
    return in_maps


def kernel(**inp):
    f32 = np.float32
    nc, CONSTS = build()
    in_maps = _prep(inp, CONSTS)
    res = run_bass_kernel_spmd(nc, in_maps, core_ids=list(range(8)))
    R = res.results
    y_p = np.stack([np.concatenate([R[c]['y'][:1024], R[c + 4]['y'][:1024]], 0) for c in range(4)], 0)
    y_s = np.concatenate([R[c]['y'][1024:1088].reshape(16, 4, D) for c in range(8)], 0)
    ret_p = np.stack([R[c]['ret_p'] for c in range(4)], 0)[None]
    ret_s = np.concatenate([R[c]['ret_s'] for c in range(8)], 0)[None]
    wkv_p = np.stack([R[c]['wkv_p'] for c in range(4)], 0)[None]
    sh_p = np.stack([R[c]['sh_p'][0] for c in range(4)], 0)[None]
    wkv_s = np.concatenate([R[c]['wkv_s'] for c in range(8)], 0)[None]
    sh_s = np.concatenate([R[c]['sh_s'] for c in range(8)], 0)[None]
    return (y_p, y_s, ret_p, wkv_p, sh_p, ret_s, wkv_s, sh_s)
```

```python
import numpy as np
from contextlib import ExitStack
import ml_dtypes
import concourse.bass as bass
import concourse.mybir as mybir
from concourse.bass_utils import run_bass_kernel_spmd

F32 = mybir.dt.float32
BF16 = mybir.dt.bfloat16
I32 = mybir.dt.int32
AF = mybir.ActivationFunctionType
ALU = mybir.AluOpType
AX = mybir.AxisListType

D = 2048
NT = 17
T = NT * 128
KC = 16
RH, RDK, RDV = 8, 128, 256
WH, WN = 16, 64
RWKV_PROJ = 3360
IN_COLS = 13600
OFF_Q, OFF_K, OFF_V, OFF_GR, OFF_ZB, OFF_G = 0, 1024, 2048, 4096, 6144, 9504
NE, EPG, DE = 32, 8, 1024
DPLE = 256
PAST = 16384
EPS = 1e-6
CAP = 128
NSLOT = NE * CAP
NTL = 9
TL = NTL * 128


class Sched:
    def __init__(self, nc):
        self.nc = nc
        self.eng = {'pe': nc.tensor, 'dve': nc.vector, 'act': nc.scalar, 'pool': nc.gpsimd, 'sp': nc.sync}
        self.sem = {}
        self.cnt = {}
        self.nsem = 0
        for e in self.eng:
            self._newsem(e)
        self.waited = {e: {} for e in self.eng}
        self.lastw = {}
        self.readers = {}
        self.RING = 8
        self.ring = {}
        for q in ('sp', 'pool', 'act'):
            self.ring[q] = [[self._mksem(), 0] for _ in range(self.RING)]
        self.ring_i = {q: 0 for q in self.ring}
        self.nins = 0

    def _mksem(self):
        self.nsem += 1
        return (self.nc.semaphore(f"sm{self.nsem}").__enter__(), self.nsem)

    def _newsem(self, e):
        self.sem[e] = self._mksem()
        self.cnt[e] = 0

    def _wait(self, e, tickets):
        w = self.waited[e]
        for (sem, sid, val) in tickets:
            if w.get(sid, 0) >= val:
                continue
            self.eng[e].wait_ge(sem, val)
            w[sid] = val

    @staticmethod
    def _is_psum(k):
        n = k[0] if isinstance(k, tuple) else k
        return isinstance(n, str) and (n in ('psT', 'bkA', 'bkO', 'psZ', 'psP', 'psB') or (len(n) == 2 and n[0] == 'b' and n[1].isdigit()))

    def _deps(self, R, W, me=None):
        t = []
        for k in R:
            if k in self.lastw:
                t.append(self.lastw[k])
            if self._is_psum(k):
                r = self.readers.get(k)
                if r:
                    t.extend(v for s_, v in r.items() if s_ != me)
        for k in W:
            if k in self.lastw:
                t.append(self.lastw[k])
            r = self.readers.get(k)
            if r:
                t.extend(r.values())
        return t

    def _record(self, tk, slot, R, W):
        for k in R:
            self.readers.setdefault(k, {})[slot] = tk
        for k in W:
            self.lastw[k] = tk
            self.readers[k] = {}

    def op(self, e, fn, R=(), W=()):
        deps = self._deps(R, W, e)
        if e == 'pe':
            pesid = self.sem['pe'][1]
            deps = [d for d in deps if d[1] != pesid]
        self._wait(e, deps)
        ins = fn(self.eng[e])
        if self.cnt[e] >= 30000:
            self._newsem(e)
        self.cnt[e] += 1
        sem, sid = self.sem[e]
        ins.then_inc(sem, 1)
        self.nins += 1
        self._record((sem, sid, self.cnt[e]), e, R, W)

    def dma(self, q, out, in_, R=(), W=(), fn=None):
        deps = self._deps(R, W)
        i = self.ring_i[q]
        self.ring_i[q] = (i + 1) % self.RING
        slot = self.ring[q][i]
        (sem, sid), val = slot
        if val > 0:
            deps.append((sem, sid, val))
        self._wait(q, deps)
        if fn is None:
            ins = self.eng[q].dma_start(out=out, in_=in_)
        else:
            ins = fn(self.eng[q])
        slot[1] = val + 16
        ins.then_inc(sem, 16)
        self.nins += 1
        self._record((sem, sid, val + 16), (q, i), R, W)

    def barrier(self):
        tk = []
        for q in self.ring:
            for (sem, sid), val in self.ring[q]:
                if val > 0:
                    tk.append((sem, sid, val))
        for e in ('pe', 'dve', 'act', 'pool'):
            sem, sid = self.sem[e]
            if self.cnt[e] > 0:
                tk.append((sem, sid, self.cnt[e]))
        for e in ('pe', 'dve', 'act', 'pool', 'sp'):
            self._wait(e, tk)

    def finish(self):
        tk = []
        for q in self.ring:
            for (sem, sid), val in self.ring[q]:
                if val > 0:
                    tk.append((sem, sid, val))
        for e in ('pe', 'dve', 'act', 'pool'):
            sem, sid = self.sem[e]
            if self.cnt[e] > 0:
                tk.append((sem, sid, self.cnt[e]))
        self._wait('sp', tk)


def _ret_consts():
    h = np.arange(RH, dtype=np.float64)
    log_g = np.log1p(-np.exp2(-5.0 - h))
    C = 128
    idx = np.arange(C, dtype=np.float64)
    diff = idx[:, None] - idx[None, :]
    scale = RDK ** -0.5
    mask = np.where(diff >= 0, np.exp(log_g[:, None, None] * np.maximum(diff, 0.0)), 0.0)
    maskT = mask.transpose(0, 2, 1) * scale
    qdec = np.exp(log_g[:, None] * (idx + 1.0))
    kdec = np.exp(log_g[:, None] * (C - 1.0 - idx)) * scale
    cdec = np.exp(log_g * C)
    r = np.arange(128)
    s_id = r // 4
    t_id = (r % 4).astype(np.float64)
    valid = r < 64
    same = (s_id[:, None] == s_id[None, :]) & valid[:, None] & valid[None, :]
    dd = t_id[:, None] - t_id[None, :]
    mask_s = np.where(same & (dd >= 0), np.exp(log_g[:, None, None] * np.maximum(dd, 0.0)), 0.0)
    maskT_s = mask_s.transpose(0, 2, 1) * scale
    qdec_s = np.exp(log_g[:, None] * (t_id + 1.0)) * valid
    kdec_s = np.exp(log_g[:, None] * (3.0 - t_id)) * scale * valid
    cdec_s = np.exp(log_g * 4.0)
    out = {
        'c_maskT': np.ascontiguousarray(maskT.transpose(1, 0, 2)).astype(np.float32),
        'c_maskT_s': np.ascontiguousarray(maskT_s.transpose(1, 0, 2)).astype(np.float32),
        'c_qdec': np.broadcast_to(qdec[None], (128, RH, 128)).astype(np.float32).copy(),
        'c_qdec_s': np.broadcast_to(qdec_s[None], (128, RH, 128)).astype(np.float32).copy(),
        'c_kdec': np.ascontiguousarray(kdec.T).astype(np.float32),
        'c_kdec_s': np.ascontiguousarray(kdec_s.T).astype(np.float32),
    }
    smi = np.zeros((128, 16, 128), np.float32)
    for s in range(16):
        smi[:, s, 4 * s:4 * s + 4] = 1.0
    smj = np.zeros((128, 16), np.float32)
    for s in range(16):
        smj[4 * s:4 * s + 4, s] = 1.0
    out['c_smi'] = smi
    out['c_smj'] = smj
    return out, [float(x) for x in cdec], [float(x) for x in cdec_s]


def _wkv_consts():
    r = np.arange(128)
    up = (r[:, None] <= r[None, :])
    sup = (r[:, None] < r[None, :])
    slo = (r[:, None] > r[None, :])
    valid = r < 64
    same = (r[:, None] // 4 == r[None, :] // 4) & valid[:, None] & valid[None, :]
    tri = np.stack([up, up & same], 1).astype(np.float32)
    ones = np.stack([np.ones((128, 128), bool), same], 1).astype(np.float32)
    m5 = np.zeros((128, 2, 5, 128), np.float32)
    for v, extra in ((0, np.ones((128, 128), bool)), (1, same)):
        m5[:, v, 0] = sup & extra
        m5[:, v, 1] = slo & extra
        m5[:, v, 2] = sup & extra
        m5[:, v, 3] = up & extra
        m5[:, v, 4] = up & extra
    tm = ((r % 4 != 0) & valid).astype(np.float32)[:, None]
    shift = np.zeros((128, 3, 128), np.float32)
    shift[:, 0] = (r[:, None] == r[None, :] - 1)
    shift[:, 1] = (r[:, None] == r[None, :] - 1) & same & (r[None, :] % 4 != 0)
    shift[127, 2, 0] = 1.0
    return {'c_tri': tri, 'c_ones': ones, 'c_m5': m5, 'c_tm': tm, 'c_shift': shift}


def _route_consts():
    r = np.arange(128)
    slt = (r[:, None] < r[None, :]).astype(np.float32)
    ecap = np.broadcast_to((np.arange(32, dtype=np.float32) * CAP)[None, :], (128, 32)).copy()
    valid = np.stack([np.ones(128, np.float32), (r < 64).astype(np.float32)], 1)
    trash = (NSLOT + r).astype(np.float32)[:, None]
    return {'c_slt': slt, 'c_ecap': ecap, 'c_valid': valid, 'c_trash': trash}


def _rope_table():
    half = RDK // 2
    inv = (10000.0 ** (-np.arange(half, dtype=np.float32) / half)).astype(np.float32)
    pos = np.zeros((T,), np.float32)
    pos[:2048] = np.arange(2048, dtype=np.float32)
    pos[2048:2112] = np.tile(PAST + np.arange(4, dtype=np.float32), 16)
    ang = pos[:, None] * inv[None, :]
    tab = np.stack([np.cos(ang), np.sin(ang)], 1).astype(np.float32)
    return tab


def build(debug_stage=99):
    import os
    ST = os.environ.get('KSTAGES', 'B,C1,C2,D,E0,E,F').split(',')
    nc = bass.Bass("TRN2", target_bir_lowering=False)
    S = Sched(nc)
    CONSTS, CDEC, CDEC_S = _ret_consts()

    def din(name, shape, dt=F32):
        return nc.dram_tensor(name, list(shape), dt, kind="ExternalInput").ap()

    def dout(name, shape, dt=F32):
        return nc.dram_tensor(name, list(shape), dt, kind="ExternalOutput").ap()

    def dscr(name, shape, dt=F32):
        return nc.dram_tensor(name, list(shape), dt, kind="Internal").ap()

    stack = [None]

    def sb(name, shape, dt=F32):
        cm = nc.sbuf_tensor(name, list(shape), dt)
        if stack[0] is not None:
            return stack[0].enter_context(cm)
        return cm.__enter__()

    x = din("x", [T, D])
    st_ret = din("st_ret", [16, RH, RDK, RDV])
    g_mix = din("g_mix", [1, D])
    w_in = din("w_in", [D, IN_COLS])
    rope = din("rope", [T, 2, 64])
    ident_d = din("ident", [128, 128])
    cst = {k: din(k, v.shape) for k, v in CONSTS.items()}

    prm = din("prm", [1, RWKV_PROJ + 7168])
    st_shift = din("st_shift", [16, RWKV_PROJ])
    st_wkv = din("st_wkv", [16, WH, WN, WN])
    wkv_w2 = din("wkv_w2", [64, 1024])
    wkv_a2 = din("wkv_a2", [64, 1024])
    wkv_g2 = din("wkv_g2", [160, 1024])
    WCONSTS = _wkv_consts()
    wc = {k: din(k, v.shape) for k, v in WCONSTS.items()}
    w_oa = din("w_oa", [D, D])
    w_ob = din("w_ob", [1024, D])
    w_out = din("w_out", [D, D])
    g_ffn = din("g_ffn", [1, D])
    wr = din("wr", [D, 36])
    rbias = din("rbias", [1, 36])
    NE_D = NE if 'E' in ST else 1
    e_gate = din("e_gate", [NE_D, D, DE])
    e_up = din("e_up", [NE_D, D, DE])
    e_down = din("e_down", [NE_D, DE, D])
    g_ple = din("g_ple", [1, D])
    w_ple_gate = din("w_ple_gate", [D, D])
    w_ple_proj = din("w_ple_proj", [DPLE, D])
    g_final = din("g_final", [1, D])
    p_in = din("p_in", [TL, DPLE])
    x_h = din("x_h", [TL, D])
    ridx = din("ridx", [128, NTL], I32)
    RCONSTS = _route_consts()
    rcd = {k: din(k, v.shape) for k, v in RCONSTS.items()}
    x1D = dscr("x1D", [TL, D], F32)
    XeD = dscr("XeD", [NSLOT + 128, D], BF16)
    YeD = dscr("YeD", [NSLOT + 128, D], F32)
    sh_p = dout("sh_p", [1, RWKV_PROJ])
    sh_s = dout("sh_s", [16, RWKV_PROJ])
    wkv_p = dout("wkv_p", [WH, WN, WN])
    wkv_s = dout("wkv_s", [16, WH, WN, WN])
    uD = dscr("uD", [T, RWKV_PROJ], F32)
    obD = dscr("obD", [T, 1024], BF16)
    y_o = dout("y", [TL, D])
    ret_p = dout("ret_p", [RH, RDK, RDV])
    ret_s = dout("ret_s", [16, RH, RDK, RDV])

    oD = dscr("oD", [T, D], BF16)

    w_in_v = w_in.rearrange("(k p) n -> p k n", p=128)

    identb = sb("identb", [128, 128], BF16)
    identf = sb("identf", [128, 128], F32)
    gbc = sb("gbc", [128, D], F32)
    hT_stack = ExitStack()
    hT = hT_stack.enter_context(nc.sbuf_tensor("hT", [128, KC, T], BF16))
    psF = nc.psum_tensor("psF", [128, 7, 512], F32).__enter__()
    psT = nc.psum_tensor("psT", [128, 1024], BF16).__enter__()

    S.dma('sp', identf[:], ident_d[:, :], W=['identf'])
    S.op('dve', lambda e: e.tensor_copy(out=identb[:], in_=identf[:]), R=['identf'], W=['identb'])
    S.dma('sp', gbc[:], g_mix[0:1, :].partition_broadcast(128), W=['gbc'])

    stack[0] = ExitStack()
    xt = [sb(f"xt{i}", [128, D], F32) for i in range(2)]
    hb = [sb(f"hb{i}", [128, D], BF16) for i in range(2)]
    junk = sb("junk", [128, D], BF16)
    ss = sb("ss", [128, 4], F32)

    def rmsnorm_tile(src, dst_bf, gtile, key_src, key_dst, u, gkey='gbc'):
        S.op('act', lambda e: e.activation(out=junk[:], in_=src, func=AF.Square, accum_out=ss[:, u:u + 1]),
             R=[key_src], W=['junk', ('ss', u)])
        S.op('dve', lambda e: e.tensor_scalar(out=ss[:, u:u + 1], in0=ss[:, u:u + 1], scalar1=1.0 / D, scalar2=EPS,
                                              op0=ALU.mult, op1=ALU.add), R=[('ss', u)], W=[('ss', u)])
        S.op('act', lambda e: e.sqrt(out=ss[:, u:u + 1], in_=ss[:, u:u + 1]), R=[('ss', u)], W=[('ss', u)])
        S.op('dve', lambda e: e.reciprocal(out=ss[:, u:u + 1], in_=ss[:, u:u + 1]), R=[('ss', u)], W=[('ss', u)])
        S.op('dve', lambda e: e.scalar_tensor_tensor(out=dst_bf, in0=src, scalar=ss[:, u:u + 1], in1=gtile,
                                                     op0=ALU.mult, op1=ALU.mult),
             R=[key_src, ('ss', u), gkey], W=[key_dst])

    def transpose_to(dstT, src_bf, key_src, key_dst, i, nk=KC):
        for half in range(0, nk, 8):
            n = min(8, nk - half)

            def f(e, half=half, n=n):
                ins = None
                for k in range(n):
                    ins = e.transpose(psT[:, k * 128:(k + 1) * 128], src_bf[:, (half + k) * 128:(half + k + 1) * 128], identb[:])
                return ins
            S.op('pe', f, R=[key_src, 'identb'], W=['psT'])
            S.op('act', lambda e, half=half, n=n: e.activation(
                out=dstT[:, half:half + n, i * 128:(i + 1) * 128],
                in_=psT[:, 0:n * 128].rearrange("p (k t) -> p k t", k=n), func=AF.Copy),
                R=['psT'], W=[(key_dst, i)])

    for i in range(NT):
        b = i % 2
        S.dma('sp', xt[b][:], x[i * 128:(i + 1) * 128, :], W=[('xt', b)])
        rmsnorm_tile(xt[b][:], hb[b][:], gbc[:], ('xt', b), ('hb', b), b)
        transpose_to(hT, hb[b], ('hb', b), 'hT', i)

    S.barrier()
    stack[0].close()
    if True:
        stack[0] = ExitStack()
        junk = sb("junkB", [128, 256], BF16)
        cs = {}
        for k, v in CONSTS.items():
            if k == 'c_smi':
                continue
            cs[k] = sb("s_" + k, list(v.shape), F32)
            S.dma('sp', cs[k][:], cst[k], W=[k])
        smi_b = sb("smi_b", [128, 16, 128], BF16)
        S.dma('pool', smi_b[:], cst['c_smi'], W=['smi_b'])
        ropet = sb("ropet", [128, NT, 2, 64], F32)
        S.dma('sp', ropet[:], rope.rearrange("(n p) a f -> p n a f", p=128), W=['rope'])

        Wqk = [sb(f"Wqk{i}", [128, KC, 256], BF16) for i in range(2)]
        Wvg = [sb(f"Wvg{i}", [128, KC, 512], BF16) for i in range(2)]
        qk_r = [sb(f"qk_r{i}", [128, 2, 128], BF16) for i in range(2)]
        rt = [sb(f"rt{i}", [128, 2, 2, 64], F32) for i in range(4)]
        v_bf = [sb(f"v_bf{i}", [128, 256], BF16) for i in range(2)]
        sg = [sb(f"sg{i}", [128, 256], F32) for i in range(2)]
        qkT = [sb(f"qkT{i}", [128, 2, 128], BF16) for i in range(2)]
        qTd = [sb(f"qTd{i}", [128, 128], BF16) for i in range(2)]
        kd = [sb(f"kd{i}", [128, 128], BF16) for i in range(2)]
        sT = [sb(f"sT{i}", [128, 128], BF16) for i in range(2)]
        Sf = sb("Sf", [128, 256], F32)
        Sb = [sb(f"Sb{i}", [128, 256], BF16) for i in range(2)]
        on = [sb(f"on{i}", [128, 256], BF16) for i in range(2)]
        oss = sb("oss", [128, 2], F32)
        S0f = sb("S0f", [128, 8, 256], F32)
        S0b = sb("S0b", [128, 16, 256], BF16)
        Snew = sb("Snew", [128, 8, 256], F32)
        qTd_m = sb("qTd_m", [128, 16, 128], BF16)
        kd_m = sb("kd_m", [128, 16, 128], BF16)

        for h in range(RH if 'B' in ST else 0):
            wb = h % 2
            S.dma('pool', Wqk[wb][:, :, 0:128], w_in_v[:, :, OFF_Q + h * 128:OFF_Q + (h + 1) * 128], W=[('Wqk', wb)])
            S.dma('pool', Wqk[wb][:, :, 128:256], w_in_v[:, :, OFF_K + h * 128:OFF_K + (h + 1) * 128], W=[('Wqk', wb)])
            S.dma('pool', Wvg[wb][:, :, 0:256], w_in_v[:, :, OFF_V + h * 256:OFF_V + (h + 1) * 256], W=[('Wvg', wb)])
            S.dma('pool', Wvg[wb][:, :, 256:512], w_in_v[:, :, OFF_GR + h * 256:OFF_GR + (h + 1) * 256], W=[('Wvg', wb)])
            S.dma('pool', S0b[:], st_ret[:, h, :, :].rearrange("s d e -> d s e"), W=['S0b'])
            S.op('dve', lambda e: e.memset(Sf[:], 0.0), W=['Sf'])
            S.op('dve', lambda e: e.memset(Sb[0][:], 0.0), W=[('Sb', 0)])

            def tileB(i, phase):
                p = i % 2
                smp = (i == NT - 1)
                bA, bB, bO = 3 * p, 3 * p + 1, 3 * p + 2
                psA = psF[:, bA, 0:256]
                psSC = psF[:, bA, 256:384]
                psB = psF[:, bB, :]
                psO = psF[:, bO, 0:256]
                psDS = psF[:, bO, 256:512]
                tsl = slice(i * 128, (i + 1) * 128)

                if phase == 1:
                    def fA(e):
                        ins = None
                        for k in range(KC):
                            ins = e.matmul(psA, lhsT=hT[:, k, tsl], rhs=Wqk[wb][:, k, :], start=(k == 0), stop=(k == KC - 1))
                        return ins
                    S.op('pe', fA, R=[('hT', i), ('Wqk', wb)], W=[('bkA', p)])

                    def fB(e):
                        ins = None
                        for k in range(KC):
                            ins = e.matmul(psB, lhsT=hT[:, k, tsl], rhs=Wvg[wb][:, k, :], start=(k == 0), stop=(k == KC - 1))
                        return ins
                    S.op('pe', fB, R=[('hT', i), ('Wvg', wb)], W=[('psB', p)])

                    return
                A4 = psA.rearrange("p (a b f) -> p a b f", a=2, b=2)
                cosb = ropet[:, i, 0:1, :].to_broadcast([128, 2, 64])
                sinb = ropet[:, i, 1:2, :].to_broadcast([128, 2, 64])
                r0, r1, r2, r3 = rt[0], rt[1], rt[2], rt[3]
                S.op('dve', lambda e: e.tensor_tensor(out=r0[:, :, 0, :], in0=A4[:, :, 0, :], in1=cosb, op=ALU.mult),
                     R=[('bkA', p), 'rope'], W=['r0a'])
                S.op('dve', lambda e: e.tensor_tensor(out=r0[:, :, 1, :], in0=A4[:, :, 1, :], in1=sinb, op=ALU.mult),
                     R=[('bkA', p), 'rope'], W=['r0b'])
                S.op('dve', lambda e: e.tensor_tensor(out=r1[:, :, 0, :], in0=A4[:, :, 0, :], in1=sinb, op=ALU.mult),
                     R=[('bkA', p), 'rope'], W=['r1a'])
                S.op('dve', lambda e: e.tensor_tensor(out=r1[:, :, 1, :], in0=A4[:, :, 1, :], in1=cosb, op=ALU.mult),
                     R=[('bkA', p), 'rope'], W=['r1b'])
                qkr4 = qk_r[p][:].rearrange("p a (b f) -> p a b f", b=2)
                S.op('dve', lambda e: e.tensor_tensor(out=qkr4[:, :, 0, :], in0=r0[:, :, 0, :], in1=r0[:, :, 1, :], op=ALU.subtract),
                     R=['r0a', 'r0b'], W=[('qk_r', p, 0)])
                S.op('dve', lambda e: e.tensor_tensor(out=qkr4[:, :, 1, :], in0=r1[:, :, 0, :], in1=r1[:, :, 1, :], op=ALU.add),
                     R=['r1a', 'r1b'], W=[('qk_r', p, 1)])
                S.op('act', lambda e: e.activation(out=v_bf[p][:], in_=psB[:, 0:256], func=AF.Copy), R=[('psB', p)], W=[('v_bf', p)])
                S.op('act', lambda e: e.activation(out=sg[p][:], in_=psB[:, 256:512], func=AF.Silu), R=[('psB', p)], W=[('sg', p)])
                tT = psT[:, 512 + p * 256:512 + (p + 1) * 256]

                def fT(e):
                    e.transpose(tT[:, 0:128], qk_r[p][:, 0, :], identb[:])
                    return e.transpose(tT[:, 128:256], qk_r[p][:, 1, :], identb[:])
                S.op('pe', fT, R=[('qk_r', p, 0), ('qk_r', p, 1), 'identb'], W=['psT'])
                S.op('act', lambda e: e.activation(out=qkT[p][:].rearrange("p a t -> p (a t)"), in_=tT, func=AF.Copy),
                     R=['psT'], W=[('qkT', p)])
                qd = cs['c_qdec_s'] if smp else cs['c_qdec']
                kdc = cs['c_kdec_s'] if smp else cs['c_kdec']
                mk = cs['c_maskT_s'] if smp else cs['c_maskT']
                S.op('dve', lambda e: e.tensor_tensor(out=qTd[p][:], in0=qkT[p][:, 0, :], in1=qd[:, h, :], op=ALU.mult),
                     R=[('qkT', p), 'c_qdec', 'c_qdec_s'], W=[('qTd', p)])
                S.op('dve', lambda e: e.tensor_scalar(out=kd[p][:], in0=qk_r[p][:, 1, :], scalar1=kdc[:, h:h + 1], scalar2=None,
                                                      op0=ALU.mult), R=[('qk_r', p, 0), ('qk_r', p, 1), 'c_kdec', 'c_kdec_s'], W=[('kd', p)])
                S.op('pe', lambda e: e.matmul(psSC, lhsT=qkT[p][:, 1, :], rhs=qkT[p][:, 0, :], start=True, stop=True),
                     R=[('qkT', p)], W=[('bkA', p)])
                S.op('dve', lambda e: e.tensor_tensor(out=sT[p][:], in0=psSC, in1=mk[:, h, :], op=ALU.mult),
                     R=[('bkA', p), 'c_maskT', 'c_maskT_s'], W=[('sT', p)])
                if not smp:
                    sbp = i % 2

                    def fO(e):
                        e.matmul(psO, lhsT=sT[p][:], rhs=v_bf[p][:], start=True, stop=False)
                        return e.matmul(psO, lhsT=qTd[p][:], rhs=Sb[sbp][:], start=False, stop=True)
                    S.op('pe', fO, R=[('sT', p), ('v_bf', p), ('qTd', p), ('Sb', sbp)], W=[('bkO', p)])
                    S.op('pe', lambda e: e.matmul(psDS, lhsT=kd[p][:], rhs=v_bf[p][:], start=True, stop=True),
                         R=[('kd', p), ('v_bf', p)], W=[('bkO', p)])
                    S.op('dve', lambda e: e.scalar_tensor_tensor(out=Sf[:], in0=Sf[:], scalar=CDEC[h], in1=psDS,
                                                                 op0=ALU.mult, op1=ALU.add), R=['Sf', ('bkO', p)], W=['Sf'])
                    S.op('act', lambda e: e.activation(out=Sb[1 - sbp][:], in_=Sf[:], func=AF.Copy), R=['Sf'], W=[('Sb', 1 - sbp)])
                    if i == NT - 2:
                        S.dma('sp', ret_p[h, :, :], Sf[:], R=['Sf'], W=['ret_p'])
                else:
                    S.op('dve', lambda e: e.tensor_tensor(out=qTd_m[:], in0=qTd[p][:].unsqueeze(1).to_broadcast([128, 16, 128]),
                                                          in1=smi_b[:], op=ALU.mult), R=[('qTd', p), 'smi_b'], W=['qTd_m'])
                    S.op('dve', lambda e: e.tensor_tensor(out=kd_m[:], in0=kd[p][:].unsqueeze(1).to_broadcast([128, 16, 128]),
                                                          in1=cs['c_smj'][:].unsqueeze(2).to_broadcast([128, 16, 128]), op=ALU.mult),
                         R=[('kd', p), 'c_smj'], W=['kd_m'])

                    def fO(e):
                        e.matmul(psO, lhsT=sT[p][:], rhs=v_bf[p][:], start=True, stop=False)
                        ins = None
                        for s in range(16):
                            ins = e.matmul(psO, lhsT=qTd_m[:, s, :], rhs=S0b[:, s, :], start=False, stop=(s == 15))
                        return ins
                    S.op('pe', fO, R=[('sT', p), ('v_bf', p), 'qTd_m', 'S0b'], W=[('bkO', p)])
                    for s2 in range(8):
                        bank6 = psF[:, 6, :]
                        if s2 % 4 == 0:
                            hf = s2 // 4
                            S.dma('sp', S0f[:], st_ret[8 * hf:8 * hf + 8, h, :, :].rearrange("s d e -> d s e"), W=['S0f'])

                        def fD(e, s2=s2):
                            e.matmul(bank6[:, 0:256], lhsT=kd_m[:, 2 * s2, :], rhs=v_bf[p][:], start=True, stop=True)
                            return e.matmul(bank6[:, 256:512], lhsT=kd_m[:, 2 * s2 + 1, :], rhs=v_bf[p][:], start=True, stop=True)
                        S.op('pe', fD, R=['kd_m', ('v_bf', p)], W=['bank6'])
                        S.op('dve', lambda e, s2=s2: e.scalar_tensor_tensor(
                            out=Snew[:, 2 * (s2 % 4):2 * (s2 % 4) + 2, :], in0=S0f[:, 2 * (s2 % 4):2 * (s2 % 4) + 2, :], scalar=CDEC_S[h],
                            in1=bank6.rearrange("p (a b) -> p a b", a=2), op0=ALU.mult, op1=ALU.add),
                            R=['S0f', 'bank6'], W=['Snew'])
                        if s2 % 4 == 3:
                            hf = s2 // 4
                            S.dma('sp', ret_s[8 * hf:8 * hf + 8, h, :, :].rearrange("s d e -> d s e"), Snew[:], R=['Snew'], W=['ret_s'])
                u = p
                S.op('act', lambda e: e.activation(out=junk[:, 0:256], in_=psO, func=AF.Square, accum_out=oss[:, u:u + 1]),
                     R=[('bkO', p)], W=['junk', ('oss', u)])
                S.op('dve', lambda e: e.tensor_scalar(out=oss[:, u:u + 1], in0=oss[:, u:u + 1], scalar1=1.0 / RDV, scalar2=EPS,
                                                      op0=ALU.mult, op1=ALU.add), R=[('oss', u)], W=[('oss', u)])
                S.op('act', lambda e: e.sqrt(out=oss[:, u:u + 1], in_=oss[:, u:u + 1]), R=[('oss', u)], W=[('oss', u)])
                S.op('dve', lambda e: e.reciprocal(out=oss[:, u:u + 1], in_=oss[:, u:u + 1]), R=[('oss', u)], W=[('oss', u)])
                S.op('dve', lambda e: e.scalar_tensor_tensor(out=on[p][:], in0=psO, scalar=oss[:, u:u + 1], in1=sg[p][:],
                                                             op0=ALU.mult, op1=ALU.mult),
                     R=[('bkO', p), ('oss', u), ('sg', p)], W=[('on', p)])
                S.dma('sp', oD[tsl, h * 256:(h + 1) * 256], on[p][:], R=[('on', p)], W=[('oD', i)])

            tileB(0, 1)
            for i in range(NT):
                if i + 1 < NT:
                    tileB(i + 1, 1)
                tileB(i, 2)

    S.barrier()
    stack[0].close()
    stack[0] = ExitStack()
    mu_bc = sb("mu_bc", [128, RWKV_PROJ], F32)
    S.dma('sp', mu_bc[:], prm[0:1, 0:RWKV_PROJ].partition_broadcast(128), W=['mu_bc'])
    shrows = sb("shrows", [128, RWKV_PROJ], F32)
    S.op('dve', lambda e: e.memset(shrows[:], 0.0), W=['shrows'])
    for s_ in range(16):
        S.dma('sp', shrows[4 * s_:4 * s_ + 1, :], st_shift[s_:s_ + 1, :], W=['shrows'])
    tmk = sb("tmk", [128, 1], F32)
    S.dma('sp', tmk[:], wc['c_tm'], W=['tmk'])
    Wz = [sb(f"Wz{i}", [128, KC, 512], BF16) for i in range(2)]
    shm = sb("shm", [128, 3, 128], F32)
    S.dma('sp', shm[:], wc['c_shift'], W=['shm'])
    zbs = [sb(f"zbs{i}", [128, 512], F32) for i in range(2)]
    dd = [sb(f"dd{i}", [128, 512], F32) for i in range(2)]
    uu = [sb(f"uu{i}", [128, 512], F32) for i in range(2)]
    c1_items = [(blk, i) for blk in range(7 if 'C1' in ST else 0) for i in range(NT)]

    def itemC1(idx, phase):
        blk, i = c1_items[idx]
        p = idx % 2
        wd_ = min(512, RWKV_PROJ - blk * 512)
        c0 = OFF_ZB + blk * 512
        wb = blk % 2
        if phase == 1 and i == 0:
            S.dma('pool', Wz[wb][:, :, 0:wd_], w_in_v[:, :, c0:c0 + wd_], W=[('Wz', wb)])
        tsl = slice(i * 128, (i + 1) * 128)
        psZ = psF[:, 2 * p, 0:wd_]
        psP = psF[:, 2 * p + 1, 0:wd_]

        if phase == 1:
            def fZ(e):
                ins = None
                for k in range(KC):
                    ins = e.matmul(psZ, lhsT=hT[:, k, tsl], rhs=Wz[wb][:, k, 0:wd_], start=(k == 0), stop=(k == KC - 1))
                return ins
            S.op('pe', fZ, R=[('hT', i), ('Wz', wb)], W=[('psZ', p)])
            return


        S.op('act', lambda e: e.activation(out=zbs[p][:, 0:wd_], in_=psZ, func=AF.Copy), R=[('psZ', p)], W=[('zbs', p)])
        carry = (0 < i < NT - 1)

        def fP(e):
            ins = e.matmul(psP, lhsT=shm[:, 1 if i == NT - 1 else 0, :], rhs=zbs[p][:, 0:wd_], start=True, stop=not carry)
            if carry:
                ins = e.matmul(psP, lhsT=shm[:, 2, :], rhs=zbs[1 - p][:, 0:wd_], start=False, stop=True)
            return ins
        S.op('pe', fP, R=[('zbs', p), ('zbs', 1 - p), 'shm'], W=[('psP', p)])
        if i == NT - 1:
            S.op('dve', lambda e: e.tensor_tensor(out=dd[p][:, 0:wd_], in0=psP, in1=shrows[:, blk * 512:blk * 512 + wd_], op=ALU.add),
                 R=[('psP', p), 'shrows'], W=[('dd', p)])
            S.op('dve', lambda e: e.tensor_tensor(out=dd[p][:, 0:wd_], in0=dd[p][:, 0:wd_], in1=zbs[p][:, 0:wd_], op=ALU.subtract),
                 R=[('dd', p), ('zbs', p)], W=[('dd', p)])
        else:
            S.op('dve', lambda e: e.tensor_tensor(out=dd[p][:, 0:wd_], in0=psP, in1=zbs[p][:, 0:wd_], op=ALU.subtract),
                 R=[('psP', p), ('zbs', p)], W=[('dd', p)])
        S.op('dve', lambda e: e.tensor_tensor(out=dd[p][:, 0:wd_], in0=dd[p][:, 0:wd_], in1=mu_bc[:, blk * 512:blk * 512 + wd_], op=ALU.mult),
             R=[('dd', p), 'mu_bc'], W=[('dd', p)])
        S.op('dve', lambda e: e.tensor_tensor(out=uu[p][:, 0:wd_], in0=dd[p][:, 0:wd_], in1=zbs[p][:, 0:wd_], op=ALU.add),
             R=[('dd', p), ('zbs', p)], W=[('uu', p)])
        S.dma('sp', uD[tsl, blk * 512:blk * 512 + wd_], uu[p][:, 0:wd_], R=[('uu', p)], W=[('uD', i)])
        if i == NT - 2:
            S.dma('sp', sh_p[0:1, blk * 512:blk * 512 + wd_], zbs[p][127:128, 0:wd_], R=[('zbs', p)], W=['sh_p'])
        if i == NT - 1:
            for s_ in range(16):
                S.dma('sp', sh_s[s_:s_ + 1, blk * 512:blk * 512 + wd_], zbs[p][4 * s_ + 3:4 * s_ + 4, 0:wd_], R=[('zbs', p)], W=['sh_s'])

    if c1_items:
        itemC1(0, 1)
    for idx in range(len(c1_items)):
        if idx + 1 < len(c1_items):
            itemC1(idx + 1, 1)
        itemC1(idx, 2)

    S.barrier()
    stack[0].close()
    stack[0] = ExitStack()
    hT_stack.close()
    NP = 4096
    O_W0, O_A0, O_KK, O_KA, O_RK, O_LW, O_LB = [1024 * j for j in range(7)]
    pb = sb("pb", [128, NP], F32)
    S.dma('sp', pb[:], prm[0:1, RWKV_PROJ:RWKV_PROJ + NP].partition_broadcast(128), W=['pb'])
    pbx = sb("pbx", [128, 1024], F32)

    def ldrow(off):
        S.dma('sp', pbx[:], prm[0:1, RWKV_PROJ + off:RWKV_PROJ + off + 1024].partition_broadcast(128), W=['pbx'])
    loraW = sb("loraW", [128, 1024], BF16)
    g2b = sb("g2b", [128, 1024], BF16)
    g2c = sb("g2c", [32, 1024], BF16)
    S.dma('pool', loraW[0:64, :], wkv_w2[:, :], W=['loraW'])
    S.dma('pool', loraW[64:128, :], wkv_a2[:, :], W=['loraW'])
    S.dma('pool', g2b[:], wkv_g2[0:128, :], W=['g2b'])
    S.dma('pool', g2c[:], wkv_g2[128:160, :], W=['g2c'])
    wcs = {}
    for k in ('c_tri', 'c_ones', 'c_m5'):
        wcs[k] = sb("s_" + k, list(WCONSTS[k].shape), BF16 if k == 'c_m5' else F32)
        S.dma('pool' if k == 'c_m5' else 'sp', wcs[k][:], wc[k], W=[k])
    smj = sb("smj2", [128, 16], F32)
    S.dma('sp', smj[:], cst['c_smj'], W=['smj2'])
    smi2 = sb("smi2", [128, 16, 128], BF16)
    S.dma('pool', smi2[:], cst['c_smi'], W=['smi2'])
    ut = sb("ut", [128, RWKV_PROJ], F32)
    lt = sb("lt", [128, 288], BF16)
    ltT = sb("ltT", [128, 3, 128], BF16)
    FA = [sb(f"FA{j}", [128, 1024], F32) for j in range(8)]
    ggb = sb("ggb", [128, 1024], BF16)
    BQ = {n: sb("BQ_" + n, [128, 1024], BF16) for n in ('at', 'bt', 'kt', 'rt', 'bh', 'kh', 'v')}
    BQ['XT'], BQ['UT'], BQ['ob'] = BQ['at'], BQ['bt'], BQ['kt']
    TQ = {n: sb("TQ_" + n, [128, 8, 128], BF16) for n in ('at', 'bt', 'kt', 'rt')}
    MS = {n: sb("MS_" + n, [128, 16, 128], BF16) for n in ('A', 'AT', 'M', 'Mbr', 'Mkr', 'P', 'IpAT')}
    gCT = sb("gCT", [128, 8, 16], F32)
    STf = sb("STf", [128, 8, 64], F32)
    STb = sb("STb", [128, 8, 64], BF16)
    st16 = sb("st16", [128, 6, 16], F32)
    S0Tf = sb("S0Tf", [128, 16, 8, 64], F32)
    som = sb("som", [64, 8, 128], F32)
    S0in = som[:].rearrange("p a b -> p (a b)")
    qm = sb("qm", [128, 16, 128], F32)
    bm = [sb(f"bm{j}", [128, 16, 128], BF16) for j in range(2)]
    S.op('dve', lambda e: e.memset(STf[:], 0.0), W=['STf'])
    S.op('dve', lambda e: e.memset(STb[:], 0.0), W=['STb'])
    for s_ in range(16 if 'C2' in ST else 0):
        S.dma('sp', S0in.rearrange("p (h j) -> p h j", h=16), st_wkv[s_].rearrange("h i j -> i h j"), W=['som'])

        def fTs(e, s_=s_):
            ins = None
            for pr in range(8):
                ins = e.transpose(psF[:, 6, pr * 64:(pr + 1) * 64], S0in[0:64, pr * 128:(pr + 1) * 128], identf[0:64, 0:64])
            return ins
        S.op('pe', fTs, R=['som', 'identf'], W=['b6'])
        S.op('act', lambda e, s_=s_: e.activation(out=S0Tf[:, s_, :, :].rearrange("p a b -> p (a b)"), in_=psF[:, 6, :], func=AF.Copy),
             R=['b6'], W=['S0Tf'])

    def V(fn, R, W):
        S.op('dve', fn, R=R, W=W)

    def A(fn, R, W):
        S.op('act', fn, R=R, W=W)

    def bank2(b):
        return psF[:, b:b + 2, :].rearrange("p a c -> p (a c)")

    def h16(ap):
        return ap.rearrange("p (h j) -> p h j", h=16)

    def bc16(col_ap):
        return col_ap.unsqueeze(2).to_broadcast([128, 16, 64])

    NC2 = int(os.environ.get('KC2N', NT))
    for i in range(NC2 if 'C2' in ST else 0):
        smp = (i == NT - 1)
        vv = 1 if smp else 0
        tsl = slice(i * 128, (i + 1) * 128)
        S.dma('sp', ut[:], uD[tsl, :], R=[('uD', i)], W=['ut'])
        r_, kx, vx = ut[:, 0:1024], ut[:, 1024:2048], ut[:, 2048:3072]
        A(lambda e: e.activation(out=lt[:, 0:64], in_=ut[:, 3072:3136], func=AF.Tanh), ['ut'], ['lt0'])
        A(lambda e: e.activation(out=lt[:, 64:128], in_=ut[:, 3136:3200], func=AF.Copy), ['ut'], ['lt1'])
        A(lambda e: e.activation(out=lt[:, 128:288], in_=ut[:, 3200:3360], func=AF.Sigmoid), ['ut'], ['lt2'])

        def fLT(e):
            e.transpose(psT[:, 0:128], lt[:, 0:128], identb[:])
            e.transpose(psT[:, 128:256], lt[:, 128:256], identb[:])
            return e.transpose(psT[0:32, 256:384], lt[:, 256:288], identb[:])
        S.op('pe', fLT, R=['lt0', 'lt1', 'lt2', 'identb'], W=['psT'])
        A(lambda e: e.activation(out=ltT[:, 0:2, :].rearrange("p a t -> p (a t)"), in_=psT[:, 0:256], func=AF.Copy), ['psT'], ['ltTa'])
        A(lambda e: e.activation(out=ltT[0:32, 2, :], in_=psT[0:32, 256:384], func=AF.Copy), ['psT'], ['ltTb'])
        pLW, pLA, pLG = bank2(0), bank2(2), bank2(4)

        def fL(e):
            for hf in range(2):
                cs_ = slice(hf * 512, (hf + 1) * 512)
                e.matmul(pLW[:, cs_], lhsT=ltT[0:64, 0, :], rhs=loraW[0:64, cs_], start=True, stop=True)
                e.matmul(pLA[:, cs_], lhsT=ltT[64:128, 0, :], rhs=loraW[64:128, cs_], start=True, stop=True)
                e.matmul(pLG[:, cs_], lhsT=ltT[:, 1, :], rhs=g2b[:, cs_], start=True, stop=False)
                ins = e.matmul(pLG[:, cs_], lhsT=ltT[0:32, 2, :], rhs=g2c[0:32, cs_], start=False, stop=True)
            return ins
        S.op('pe', fL, R=['ltTa', 'ltTb', 'loraW', 'g2b', 'g2c'], W=['b0', 'b1', 'b2', 'b3', 'b4', 'b5'])
        logw, a_s, kkn, kmod, tmpA, tmpB, egi, etd = FA
        gg = ggb
        eg = kkn
        V(lambda e: e.tensor_tensor(out=tmpA[:], in0=pLW, in1=pb[:, O_W0:O_W0 + 1024], op=ALU.add), ['b0', 'b1', 'pb'], ['tmpA'])
        A(lambda e: e.activation(out=tmpA[:], in_=tmpA[:], func=AF.Sigmoid), ['tmpA'], ['tmpA'])
        V(lambda e: e.tensor_scalar(out=logw[:], in0=tmpA[:], scalar1=-0.6065306597126334, scalar2=None, op0=ALU.mult), ['tmpA'], ['logw'])
        V(lambda e: e.tensor_tensor(out=tmpB[:], in0=pLA, in1=pb[:, O_A0:O_A0 + 1024], op=ALU.add), ['b2', 'b3', 'pb'], ['tmpB'])
        A(lambda e: e.activation(out=a_s[:], in_=tmpB[:], func=AF.Sigmoid), ['tmpB'], ['a_s'])
        A(lambda e: e.activation(out=gg[:], in_=pLG, func=AF.Copy), ['b4', 'b5'], ['gg'])
        pC, pTt = bank2(0), bank2(2)

        def fC(e):
            for hf in range(2):
                cs_ = slice(hf * 512, (hf + 1) * 512)
                e.matmul(pC[:, cs_], lhsT=wcs['c_tri'][:, vv, :], rhs=logw[:, cs_], start=True, stop=True)
                ins = e.matmul(pTt[:, cs_], lhsT=wcs['c_ones'][:, vv, :], rhs=logw[:, cs_], start=True, stop=True)
            return ins
        S.op('pe', fC, R=['logw', 'c_tri', 'c_ones'], W=['b0', 'b1', 'b2', 'b3'])
        ncol = 16 if smp else 1

        def fG(e):
            ins = None
            for pr in range(8):
                ins = e.matmul(psF[:, 6, pr * 16:pr * 16 + ncol], lhsT=logw[:, pr * 128:(pr + 1) * 128],
                               rhs=(smj[:, 0:16] if smp else wcs['c_ones'][:, 0, 0:1]), start=True, stop=True)
            return ins
        S.op('pe', fG, R=['logw', 'smj2', 'c_ones'], W=['b6'])
        A(lambda e: e.activation(out=gCT[:, :, 0:ncol], in_=psF[:, 6, 0:128].rearrange("p (a b) -> p a b", a=8)[:, :, 0:ncol], func=AF.Exp),
          ['b6'], ['gCT'])
        A(lambda e: e.activation(out=eg[:], in_=pC, func=AF.Exp), ['b0', 'b1'], ['kkn'])
        V(lambda e: e.tensor_tensor(out=BQ['rt'][:], in0=r_, in1=eg[:], op=ALU.mult), ['ut', 'kkn'], ['q_rt'])
        V(lambda e: e.tensor_scalar(out=tmpA[:], in0=pC, scalar1=-1.0, scalar2=None, op0=ALU.mult), ['b0', 'b1'], ['tmpA'])
        A(lambda e: e.activation(out=egi[:], in_=tmpA[:], func=AF.Exp), ['tmpA'], ['egi'])
        V(lambda e: e.tensor_tensor(out=tmpB[:], in0=pTt, in1=tmpA[:], op=ALU.add), ['b2', 'b3', 'tmpA'], ['tmpB'])
        A(lambda e: e.activation(out=etd[:], in_=tmpB[:], func=AF.Exp), ['tmpB'], ['etd'])
        V(lambda e: e.tensor_tensor(out=tmpB[:], in0=tmpA[:], in1=logw[:], op=ALU.add), ['tmpA', 'logw'], ['tmpB'])
        A(lambda e: e.activation(out=tmpB[:], in_=tmpB[:], func=AF.Exp, scale=-1.0), ['tmpB'], ['tmpB'])
        V(lambda e: e.tensor_tensor(out=kkn[:], in0=kx, in1=pb[:, O_KK:O_KK + 1024], op=ALU.mult), ['ut', 'pb'], ['kkn'])
        A(lambda e: e.activation(out=tmpA[:], in_=kkn[:], func=AF.Square), ['kkn'], ['tmpA'])
        V(lambda e: e.tensor_reduce(out=st16[:, 0, :], in_=h16(tmpA[:]), axis=AX.X, op=ALU.add), ['tmpA'], ['st0'])
        A(lambda e: e.sqrt(out=st16[:, 0, :], in_=st16[:, 0, :]), ['st0'], ['st0'])
        V(lambda e: e.tensor_scalar(out=st16[:, 0, :], in0=st16[:, 0, :], scalar1=1e-12, scalar2=None, op0=ALU.max), ['st0'], ['st0'])
        V(lambda e: e.reciprocal(out=st16[:, 0, :], in_=st16[:, 0, :]), ['st0'], ['st0'])
        V(lambda e: e.tensor_tensor(out=h16(kkn[:]), in0=h16(kkn[:]), in1=bc16(st16[:, 0, :]), op=ALU.mult), ['kkn', 'st0'], ['kkn'])
        V(lambda e: e.scalar_tensor_tensor(out=kmod[:], in0=a_s[:], scalar=-1.0, in1=pb[:, O_KA:O_KA + 1024], op0=ALU.add, op1=ALU.mult),
          ['a_s', 'pb'], ['kmod'])
        V(lambda e: e.scalar_tensor_tensor(out=kmod[:], in0=kmod[:], scalar=1.0, in1=kx, op0=ALU.add, op1=ALU.mult), ['kmod', 'ut'], ['kmod'])
        V(lambda e: e.tensor_tensor(out=tmpA[:], in0=r_, in1=kmod[:], op=ALU.mult), ['ut', 'kmod'], ['tmpA'])
        ldrow(O_RK)
        V(lambda e: e.tensor_tensor(out=tmpA[:], in0=tmpA[:], in1=pbx[:], op=ALU.mult), ['tmpA', 'pbx'], ['tmpA'])
        V(lambda e: e.tensor_reduce(out=st16[:, 1, :], in_=h16(tmpA[:]), axis=AX.X, op=ALU.add), ['tmpA'], ['st1'])
        V(lambda e: e.scalar_tensor_tensor(out=BQ['at'][:], in0=kkn[:], scalar=-1.0, in1=tmpB[:], op0=ALU.mult, op1=ALU.mult),
          ['kkn', 'tmpB'], ['q_at'])
        V(lambda e: e.tensor_tensor(out=tmpA[:], in0=kkn[:], in1=a_s[:], op=ALU.mult), ['kkn', 'a_s'], ['tmpA'])
        V(lambda e: e.tensor_tensor(out=BQ['bt'][:], in0=tmpA[:], in1=egi[:], op=ALU.mult), ['tmpA', 'egi'], ['q_bt'])
        V(lambda e: e.tensor_tensor(out=BQ['bh'][:], in0=tmpA[:], in1=etd[:], op=ALU.mult), ['tmpA', 'etd'], ['q_bh'])
        V(lambda e: e.tensor_tensor(out=BQ['kt'][:], in0=kmod[:], in1=egi[:], op=ALU.mult), ['kmod', 'egi'], ['q_kt'])
        V(lambda e: e.tensor_tensor(out=BQ['kh'][:], in0=kmod[:], in1=etd[:], op=ALU.mult), ['kmod', 'etd'], ['q_kh'])
        A(lambda e: e.activation(out=BQ['v'][:], in_=vx, func=AF.Copy), ['ut'], ['q_v'])
        for n in ('at', 'bt', 'kt', 'rt'):
            def fTq(e, n=n):
                ins = None
                for pr in range(8):
                    ins = e.transpose(psT[:, pr * 128:(pr + 1) * 128], BQ[n][:, pr * 128:(pr + 1) * 128], identb[:])
                return ins
            S.op('pe', fTq, R=['q_' + n, 'identb'], W=['psT'])
            A(lambda e, n=n: e.activation(out=TQ[n][:].rearrange("p a t -> p (a t)"), in_=psT[:, :], func=AF.Copy), ['psT'], ['T_' + n])
        m5 = wcs['c_m5']
        for hd in range(16):
            pr, off = hd // 2, 64 * (hd % 2)
            sl = slice(off, off + 64)
            pb_ = 4 + (hd % 2)
            p3 = psF[:, pb_, 0:384]
            p2 = psF[:, pb_, 384:512]

            def f5(e):
                e.matmul(p3[:, 0:128], lhsT=TQ['bt'][sl, pr, :], rhs=TQ['at'][sl, pr, :], start=True, stop=True)
                e.matmul(p3[:, 128:256], lhsT=TQ['at'][sl, pr, :], rhs=TQ['bt'][sl, pr, :], start=True, stop=True)
                return e.matmul(p3[:, 256:384], lhsT=TQ['kt'][sl, pr, :], rhs=TQ['at'][sl, pr, :], start=True, stop=True)
            S.op('pe', f5, R=['T_at', 'T_bt', 'T_kt'], W=[f'b{pb_}'])
            V(lambda e: e.tensor_tensor(out=MS['A'][:, hd, :], in0=p3[:, 0:128], in1=m5[:, vv, 0, :], op=ALU.mult), [f'b{pb_}', 'c_m5'], [('A', hd)])
            V(lambda e: e.tensor_tensor(out=MS['AT'][:, hd, :], in0=p3[:, 128:256], in1=m5[:, vv, 1, :], op=ALU.mult), [f'b{pb_}', 'c_m5'], [('AT', hd)])
            V(lambda e: e.tensor_tensor(out=MS['M'][:, hd, :], in0=p3[:, 256:384], in1=m5[:, vv, 2, :], op=ALU.mult), [f'b{pb_}', 'c_m5'], [('M', hd)])
            pq = psF[:, 6, (hd % 2) * 256:(hd % 2) * 256 + 256]

            def f2(e):
                e.matmul(pq[:, 0:128], lhsT=TQ['bt'][sl, pr, :], rhs=TQ['rt'][sl, pr, :], start=True, stop=True)
                return e.matmul(pq[:, 128:256], lhsT=TQ['kt'][sl, pr, :], rhs=TQ['rt'][sl, pr, :], start=True, stop=True)
            S.op('pe', f2, R=['T_bt', 'T_kt', 'T_rt'], W=['b6'])
            V(lambda e: e.tensor_tensor(out=MS['Mbr'][:, hd, :], in0=pq[:, 0:128], in1=m5[:, vv, 3, :], op=ALU.mult), ['b6', 'c_m5'], [('Mbr', hd)])
            V(lambda e: e.tensor_tensor(out=MS['Mkr'][:, hd, :], in0=pq[:, 128:256], in1=m5[:, vv, 4, :], op=ALU.mult), ['b6', 'c_m5'], [('Mkr', hd)])
        identb4 = identb[:].unsqueeze(1).to_broadcast([128, 4, 128])
        GS = [slice(4 * g, 4 * g + 4) for g in range(4)]
        GK = [[('A', hd) for hd in range(4 * g, 4 * g + 4)] for g in range(4)]
        GKT = [[('AT', hd) for hd in range(4 * g, 4 * g + 4)] for g in range(4)]
        for g in range(4):
            V(lambda e: e.tensor_tensor(out=MS['P'][:, GS[g], :], in0=MS['A'][:, GS[g], :], in1=identb4, op=ALU.add), GK[g] + ['identb'], [('P', g)])
        nlev = 2 if smp else 7
        for lev in range(1, nlev):
            last = (lev == nlev - 1)
            for g in range(4):
                gs, gk, gkT = GS[g], GK[g], GKT[g]
                bks = [f'b{2 * (g % 2)}', f'b{2 * (g % 2) + 1}']
                pa = bank2(2 * (g % 2)).rearrange("p (h a t) -> p h a t", h=4, a=2)

                def fa(e):
                    ins = None
                    for q in range(4):
                        hd = 4 * g + q
                        if not last:
                            e.matmul(pa[:, q, 0, :], lhsT=MS['AT'][:, hd, :], rhs=MS['A'][:, hd, :], start=True, stop=True)
                        ins = e.matmul(pa[:, q, 1, :], lhsT=MS['A'][:, hd, :], rhs=MS['AT'][:, hd, :], start=True, stop=True)
                    return ins
                S.op('pe', fa, R=gk + gkT, W=bks)
                V(lambda e: e.tensor_tensor(out=MS['IpAT'][:, gs, :], in0=pa[:, :, 1, :], in1=identb4, op=ALU.add), bks + ['identb'], [('IpAT', g)])
                if not last:
                    A(lambda e: e.activation(out=MS['A'][:, gs, :], in_=pa[:, :, 0, :], func=AF.Copy), bks, gk)
                    A(lambda e: e.activation(out=MS['AT'][:, gs, :], in_=pa[:, :, 1, :], func=AF.Copy), bks, gkT)
            for g in range(4):
                gs = GS[g]
                pbk = psF[:, 4 + (g % 2), :].rearrange("p (h t) -> p h t", h=4)

                def fb(e):
                    ins = None
                    for q in range(4):
                        hd = 4 * g + q
                        ins = e.matmul(pbk[:, q, :], lhsT=MS['IpAT'][:, hd, :], rhs=MS['P'][:, hd, :], start=True, stop=True)
                    return ins
                S.op('pe', fb, R=[('IpAT', g), ('P', g)], W=[f'b{4 + (g % 2)}'])
                A(lambda e: e.activation(out=MS['P'][:, gs, :], in_=pbk, func=AF.Copy), [f'b{4 + (g % 2)}'], [('P', g)])
        pX, pU, pY = bank2(0), bank2(2), bank2(4)
        Pk = [('P', g) for g in range(4)]
        Mk = [('M', hd) for hd in range(16)]
        if not smp:
            def fX(e):
                ins = None
                for hd in range(16):
                    pr, off = hd // 2, 64 * (hd % 2)
                    sl = slice(off, off + 64)
                    cs_ = slice(hd * 64, hd * 64 + 64)
                    e.matmul(pX[:, cs_], lhsT=TQ['at'][sl, pr, :], rhs=STb[sl, pr, :], start=True, stop=False)
                    ins = e.matmul(pX[:, cs_], lhsT=MS['M'][:, hd, :], rhs=BQ['v'][:, cs_], start=False, stop=True)
                return ins
            S.op('pe', fX, R=['T_at', 'STb', 'q_v'] + Mk, W=['b0', 'b1'])
        else:
            def fX(e):
                ins = None
                for hd in range(16):
                    pr, off = hd // 2, 64 * (hd % 2)
                    sl = slice(off, off + 64)
                    cs_ = slice(hd * 64, hd * 64 + 64)
                    S.op('dve', lambda e2: e2.tensor_tensor(out=qm[sl, :, :], in0=TQ['at'][sl, pr, :].unsqueeze(1).to_broadcast([64, 16, 128]),
                                                            in1=smi2[sl, :, :], op=ALU.mult), R=['T_at', 'smi2'], W=['qm'])

                    def fx1(e3):
                        for s_ in range(16):
                            e3.matmul(pX[:, cs_], lhsT=qm[sl, s_, :], rhs=S0Tf[sl, s_, pr, :], start=(s_ == 0), stop=False)
                        return e3.matmul(pX[:, cs_], lhsT=MS['M'][:, hd, :], rhs=BQ['v'][:, cs_], start=False, stop=True)
                    S.op('pe', fx1, R=['qm', 'S0Tf', 'q_v', ('M', hd)], W=['b0', 'b1'])
            fX(None)
        A(lambda e: e.activation(out=BQ['XT'][:], in_=pX, func=AF.Copy), ['b0', 'b1'], ['q_at'])

        def fU(e):
            ins = None
            for hd in range(16):
                cs_ = slice(hd * 64, hd * 64 + 64)
                ins = e.matmul(pU[:, cs_], lhsT=MS['P'][:, hd, :], rhs=BQ['XT'][:, cs_], start=True, stop=True)
            return ins
        S.op('pe', fU, R=['q_at'] + Pk, W=['b2', 'b3'])
        A(lambda e: e.activation(out=BQ['UT'][:], in_=pU, func=AF.Copy), ['b2', 'b3'], ['q_bt'])
        Mbk = [('Mbr', hd) for hd in range(16)] + [('Mkr', hd) for hd in range(16)]
        if not smp:
            def fY(e):
                ins = None
                for hd in range(16):
                    pr, off = hd // 2, 64 * (hd % 2)
                    sl = slice(off, off + 64)
                    cs_ = slice(hd * 64, hd * 64 + 64)
                    e.matmul(pY[:, cs_], lhsT=TQ['rt'][sl, pr, :], rhs=STb[sl, pr, :], start=True, stop=False)
                    e.matmul(pY[:, cs_], lhsT=MS['Mbr'][:, hd, :], rhs=BQ['UT'][:, cs_], start=False, stop=False)
                    ins = e.matmul(pY[:, cs_], lhsT=MS['Mkr'][:, hd, :], rhs=BQ['v'][:, cs_], start=False, stop=True)
                return ins
            S.op('pe', fY, R=['T_rt', 'STb', 'q_bt', 'q_v'] + Mbk, W=['b4', 'b5'])
        else:
            for hd in range(16):
                pr, off = hd // 2, 64 * (hd % 2)
                sl = slice(off, off + 64)
                cs_ = slice(hd * 64, hd * 64 + 64)
                V(lambda e: e.tensor_tensor(out=qm[sl, :, :], in0=TQ['rt'][sl, pr, :].unsqueeze(1).to_broadcast([64, 16, 128]),
                                            in1=smi2[sl, :, :], op=ALU.mult), ['T_rt', 'smi2'], ['qm'])

                def fy1(e):
                    for s_ in range(16):
                        e.matmul(pY[:, cs_], lhsT=qm[sl, s_, :], rhs=S0Tf[sl, s_, pr, :], start=(s_ == 0), stop=False)
                    e.matmul(pY[:, cs_], lhsT=MS['Mbr'][:, hd, :], rhs=BQ['UT'][:, cs_], start=False, stop=False)
                    return e.matmul(pY[:, cs_], lhsT=MS['Mkr'][:, hd, :], rhs=BQ['v'][:, cs_], start=False, stop=True)
                S.op('pe', fy1, R=['qm', 'S0Tf', 'q_bt', 'q_v', ('Mbr', hd), ('Mkr', hd)], W=['b4', 'b5'])
        if not smp:
            pS = bank2(0).rearrange("p (h c) -> p h c", h=16)

            def fS(e):
                ins = None
                for hd in range(16):
                    pr = hd // 2
                    cs_ = slice(hd * 64, hd * 64 + 64)
                    ps_ = slice(pr * 128, (pr + 1) * 128)
                    e.matmul(pS[:, hd, :], lhsT=BQ['bh'][:, ps_], rhs=BQ['UT'][:, cs_], start=True, stop=False)
                    ins = e.matmul(pS[:, hd, :], lhsT=BQ['kh'][:, ps_], rhs=BQ['v'][:, cs_], start=False, stop=True)
                return ins
            S.op('pe', fS, R=['q_bh', 'q_kh', 'q_bt', 'q_v', 'q_at'], W=['b0', 'b1'])
            for h2 in range(2):
                sl = slice(64 * h2, 64 * h2 + 64)
                V(lambda e: e.tensor_tensor(out=STf[sl, :, :], in0=STf[sl, :, :], in1=gCT[sl, :, 0:1].to_broadcast([64, 8, 64]), op=ALU.mult),
                  ['STf', 'gCT'], ['STf'])
                V(lambda e: e.tensor_tensor(out=STf[sl, :, :], in0=STf[sl, :, :],
                                            in1=pS.rearrange("p (a b) c -> p a b c", b=2)[sl, :, h2, :], op=ALU.add), ['STf', 'b0', 'b1'], ['STf'])
            A(lambda e: e.activation(out=STb[:].rearrange("p a b -> p (a b)"), in_=STf[:].rearrange("p a b -> p (a b)"), func=AF.Copy), ['STf'], ['STb'])
            if i == NT - 2:
                def fTo(e):
                    ins = None
                    for pr in range(8):
                        ins = e.transpose(bank2(2)[0:64, pr * 128:(pr + 1) * 128], STf[:, pr, :], identf[:])
                    return ins
                S.op('pe', fTo, R=['STf', 'identf'], W=['b2', 'b3'])
                A(lambda e: e.activation(out=som[:].rearrange("p a b -> p (a b)"), in_=bank2(2)[0:64, :], func=AF.Copy), ['b2', 'b3'], ['som'])
                S.dma('sp', wkv_p.rearrange("(a b) i j -> i a b j", b=2), som[:].rearrange("p a (b j) -> p a b j", b=2), R=['som'], W=['wkv_p'])
        else:
            for pr in range(8):
                ps_ = slice(pr * 128, (pr + 1) * 128)
                V(lambda e: e.tensor_tensor(out=bm[0][:], in0=BQ['bh'][:, ps_].unsqueeze(1).to_broadcast([128, 16, 128]),
                                            in1=smj[:].unsqueeze(2).to_broadcast([128, 16, 128]), op=ALU.mult), ['q_bh', 'smj2'], ['bm0'])
                V(lambda e: e.tensor_tensor(out=bm[1][:], in0=BQ['kh'][:, ps_].unsqueeze(1).to_broadcast([128, 16, 128]),
                                            in1=smj[:].unsqueeze(2).to_broadcast([128, 16, 128]), op=ALU.mult), ['q_kh', 'smj2'], ['bm1'])
                pS4 = psF[:, 0:4, :].rearrange("p a c -> p (a c)").rearrange("p (s b c) -> p s b c", s=16, b=2)

                def fSs(e):
                    ins = None
                    for s_ in range(16):
                        for h2 in range(2):
                            hd = 2 * pr + h2
                            cs_ = slice(hd * 64, hd * 64 + 64)
                            e.matmul(pS4[:, s_, h2, :], lhsT=bm[0][:, s_, :], rhs=BQ['UT'][:, cs_], start=True, stop=False)
                            ins = e.matmul(pS4[:, s_, h2, :], lhsT=bm[1][:, s_, :], rhs=BQ['v'][:, cs_], start=False, stop=True)
                    return ins
                S.op('pe', fSs, R=['bm0', 'bm1', 'q_bt', 'q_v', 'q_at'], W=['b0', 'b1', 'b2', 'b3'])
                for h2 in range(2):
                    sl = slice(64 * h2, 64 * h2 + 64)
                    V(lambda e: e.tensor_tensor(out=S0Tf[sl, :, pr, :], in0=S0Tf[sl, :, pr, :],
                                                in1=gCT[sl, pr, :].unsqueeze(2).to_broadcast([64, 16, 64]), op=ALU.mult),
                      ['S0Tf', 'gCT'], ['S0Tf'])
                    V(lambda e: e.tensor_tensor(out=S0Tf[sl, :, pr, :], in0=S0Tf[sl, :, pr, :], in1=pS4[sl, :, h2, :], op=ALU.add),
                      ['S0Tf', 'b0', 'b1', 'b2', 'b3'], ['S0Tf'])
            for s_ in range(16):
                def fTo(e):
                    ins = None
                    for pr in range(8):
                        ins = e.transpose(bank2(0)[0:64, pr * 128:(pr + 1) * 128], S0Tf[:, s_, pr, :], identf[:])
                    return ins
                S.op('pe', fTo, R=['S0Tf', 'identf'], W=['b0', 'b1'])
                A(lambda e: e.activation(out=som[:].rearrange("p a b -> p (a b)"), in_=bank2(0)[0:64, :], func=AF.Copy), ['b0', 'b1'], ['som'])
                S.dma('sp', wkv_s[s_].rearrange("(a b) i j -> i a b j", b=2), som[:].rearrange("p a (b j) -> p a b j", b=2), R=['som'], W=['wkv_s'])
        ysb, ysq = tmpA, tmpB
        A(lambda e: e.activation(out=ysb[:], in_=pY, func=AF.Copy), ['b4', 'b5'], ['tmpA'])
        A(lambda e: e.activation(out=ysq[:], in_=pY, func=AF.Square), ['b4', 'b5'], ['tmpB'])
        V(lambda e: e.tensor_reduce(out=st16[:, 2, :], in_=h16(ysb[:]), axis=AX.X, op=ALU.add), ['tmpA'], ['st2'])
        V(lambda e: e.tensor_reduce(out=st16[:, 3, :], in_=h16(ysq[:]), axis=AX.X, op=ALU.add), ['tmpB'], ['st3'])
        V(lambda e: e.tensor_scalar(out=st16[:, 2, :], in0=st16[:, 2, :], scalar1=1.0 / 64, scalar2=None, op0=ALU.mult), ['st2'], ['st2'])
        V(lambda e: e.tensor_tensor(out=st16[:, 4, :], in0=st16[:, 2, :], in1=st16[:, 2, :], op=ALU.mult), ['st2'], ['st4'])
        V(lambda e: e.scalar_tensor_tensor(out=st16[:, 3, :], in0=st16[:, 3, :], scalar=1.0 / 64, in1=st16[:, 4, :], op0=ALU.mult, op1=ALU.subtract),
          ['st3', 'st4'], ['st3'])
        V(lambda e: e.tensor_scalar(out=st16[:, 3, :], in0=st16[:, 3, :], scalar1=64e-5, scalar2=None, op0=ALU.add), ['st3'], ['st3'])
        A(lambda e: e.sqrt(out=st16[:, 3, :], in_=st16[:, 3, :]), ['st3'], ['st3'])
        V(lambda e: e.reciprocal(out=st16[:, 3, :], in_=st16[:, 3, :]), ['st3'], ['st3'])
        V(lambda e: e.tensor_tensor(out=h16(ysb[:]), in0=h16(ysb[:]), in1=bc16(st16[:, 2, :]), op=ALU.subtract), ['tmpA', 'st2'], ['tmpA'])
        V(lambda e: e.tensor_tensor(out=h16(ysb[:]), in0=h16(ysb[:]), in1=bc16(st16[:, 3, :]), op=ALU.mult), ['tmpA', 'st3'], ['tmpA'])
        ldrow(O_LW)
        V(lambda e: e.tensor_tensor(out=ysb[:], in0=ysb[:], in1=pbx[:], op=ALU.mult), ['tmpA', 'pbx'], ['tmpA'])
        ldrow(O_LB)
        V(lambda e: e.tensor_tensor(out=ysb[:], in0=ysb[:], in1=pbx[:], op=ALU.add), ['tmpA', 'pbx'], ['tmpA'])
        V(lambda e: e.tensor_tensor(out=h16(ysq[:]), in0=h16(vx), in1=bc16(st16[:, 1, :]), op=ALU.mult), ['ut', 'st1', 'tmpB'], ['tmpB'])
        V(lambda e: e.tensor_tensor(out=ysb[:], in0=ysb[:], in1=ysq[:], op=ALU.add), ['tmpA', 'tmpB'], ['tmpA'])
        V(lambda e: e.tensor_tensor(out=BQ['ob'][:], in0=ysb[:], in1=gg[:], op=ALU.mult), ['tmpA', 'gg'], ['q_kt'])
        S.dma('sp', obD[tsl, :], BQ['ob'][:], R=['q_kt'], W=[('obD', i)])

    S.barrier()
    stack[0].close()
    stack[0] = ExitStack()
    w_oa_v = w_oa.rearrange("(k p) n -> p k n", p=128)
    w_ob_v = w_ob.rearrange("(k p) n -> p k n", p=128)
    w_out_v = w_out.rearrange("(k p) n -> p k n", p=128)
    junk = sb("junkD", [128, D], BF16)
    ss = sb("ssD", [128, 4], F32)
    xg = [sb(f"xg{j}", [128, D], F32) for j in range(4)]
    hbD = [sb(f"hbD{j}", [128, D], BF16) for j in range(2)]
    ost = [sb(f"ost{j}", [128, D], BF16) for j in range(2)]
    hTg = sb("hTg", [128, KC, 512], BF16)
    oTg = sb("oTg", [128, KC, 512], BF16)
    obTg = sb("obTg", [128, 8, 512], BF16)
    mtile = [sb(f"mtile{j}", [128, D], BF16) for j in range(4)]
    WA = [sb(f"WA{j}", [128, KC, 512], BF16) for j in range(4)]
    WB = [sb(f"WB{j}", [128, 8, 512], BF16) for j in range(2)]
    sgA = [sb(f"sgA{j}", [128, 512], F32) for j in range(2)]
    sgB = [sb(f"sgB{j}", [128, 512], F32) for j in range(2)]
    ridx_sb = sb("ridx_sb", [128, NTL], I32)
    S.dma('sp', ridx_sb[:], ridx[:, :], W=['ridx_sb'])
    wa_i = [0]
    wb_i = [0]

    def nextWA():
        j = wa_i[0] % 4
        wa_i[0] += 1
        return j

    def mm16(ps, lhsT_fn, W_, nk=KC):
        def f(e):
            ins = None
            for k in range(nk):
                ins = e.matmul(ps, lhsT=lhsT_fn(k), rhs=W_[:, k, :], start=(k == 0), stop=(k == nk - 1))
            return ins
        return f

    groups = [(0, 4), (4, 4), (8, 1)]
    def loadD(blk):
        cs_ = slice(blk * 512, (blk + 1) * 512)
        a1, a2, a3 = nextWA(), nextWA(), nextWA()
        b1 = wb_i[0] % 2
        wb_i[0] += 1
        S.dma('pool', WA[a1][:], w_oa_v[:, :, cs_], W=[('WA', a1)])
        S.dma('pool', WB[b1][:], w_ob_v[:, :, cs_], W=[('WB', b1)])
        S.dma('pool', WA[a2][:], w_in_v[:, :, OFF_G + blk * 512:OFF_G + (blk + 1) * 512], W=[('WA', a2)])
        S.dma('pool', WA[a3][:], w_in_v[:, :, OFF_G + D + blk * 512:OFF_G + D + (blk + 1) * 512], W=[('WA', a3)])
        return a1, a2, a3, b1

    for (i0, n) in (groups if 'D' in ST else []):
        pre0 = loadD(0)
        for j in range(n):
            i = i0 + j
            S.dma('sp', xg[j][:], x_h[i * 128:(i + 1) * 128, :], W=[('xg', j)])
            rmsnorm_tile(xg[j][:], hbD[j % 2][:], gbc[:], ('xg', j), ('hbD', j % 2), j % 2)
            transpose_to(hTg, hbD[j % 2], ('hbD', j % 2), 'hTg', j)
            S.dma('pool', None, None, R=['ridx_sb'], W=[('ost', 0)],
                  fn=lambda e: e.indirect_dma_start(out=ost[0][:, :], out_offset=None, in_=oD[:, :],
                                                    in_offset=bass.IndirectOffsetOnAxis(ap=ridx_sb[:, i:i + 1], axis=0)))
            transpose_to(oTg, ost[0], ('ost', 0), 'oTg', j)
            S.dma('pool', None, None, R=['ridx_sb'], W=[('ost', 1)],
                  fn=lambda e: e.indirect_dma_start(out=ost[1][:, 0:1024], out_offset=None, in_=obD[:, :],
                                                    in_offset=bass.IndirectOffsetOnAxis(ap=ridx_sb[:, i:i + 1], axis=0)))
            transpose_to(obTg, ost[1], ('ost', 1), 'obTg', j, nk=8)
        for blk in range(4):
            cs_ = slice(blk * 512, (blk + 1) * 512)
            a1, a2, a3, b1 = pre0 if blk == 0 else loadD(blk)
            for j in range(n):
                q = j % 2
                tj = slice(j * 128, (j + 1) * 128)
                pGA, pGB, pYA, pYB = psF[:, q, :], psF[:, 2 + q, :], psF[:, 4 + q, :], psF[:, 6, :]
                S.op('pe', mm16(pGA, lambda k: hTg[:, k, tj], WA[a2]), R=[('hTg', j), ('WA', a2)], W=[f'b{q}'])
                S.op('pe', mm16(pGB, lambda k: hTg[:, k, tj], WA[a3]), R=[('hTg', j), ('WA', a3)], W=[f'b{2 + q}'])
                S.op('pe', mm16(pYA, lambda k: oTg[:, k, tj], WA[a1]), R=[('oTg', j), ('WA', a1)], W=[f'b{4 + q}'])
                S.op('pe', mm16(pYB, lambda k: obTg[:, k, tj], WB[b1], nk=8), R=[('obTg', j), ('WB', b1)], W=['b6'])
                S.op('act', lambda e: e.activation(out=sgA[q][:], in_=pGA, func=AF.Sigmoid), R=[f'b{q}'], W=[('sgA', q)])
                S.op('act', lambda e: e.activation(out=sgB[q][:], in_=pGB, func=AF.Sigmoid), R=[f'b{2 + q}'], W=[('sgB', q)])
                S.op('dve', lambda e: e.tensor_tensor(out=sgA[q][:], in0=sgA[q][:], in1=pYA, op=ALU.mult), R=[('sgA', q), f'b{4 + q}'], W=[('sgA', q)])
                S.op('dve', lambda e: e.tensor_tensor(out=sgB[q][:], in0=sgB[q][:], in1=pYB, op=ALU.mult), R=[('sgB', q), 'b6'], W=[('sgB', q)])
                S.op('dve', lambda e: e.tensor_tensor(out=mtile[j][:, cs_], in0=sgA[q][:], in1=sgB[q][:], op=ALU.add),
                     R=[('sgA', q), ('sgB', q)], W=[('mtile', j)])
        for j in range(n):
            transpose_to(oTg, mtile[j], ('mtile', j), 'oTg', j)
        for blk in range(4):
            cs_ = slice(blk * 512, (blk + 1) * 512)
            a1 = nextWA()
            S.dma('pool', WA[a1][:], w_out_v[:, :, cs_], W=[('WA', a1)])
            for j in range(n):
                q = j % 2
                tj = slice(j * 128, (j + 1) * 128)
                S.op('pe', mm16(psF[:, q, :], lambda k: oTg[:, k, tj], WA[a1]), R=[('oTg', j), ('WA', a1)], W=[f'b{q}'])
                S.op('dve', lambda e: e.tensor_tensor(out=xg[j][:, cs_], in0=xg[j][:, cs_], in1=psF[:, q, :], op=ALU.add),
                     R=[('xg', j), f'b{q}'], W=[('xg', j)])
        for j in range(n):
            i = i0 + j
            S.dma('sp', x1D[i * 128:(i + 1) * 128, :], xg[j][:], R=[('xg', j)], W=[('x1D', i)])

    S.barrier()
    stack[0].close()
    stack[0] = None
    slots_i = sb("slots_i", [128, NTL, 2], I32)
    wts = sb("wts", [128, NTL, 2], F32)
    stack[0] = ExitStack()
    junk = sb("junkE", [128, D], BF16)
    ss = sb("ssE", [128, 4], F32)
    S.dma('sp', gbc[:], g_ffn[0:1, :].partition_broadcast(128), W=['gbc'])
    wr_sb = sb("wr_sb", [128, KC, 36], F32)
    S.dma('sp', wr_sb[:], wr.rearrange("(k p) n -> p k n", p=128), W=['wr_sb'])
    rb_bc = sb("rb_bc", [128, 36], F32)
    S.dma('sp', rb_bc[:], rbias[0:1, :].partition_broadcast(128), W=['rb_bc'])
    rc = {}
    for k in ('c_slt', 'c_ecap', 'c_valid', 'c_trash'):
        rc[k] = sb("s_" + k, list(RCONSTS[k].shape), F32)
        S.dma('sp', rc[k][:], rcd[k], W=[k])
    ones_f = sb("ones_f", [128, 128], F32)
    S.op('dve', lambda e: e.memset(ones_f[:], 1.0), W=['ones_f'])
    base = sb("base", [128, 32], F32)
    S.op('dve', lambda e: e.memset(base[:], 0.0), W=['base'])
    xr = [sb(f"xr{j}", [128, D], F32) for j in range(2)]
    h2f = sb("h2f", [128, D], F32)
    h2b = [sb(f"h2b{j}", [128, D], BF16) for j in range(2)]
    h2T = sb("h2T", [128, KC, 128], F32)
    lg = sb("lg", [128, 36], F32)
    sm = sb("sm", [128, 16], F32)
    gm = sb("gm", [128, 4], F32)
    elm = sb("elm", [128, 32], F32)
    elm2 = sb("elm2", [128, 32], F32)
    oh0 = sb("oh0", [128, 32], F32)
    oh1 = sb("oh1", [128, 32], F32)
    ohs = sb("ohs", [128, 32], F32)
    cb = sb("cb", [128, 32], F32)
    cb2 = sb("cb2", [128, 32], F32)
    t32 = sb("t32", [128, 32], F32)

    def V(fn, R, W):
        S.op('dve', fn, R=R, W=W)

    def A(fn, R, W):
        S.op('act', fn, R=R, W=W)

    S.op('dve', lambda e: e.memset(h2b[0][:], 0.0), W=[('h2b', 0)])
    XeD_v = XeD.rearrange("(n p) d -> p n d", p=128)
    for z0 in range(0, NE * CAP // 128 + 1, 11):
        z1 = min(z0 + 11, NE * CAP // 128 + 1)
        S.dma('sp', XeD_v[:, z0:z1, :], h2b[0][:].unsqueeze(1).to_broadcast([128, z1 - z0, D]), R=[('h2b', 0)], W=['XeD'])
    for i in range(NTL if 'E0' in ST else 0):
        q = i % 2
        S.dma('sp', xr[q][:], x1D[i * 128:(i + 1) * 128, :], R=[('x1D', i)], W=[('xr', q)])
        rmsnorm_tile(xr[q][:], h2f[:], gbc[:], ('xr', q), 'h2f', q)
        A(lambda e: e.activation(out=h2b[q][:], in_=h2f[:], func=AF.Copy), ['h2f'], [('h2b', q)])
        for half in range(4):
            def fT4(e, half=half):
                ins = None
                for k in range(4):
                    kk_ = half * 4 + k
                    ins = e.transpose(psF[:, half % 2, k * 128:(k + 1) * 128], h2f[:, kk_ * 128:(kk_ + 1) * 128], identf[:])
                return ins
            S.op('pe', fT4, R=['h2f', 'identf'], W=[f'b{half % 2}'])
            A(lambda e, half=half: e.activation(out=h2T[:, half * 4:half * 4 + 4, :].rearrange("p a t -> p (a t)"), in_=psF[:, half % 2, :], func=AF.Copy),
              [f'b{half % 2}'], ['h2T'])
        pR = psF[:, 2, 0:36]

        def fR(e):
            ins = None
            for k in range(KC):
                ins = e.matmul(pR, lhsT=h2T[:, k, :], rhs=wr_sb[:, k, :], start=(k == 0), stop=(k == KC - 1))
            return ins
        S.op('pe', fR, R=['h2T', 'wr_sb'], W=['b2'])
        V(lambda e: e.tensor_tensor(out=lg[:], in0=pR, in1=rb_bc[:], op=ALU.add), ['b2', 'rb_bc'], ['lg'])
        V(lambda e: e.tensor_reduce(out=sm[:, 0:1], in_=lg[:, 0:4], axis=AX.X, op=ALU.max), ['lg'], ['sm0'])
        V(lambda e: e.tensor_scalar(out=gm[:], in0=lg[:, 0:4], scalar1=sm[:, 0:1], scalar2=None, op0=ALU.subtract), ['lg', 'sm0'], ['gm'])
        A(lambda e: e.activation(out=t32[:, 0:4], in_=gm[:], func=AF.Exp), ['gm'], ['t32'])
        V(lambda e: e.tensor_reduce(out=sm[:, 1:2], in_=t32[:, 0:4], axis=AX.X, op=ALU.add), ['t32'], ['sm1'])
        V(lambda e: e.reciprocal(out=sm[:, 1:2], in_=sm[:, 1:2]), ['sm1'], ['sm1'])
        V(lambda e: e.tensor_scalar(out=gm[:], in0=gm[:], scalar1=0.0, scalar2=None, op0=ALU.is_equal), ['gm'], ['gm'])
        V(lambda e: e.tensor_scalar(out=gm[:], in0=gm[:], scalar1=-1.0, scalar2=1e30, op0=ALU.add, op1=ALU.mult), ['gm'], ['gm'])
        V(lambda e: e.tensor_tensor(out=elm[:].rearrange("p (g x) -> p g x", g=4), in0=lg[:, 4:36].rearrange("p (g x) -> p g x", g=4),
                                    in1=gm[:].unsqueeze(2).to_broadcast([128, 4, 8]), op=ALU.add), ['lg', 'gm'], ['elm'])
        V(lambda e: e.tensor_reduce(out=sm[:, 2:3], in_=elm[:], axis=AX.X, op=ALU.max), ['elm'], ['sm2'])
        V(lambda e: e.tensor_scalar(out=oh0[:], in0=elm[:], scalar1=sm[:, 2:3], scalar2=None, op0=ALU.is_equal), ['elm', 'sm2'], ['oh0'])
        V(lambda e: e.scalar_tensor_tensor(out=elm2[:], in0=oh0[:], scalar=-1e30, in1=elm[:], op0=ALU.mult, op1=ALU.add), ['oh0', 'elm'], ['elm2'])
        V(lambda e: e.tensor_reduce(out=sm[:, 3:4], in_=elm2[:], axis=AX.X, op=ALU.max), ['elm2'], ['sm3'])
        V(lambda e: e.tensor_scalar(out=oh1[:], in0=elm2[:], scalar1=sm[:, 3:4], scalar2=None, op0=ALU.is_equal), ['elm2', 'sm3'], ['oh1'])
        V(lambda e: e.tensor_tensor(out=sm[:, 4:5], in0=sm[:, 3:4], in1=sm[:, 2:3], op=ALU.subtract), ['sm2', 'sm3'], ['sm4'])
        A(lambda e: e.activation(out=sm[:, 4:5], in_=sm[:, 4:5], func=AF.Exp), ['sm4'], ['sm4'])
        V(lambda e: e.tensor_scalar(out=sm[:, 5:6], in0=sm[:, 4:5], scalar1=1.0, scalar2=None, op0=ALU.add), ['sm4'], ['sm5'])
        V(lambda e: e.reciprocal(out=sm[:, 5:6], in_=sm[:, 5:6]), ['sm5'], ['sm5'])
        V(lambda e: e.tensor_tensor(out=wts[:, i, 0:1], in0=sm[:, 5:6], in1=sm[:, 1:2], op=ALU.mult), ['sm5', 'sm1'], [('wts', i)])
        V(lambda e: e.tensor_tensor(out=wts[:, i, 1:2], in0=wts[:, i, 0:1], in1=sm[:, 4:5], op=ALU.mult), [('wts', i), 'sm4'], [('wts', i)])
        vcol = rc['c_valid'][:, 1:2] if i == NTL - 1 else rc['c_valid'][:, 0:1]
        V(lambda e: e.tensor_tensor(out=ohs[:], in0=oh0[:], in1=oh1[:], op=ALU.add), ['oh0', 'oh1'], ['ohs'])
        V(lambda e: e.tensor_scalar(out=ohs[:], in0=ohs[:], scalar1=vcol, scalar2=None, op0=ALU.mult), ['ohs', 'c_valid'], ['ohs'])
        pP, pTot = psF[:, 3, 0:32], psF[:, 3, 64:96]

        def fP2(e):
            e.matmul(pP, lhsT=rc['c_slt'][:], rhs=ohs[:], start=True, stop=True)
            return e.matmul(pTot, lhsT=ones_f[:], rhs=ohs[:], start=True, stop=True)
        S.op('pe', fP2, R=['ohs', 'c_slt', 'ones_f'], W=['b3'])
        V(lambda e: e.tensor_tensor(out=cb[:], in0=pP, in1=base[:], op=ALU.add), ['b3', 'base'], ['cb'])
        V(lambda e: e.tensor_tensor(out=base[:], in0=base[:], in1=pTot, op=ALU.add), ['b3', 'base'], ['base'])
        V(lambda e: e.tensor_tensor(out=cb2[:], in0=cb[:], in1=rc['c_ecap'][:], op=ALU.add), ['cb', 'c_ecap'], ['cb2'])
        for k_, oh_ in ((0, oh0), (1, oh1)):
            c0_ = 6 + 4 * k_
            V(lambda e: e.tensor_tensor(out=t32[:], in0=oh_[:], in1=cb[:], op=ALU.mult), ['oh0', 'oh1', 'cb', 't32'], ['t32'])
            V(lambda e: e.tensor_reduce(out=sm[:, c0_:c0_ + 1], in_=t32[:], axis=AX.X, op=ALU.add), ['t32'], [('smk', k_)])
            V(lambda e: e.tensor_tensor(out=t32[:], in0=oh_[:], in1=cb2[:], op=ALU.mult), ['oh0', 'oh1', 'cb2', 't32'], ['t32'])
            V(lambda e: e.tensor_reduce(out=sm[:, c0_ + 1:c0_ + 2], in_=t32[:], axis=AX.X, op=ALU.add), ['t32'], [('smk', k_)])
            V(lambda e: e.tensor_scalar(out=sm[:, c0_:c0_ + 1], in0=sm[:, c0_:c0_ + 1], scalar1=float(CAP) - 0.5, scalar2=None,
                                        op0=ALU.is_lt), [('smk', k_)], [('smk', k_)])
            V(lambda e: e.tensor_tensor(out=sm[:, c0_:c0_ + 1], in0=sm[:, c0_:c0_ + 1], in1=vcol, op=ALU.mult), [('smk', k_), 'c_valid'], [('smk', k_)])
            V(lambda e: e.tensor_tensor(out=sm[:, c0_ + 1:c0_ + 2], in0=sm[:, c0_ + 1:c0_ + 2], in1=rc['c_trash'][:, 0:1], op=ALU.subtract),
              [('smk', k_), 'c_trash'], [('smk', k_)])
            V(lambda e: e.tensor_tensor(out=sm[:, c0_ + 1:c0_ + 2], in0=sm[:, c0_ + 1:c0_ + 2], in1=sm[:, c0_:c0_ + 1], op=ALU.mult), [('smk', k_)], [('smk', k_)])
            V(lambda e: e.tensor_tensor(out=sm[:, c0_ + 1:c0_ + 2], in0=sm[:, c0_ + 1:c0_ + 2], in1=rc['c_trash'][:, 0:1], op=ALU.add),
              [('smk', k_), 'c_trash'], [('smk', k_)])
            V(lambda e: e.tensor_copy(out=slots_i[:, i, k_:k_ + 1], in_=sm[:, c0_ + 1:c0_ + 2]), [('smk', k_)], [('slots', i, k_)])
            S.dma('pool', None, None, R=[('h2b', q), ('slots', i, k_)], W=['XeD'],
                  fn=lambda e: e.indirect_dma_start(out=XeD[:, :], out_offset=bass.IndirectOffsetOnAxis(ap=slots_i[:, i, k_:k_ + 1], axis=0),
                                                    in_=h2b[q][:, :], in_offset=None))

    S.barrier()
    stack[0].close()
    stack[0] = ExitStack()
    eg_v = e_gate.rearrange("e (k p) n -> e p k n", p=128)
    eu_v = e_up.rearrange("e (k p) n -> e p k n", p=128)
    ed_v = e_down.rearrange("e (k p) n -> e p k n", p=128)
    WE = [sb(f"WE{j}", [128, KC, 512], BF16) for j in range(9)]
    xe = [sb(f"xe{j}", [128, D], BF16) for j in range(2)]
    XeT = sb("XeT", [128, KC, CAP], BF16)
    actT = sb("actT", [128, 8, CAP], BF16)
    sl_ = [sb(f"sl{j}", [128, CAP], F32) for j in range(2)]
    ye = [sb(f"ye{j}", [128, D], F32) for j in range(2)]
    we_i = [0]

    def nextWE():
        j = we_i[0] % 9
        we_i[0] += 1
        return j

    cnt = 0
    for ex in range(NE if 'E' in ST else 0):
        for t2 in range(CAP // 128):
            S.dma('sp', xe[t2][:], XeD[ex * CAP + t2 * 128:ex * CAP + (t2 + 1) * 128, :], R=['XeD'], W=[('xe', t2)])
            transpose_to(XeT, xe[t2], ('xe', t2), 'XeT', t2)
        for fh in range(2):
            wg, wu = nextWE(), nextWE()
            S.dma('pool', WE[wg][:], eg_v[ex, :, :, fh * 512:(fh + 1) * 512], W=[('WE', wg)])
            S.dma('pool', WE[wu][:], eu_v[ex, :, :, fh * 512:(fh + 1) * 512], W=[('WE', wu)])
            for fc in range(4):
                q = cnt % 2
                cnt += 1
                pG, pU_ = psF[:, q, 0:CAP], psF[:, q, 256:256 + CAP]

                def fGU(e):
                    for k in range(KC):
                        e.matmul(pG, lhsT=WE[wg][:, k, fc * 128:(fc + 1) * 128], rhs=XeT[:, k, :], start=(k == 0), stop=(k == KC - 1))
                    ins = None
                    for k in range(KC):
                        ins = e.matmul(pU_, lhsT=WE[wu][:, k, fc * 128:(fc + 1) * 128], rhs=XeT[:, k, :], start=(k == 0), stop=(k == KC - 1))
                    return ins
                S.op('pe', fGU, R=[('XeT', t_) for t_ in range(CAP // 128)] + [('WE', wg), ('WE', wu)], W=[f'b{q}'])
                A(lambda e: e.activation(out=sl_[q][:], in_=pG, func=AF.Silu), [f'b{q}'], [('sl', q)])
                V(lambda e: e.tensor_tensor(out=actT[:, fh * 4 + fc, :], in0=sl_[q][:], in1=pU_, op=ALU.mult), [('sl', q), f'b{q}'], ['actT'])
        wd0, wd1 = nextWE(), nextWE()
        S.dma('pool', WE[wd0][:, 0:8, :], ed_v[ex, :, :, 0:512], W=[('WE', wd0)])
        S.dma('pool', WE[wd0][:, 8:16, :], ed_v[ex, :, :, 512:1024], W=[('WE', wd0)])
        S.dma('pool', WE[wd1][:, 0:8, :], ed_v[ex, :, :, 1024:1536], W=[('WE', wd1)])
        S.dma('pool', WE[wd1][:, 8:16, :], ed_v[ex, :, :, 1536:2048], W=[('WE', wd1)])
        for t2 in range(CAP // 128):
            for cbk in range(4):
                wsel = WE[wd0] if cbk < 2 else WE[wd1]
                wkey = ('WE', wd0) if cbk < 2 else ('WE', wd1)
                ko = 8 * (cbk % 2)
                q = cnt % 2
                cnt += 1
                pD = psF[:, 2 + q, :]

                def fDn(e):
                    ins = None
                    for k in range(8):
                        ins = e.matmul(pD, lhsT=actT[:, k, t2 * 128:(t2 + 1) * 128], rhs=wsel[:, ko + k, :], start=(k == 0), stop=(k == 7))
                    return ins
                S.op('pe', fDn, R=['actT', wkey], W=[f'b{2 + q}'])
                A(lambda e: e.activation(out=ye[t2][:, cbk * 512:(cbk + 1) * 512], in_=pD, func=AF.Copy), [f'b{2 + q}'], [('ye', t2)])
            S.dma('sp', YeD[ex * CAP + t2 * 128:ex * CAP + (t2 + 1) * 128, :], ye[t2][:], R=[('ye', t2)], W=['YeD'])

    S.barrier()
    stack[0].close()
    stack[0] = ExitStack()
    junk = sb("junkF", [128, D], BF16)
    ss = sb("ssF", [128, 4], F32)
    gfin = sb("gfin", [128, D], F32)
    S.dma('sp', gbc[:], g_ple[0:1, :].partition_broadcast(128), W=['gbc'])
    S.dma('sp', gfin[:], g_final[0:1, :].partition_broadcast(128), W=['gfin'])
    wp_v = w_ple_gate.rearrange("(k p) n -> p k n", p=128)
    wpp_v = w_ple_proj.rearrange("(k p) n -> p k n", p=128)
    xg = [sb(f"xf{j}", [128, D], F32) for j in range(4)]
    yg = [sb(f"yg{j}", [128, D], F32) for j in range(2)]
    hbF = [sb(f"hbF{j}", [128, D], BF16) for j in range(2)]
    h3T = sb("h3T", [128, KC, 512], BF16)
    pt = sb("pt", [128, DPLE], F32)
    ptb = sb("ptb", [128, DPLE], BF16)
    pT = sb("pT", [128, 2, 512], BF16)
    WP = [sb(f"WP{j}", [128, KC, 512], BF16) for j in range(2)]
    WPP = [sb(f"WPP{j}", [128, 2, 512], BF16) for j in range(2)]
    sgF = [sb(f"sgF{j}", [128, 512], F32) for j in range(2)]
    yout = [sb(f"yout{j}", [128, D], F32) for j in range(2)]
    wcnt = 0
    V(lambda e: e.memset(yg[0][:], 0.0), [], [('yg', 0)])
    S.dma('sp', YeD[NSLOT:NSLOT + 128, :], yg[0][:], R=[('yg', 0)], W=['YeD'])
    def loadF(blk):
        nonlocal_w = wcnt_box[0] % 2
        wcnt_box[0] += 1
        cs_ = slice(blk * 512, (blk + 1) * 512)
        S.dma('pool', WP[nonlocal_w][:], wp_v[:, :, cs_], W=[('WP', nonlocal_w)])
        S.dma('pool', WPP[nonlocal_w][:], wpp_v[:, :, cs_], W=[('WPP', nonlocal_w)])
        return nonlocal_w

    wcnt_box = [0]
    for (i0, n) in (groups if 'F' in ST else []):
        preF = loadF(0)
        for j in range(n):
            i = i0 + j
            S.dma('sp', xg[j][:], x1D[i * 128:(i + 1) * 128, :], R=[('x1D', i)], W=[('xf', j)])
            for k_ in range(2):
                V(lambda e: e.memset(yg[k_][:], 0.0), [], [('yg', k_)])
                S.dma('pool', None, None, R=['YeD', ('slots', i, k_)], W=[('yg', k_)],
                      fn=lambda e: e.indirect_dma_start(out=yg[k_][:, :], out_offset=None, in_=YeD[:, :],
                                                        in_offset=bass.IndirectOffsetOnAxis(ap=slots_i[:, i, k_:k_ + 1], axis=0)))
                V(lambda e: e.scalar_tensor_tensor(out=xg[j][:], in0=yg[k_][:], scalar=wts[:, i, k_:k_ + 1], in1=xg[j][:],
                                                   op0=ALU.mult, op1=ALU.add), [('yg', k_), ('wts', i), ('xf', j)], [('xf', j)])
            rmsnorm_tile(xg[j][:], hbF[j % 2][:], gbc[:], ('xf', j), ('hbF', j % 2), j % 2)
            transpose_to(h3T, hbF[j % 2], ('hbF', j % 2), 'h3T', j)
            S.dma('sp', pt[:], p_in[i * 128:(i + 1) * 128, :], W=['pt'])
            A(lambda e: e.activation(out=ptb[:], in_=pt[:], func=AF.Copy), ['pt'], ['ptb'])
            transpose_to(pT, ptb, 'ptb', 'pT', j, nk=2)
        for blk in range(4):
            cs_ = slice(blk * 512, (blk + 1) * 512)
            w1 = preF if blk == 0 else loadF(blk)
            for j in range(n):
                q = j % 2
                tj = slice(j * 128, (j + 1) * 128)
                S.op('pe', mm16(psF[:, q, :], lambda k: h3T[:, k, tj], WP[w1]), R=[('h3T', j), ('WP', w1)], W=[f'b{q}'])
                S.op('pe', mm16(psF[:, 2 + q, :], lambda k: pT[:, k, tj], WPP[w1], nk=2), R=[('pT', j), ('WPP', w1)], W=[f'b{2 + q}'])
                A(lambda e: e.activation(out=sgF[q][:], in_=psF[:, q, :], func=AF.Sigmoid), [f'b{q}'], [('sgF', q)])
                V(lambda e: e.tensor_tensor(out=sgF[q][:], in0=sgF[q][:], in1=psF[:, 2 + q, :], op=ALU.mult), [('sgF', q), f'b{2 + q}'], [('sgF', q)])
                V(lambda e: e.tensor_tensor(out=xg[j][:, cs_], in0=xg[j][:, cs_], in1=sgF[q][:], op=ALU.add), [('xf', j), ('sgF', q)], [('xf', j)])
        for j in range(n):
            i = i0 + j
            rmsnorm_tile(xg[j][:], yout[j % 2][:], gfin[:], ('xf', j), ('yout', j % 2), j % 2, gkey='gfin')
            S.dma('sp', y_o[i * 128:(i + 1) * 128, :], yout[j % 2][:], R=[('yout', j % 2)], W=[('y', i)])
    S.finish()
    CONSTS = dict(CONSTS)
    CONSTS.update(WCONSTS)
    CONSTS.update(RCONSTS)
    return nc, CONSTS


_CACHE = {}


def _prep(inp, CONSTS, cores=range(8)):
    f32 = np.float32
    rope = _rope_table()
    ident = np.eye(128, dtype=f32)
    xp = np.asarray(inp['x_prompt'], f32)
    xs = np.asarray(inp['x_sample'], f32)
    prm = np.concatenate([np.asarray(inp[k], f32).reshape(-1) for k in
                          ('wkv_mu', 'wkv_w0', 'wkv_a0', 'wkv_k_k', 'wkv_k_a', 'wkv_r_k', 'wkv_ln_w', 'wkv_ln_b')])[None, :]
    W = {k: np.ascontiguousarray(np.asarray(inp[k], f32)[0]) for k in
         ('w_oa', 'w_ob', 'w_out', 'e_gate', 'e_up', 'e_down', 'w_ple_gate', 'w_ple_proj')}
    for k in ('g_ffn', 'g_ple'):
        W[k] = np.ascontiguousarray(np.asarray(inp[k], f32).reshape(1, D))
    W['g_final'] = np.ascontiguousarray(np.asarray(inp['g_final'], f32).reshape(1, D))
    W['wr'] = np.ascontiguousarray(np.concatenate([np.asarray(inp['router_g_w'], f32)[0], np.asarray(inp['router_e_w'], f32)[0]], 1))
    W['rbias'] = np.ascontiguousarray(np.concatenate([np.asarray(inp['router_g_b'], f32)[0], np.asarray(inp['router_e_b'], f32)[0]])[None, :])
    pp_ = np.asarray(inp['p_prompt'], f32)[0]
    ps_ = np.asarray(inp['p_sample'], f32)[0]
    import os
    if 'E' not in os.environ.get('KSTAGES', 'E').split(','):
        for k in ('e_gate', 'e_up', 'e_down'):
            W[k] = W[k][0:1]
    in_maps = []
    for c in cores:
        hf = c // 4
        pc = np.zeros((TL, DPLE), f32)
        pc[:1024] = pp_[c % 4][1024 * hf:1024 * hf + 1024]
        pc[1024:1088] = ps_[16 * c:16 * c + 16].reshape(64, DPLE)
        xh = np.zeros((TL, D), f32)
        xh[:1024] = xp[c % 4][1024 * hf:1024 * hf + 1024]
        xh[1024:1088] = xs[16 * c:16 * c + 16].reshape(64, D)
        ridx = np.zeros((128, NTL), np.int32)
        for l in range(NTL):
            gt = 8 * hf + l if l < 8 else 16
            ridx[:, l] = gt * 128 + np.arange(128)
        xc = np.zeros((T, D), f32)
        xc[:2048] = xp[c % 4]
        xc[2048:2112] = xs[16 * c:16 * c + 16].reshape(64, D)
        m = {
            'x': xc,
            'st_ret': np.ascontiguousarray(inp['state_ret'][0, 16 * c:16 * c + 16]),
            'g_mix': np.ascontiguousarray(inp['g_mix']),
            'w_in': np.ascontiguousarray(inp['w_in'][0]),
            'rope': rope,
            'ident': ident,
            'prm': prm,
            'st_shift': np.ascontiguousarray(inp['state_shift'][0, 16 * c:16 * c + 16]),
            'st_wkv': np.ascontiguousarray(inp['state_wkv'][0, 16 * c:16 * c + 16]),
            'wkv_w2': np.ascontiguousarray(inp['wkv_w2'][0]),
            'wkv_a2': np.ascontiguousarray(inp['wkv_a2'][0]),
            'wkv_g2': np.ascontiguousarray(inp['wkv_g2'][0]),
            'w_oa': W['w_oa'], 'w_ob': W['w_ob'], 'w_out': W['w_out'], 'g_ffn': W['g_ffn'], 'wr': W['wr'], 'rbias': W['rbias'],
            'e_gate': W['e_gate'], 'e_up': W['e_up'], 'e_down': W['e_down'], 'g_ple': W['g_ple'],
            'w_ple_gate': W['w_ple_gate'], 'w_ple_proj': W['w_ple_proj'], 'g_final': W['g_final'],
            'p_in': pc, 'x_h': xh, 'ridx': ridx,
        }
        m.update(CONSTS)
        in_maps.append(m)
    return in_maps


def kernel(**inp):
    f32 = np.float32
    nc, CONSTS = build()
    in_maps = _prep(inp, CONSTS)
    res = run_bass_kernel_spmd(nc, in_maps, core_ids=list(range(8)))
    R = res.results
    y_p = np.stack([np.concatenate([R[c]['y'][:1024], R[c + 4]['y'][:1024]], 0) for c in range(4)], 0)
    y_s = np.concatenate([R[c]['y'][1024:1088].reshape(16, 4, D) for c in range(8)], 0)
    ret_p = np.stack([R[c]['ret_p'] for c in range(4)], 0)[None]
    ret_s = np.concatenate([R[c]['ret_s'] for c in range(8)], 0)[None]
    wkv_p = np.stack([R[c]['wkv_p'] for c in range(4)], 0)[None]
    sh_p = np.stack([R[c]['sh_p'][0] for c in range(4)], 0)[None]
    wkv_s = np.concatenate([R[c]['wkv_s'] for c in range(8)], 0)[None]
    sh_s = np.concatenate([R[c]['sh_s'] for c in range(8)], 0)[None]
    return (y_p, y_s, ret_p, wkv_p, sh_p, ret_s, wkv_s, sh_s)
```
